# Optimizing a Trainium2 kernel written in Bass

```python
import jax
import jax.numpy as jnp
from jax import lax
import numpy as np

D_MODEL = 1024
BATCH = 8
SEQ = 4096
DEPTH = 2

CHUNK = 64
HGRN_WIDTH = D_MODEL // 2
CONV_WIDTH = D_MODEL - HGRN_WIDTH
HGRN_EXPAND = 128
HGRN_HEADS = HGRN_WIDTH // HGRN_EXPAND
HGRN_HEAD_V = HGRN_WIDTH // HGRN_HEADS
CONV_KERNEL = 31
IN_COLS = 4 * HGRN_WIDTH + 2 * CONV_WIDTH
N_EXPERTS = 16
N_EXPERT_GROUPS = 4
EXPERTS_PER_GROUP = N_EXPERTS // N_EXPERT_GROUPS
TOP_K = 2
D_EXPERT = D_MODEL // 2
ALPHA = (2 * DEPTH) ** 0.25
BETA = (8 * DEPTH) ** -0.25
EPS = 1e-5

kernel_name = "hybrid_hgrn2_conformer_conv_grouped_moe_deepnorm"


def _layernorm(x):
    xf = x.astype(jnp.float32)
    mu = jnp.mean(xf, axis=-1, keepdims=True)
    var = jnp.mean(jnp.square(xf - mu), axis=-1, keepdims=True)
    return (xf - mu) * lax.rsqrt(var + EPS)


def _modulate(x, shift, scale):
    return (_layernorm(x) * (1.0 + scale) + shift).astype(x.dtype)


def _post_ln(z, g, b):
    return (_layernorm(z) * g + b).astype(z.dtype)


def _layer_lower_bounds(lb_param):
    p = jax.nn.softmax(lb_param.astype(jnp.float32), axis=0)
    cs = jnp.cumsum(p, axis=0)
    return cs - cs[0:1]


def _hgrn2(q, f_logit, i, lb):
    B, T = q.shape[0], q.shape[1]
    n = T // CHUNK
    f32 = jnp.float32
    lbf = lb.astype(f32)
    g = jnp.logaddexp(jnp.log(lbf), jnp.log1p(-lbf) + jax.nn.log_sigmoid(f_logit.astype(f32)))
    k = -jnp.expm1(g)
    qf = jax.nn.silu(q.astype(f32))
    vf = i.astype(f32)

    def to_chunks(t):
        return t.reshape(B, n, CHUNK, t.shape[2], t.shape[3]).transpose(1, 0, 3, 2, 4)

    causal = jnp.tril(jnp.ones((CHUNK, CHUNK), dtype=bool))

    def step(S, inp):
        qc, kc, vc, gc = inp
        b = jnp.cumsum(gc, axis=2)
        diff = b[:, :, :, None, :] - b[:, :, None, :, :]
        decay = jnp.exp(jnp.where(causal[:, :, None], diff, -jnp.inf))
        scores = jnp.einsum('bhtk,bhsk,bhtsk->bhts', qc, kc, decay)
        o = (jnp.einsum('bhts,bhsv->bhtv', scores, vc)
             + jnp.einsum('bhtk,bhkv->bhtv', qc * jnp.exp(b), S))
        b_last = b[:, :, -1, :]
        S_new = (jnp.exp(b_last)[..., None] * S
                 + jnp.einsum('bhsk,bhsv->bhkv', kc * jnp.exp(b_last[:, :, None, :] - b), vc))
        return S_new, o

    S0 = jnp.zeros((B, HGRN_HEADS, HGRN_EXPAND, HGRN_HEAD_V), f32)
    _, o = lax.scan(step, S0, (to_chunks(qf), to_chunks(k), to_chunks(vf), to_chunks(g)))
    return o.transpose(1, 0, 3, 2, 4).reshape(B, T, HGRN_HEADS, HGRN_HEAD_V)


def _mixer(u, w_in, b_in, lb, norm_w, conv_w, conv_b, conv_g, conv_bb, w_out, b_out):
    B, T, _ = u.shape
    dt = u.dtype
    proj = u @ w_in + b_in
    H, C = HGRN_WIDTH, CONV_WIDTH
    q, f, i, og, ca, cgt = jnp.split(proj, [H, 2 * H, 3 * H, 4 * H, 4 * H + C], axis=-1)
    hs = (B, T, HGRN_HEADS, HGRN_EXPAND)
    o = _hgrn2(q.reshape(hs), f.reshape(hs), i.reshape(B, T, HGRN_HEADS, HGRN_HEAD_V),
               lb.reshape(HGRN_HEADS, HGRN_EXPAND))
    o = o * lax.rsqrt(jnp.mean(jnp.square(o), axis=-1, keepdims=True) + EPS) * norm_w
    h_a = (o.reshape(B, T, H) * jax.nn.silu(og.astype(jnp.float32))).astype(dt)
    a = ca * jax.nn.sigmoid(cgt)
    a = lax.conv_general_dilated(a, conv_w[:, None, :], window_strides=(1,),
                                 padding=[(CONV_KERNEL - 1, 0)],
                                 dimension_numbers=('NWC', 'WIO', 'NWC'),
                                 feature_group_count=CONV_WIDTH) + conv_b
    h_b = jax.nn.silu(_layernorm(a) * conv_g + conv_bb).astype(dt)
    return jnp.concatenate([h_a, h_b], axis=-1) @ w_out + b_out


def _moe(u, router_w, router_bias, w_gate, w_up, w_down):
    B, T, D = u.shape
    dt = u.dtype
    t = u.reshape(B * T, D)
    s = jax.nn.sigmoid((t @ router_w).astype(jnp.float32))
    sb = s + router_bias.astype(jnp.float32)
    grp_score = jnp.sum(lax.top_k(sb.reshape(-1, N_EXPERT_GROUPS, EXPERTS_PER_GROUP), TOP_K)[0], axis=-1)
    gsel = jnp.argmax(grp_score, axis=-1)
    in_group = (jnp.arange(N_EXPERTS) // EXPERTS_PER_GROUP)[None, :] == gsel[:, None]
    _, idx = lax.top_k(jnp.where(in_group, sb, -jnp.inf), TOP_K)
    w_sel = jnp.take_along_axis(s, idx, axis=-1)
    w_sel = w_sel / jnp.sum(w_sel, axis=-1, keepdims=True)
    combine = jnp.sum(jax.nn.one_hot(idx, N_EXPERTS, dtype=jnp.float32) * w_sel[..., None], axis=1).astype(dt)
    y = jnp.zeros_like(t)
    for e in range(N_EXPERTS):
        h = jax.nn.silu(t @ w_gate[e]) * (t @ w_up[e])
        y = y + combine[:, e:e + 1] * (h @ w_down[e])
    return y.reshape(B, T, D)


def setup_inputs(seed: int = 0) -> dict:
    key = jax.random.key(seed)
    ks = jax.random.split(key, 24)
    f32 = jnp.float32
    D = D_MODEL

    def nrm(k, shape, s):
        return jax.random.normal(k, shape, f32) * s

    return {
        "x": nrm(ks[0], (BATCH, SEQ, D), 1.0),
        "c": nrm(ks[1], (BATCH, D), 1.0),
        "ada_w": nrm(ks[2], (DEPTH, D, 6 * D), 0.5 * D ** -0.5),
        "ada_b": nrm(ks[3], (DEPTH, 6 * D), 0.01),
        "w_in": nrm(ks[4], (DEPTH, D, IN_COLS), D ** -0.5),
        "b_in": nrm(ks[5], (DEPTH, IN_COLS), 0.01),
        "hgrn_lb": nrm(ks[6], (DEPTH, HGRN_WIDTH), 0.5),
        "hgrn_norm_w": 1.0 + nrm(ks[7], (DEPTH, HGRN_HEAD_V), 0.01),
        "conv_w": nrm(ks[8], (DEPTH, CONV_KERNEL, CONV_WIDTH), CONV_KERNEL ** -0.5),
        "conv_b": nrm(ks[9], (DEPTH, CONV_WIDTH), 0.01),
        "conv_ln_g": 1.0 + nrm(ks[10], (DEPTH, CONV_WIDTH), 0.01),
        "conv_ln_b": nrm(ks[11], (DEPTH, CONV_WIDTH), 0.01),
        "w_out": nrm(ks[12], (DEPTH, HGRN_WIDTH + CONV_WIDTH, D), BETA * D ** -0.5),
        "b_out": nrm(ks[13], (DEPTH, D), 0.01),
        "ln1_g": 1.0 + nrm(ks[14], (DEPTH, D), 0.01),
        "ln1_b": nrm(ks[15], (DEPTH, D), 0.01),
        "router_w": nrm(ks[16], (D, N_EXPERTS), D ** -0.5),
        "router_bias": nrm(ks[17], (N_EXPERTS,), 0.01),
        "w_gate": nrm(ks[18], (DEPTH, N_EXPERTS, D, D_EXPERT), D ** -0.5),
        "w_up": nrm(ks[19], (DEPTH, N_EXPERTS, D, D_EXPERT), D ** -0.5),
        "w_down": nrm(ks[20], (DEPTH, N_EXPERTS, D_EXPERT, D), BETA * D_EXPERT ** -0.5),
        "ln2_g": 1.0 + nrm(ks[21], (DEPTH, D), 0.01),
        "ln2_b": nrm(ks[22], (DEPTH, D), 0.01),
    }


def reference(x, c, ada_w, ada_b, w_in, b_in, hgrn_lb, hgrn_norm_w, conv_w, conv_b,
              conv_ln_g, conv_ln_b, w_out, b_out, ln1_g, ln1_b, router_w, router_bias,
              w_gate, w_up, w_down, ln2_g, ln2_b):
    lb_all = _layer_lower_bounds(hgrn_lb)
    cond = jax.nn.silu(c)
    for l in range(DEPTH):
        mod = cond @ ada_w[l] + ada_b[l]
        sh1, sc1, g1, sh2, sc2, g2 = jnp.split(mod[:, None, :], 6, axis=-1)
        u = _modulate(x, sh1, sc1)
        y = _mixer(u, w_in[l], b_in[l], lb_all[l], hgrn_norm_w[l], conv_w[l], conv_b[l],
                   conv_ln_g[l], conv_ln_b[l], w_out[l], b_out[l])
        x = _post_ln(ALPHA * x + g1 * y, ln1_g[l], ln1_b[l])
        u = _modulate(x, sh2, sc2)
        y = _moe(u, router_w, router_bias, w_gate[l], w_up[l], w_down[l])
        x = _post_ln(ALPHA * x + g2 * y, ln2_g[l], ln2_b[l])
    return x
```

```python
import numpy as np
from contextlib import ExitStack
import concourse.bass as bass
import concourse.mybir as mybir
from concourse.bass_utils import run_bass_kernel_spmd

F32 = mybir.dt.float32
BF16 = mybir.dt.bfloat16
U8 = mybir.dt.uint8
AF = mybir.ActivationFunctionType
ALU = mybir.AluOpType
AX = mybir.AxisListType

T = 4096
D = 1024
NT = 32
NB = 8
NE = 16
ALPHA = 4.0 ** 0.25
EPS = 1e-5
NCORES = 8


class Prog:
    def __init__(self, nc):
        self.nc = nc
        self.ops = []
        self.eng = {"pe": nc.tensor, "act": nc.scalar, "dve": nc.vector,
                    "pool": nc.gpsimd, "sp": nc.sync}

    def add(self, eng, fn, r=(), w=()):
        self.ops.append(dict(eng=eng, fn=fn, r=tuple(r), w=tuple(w), dma=None, bar=False))

    def dma(self, q, fn, r=(), w=(), sem=None):
        assert sem is not None
        self.ops.append(dict(eng=q, fn=fn, r=tuple(r), w=tuple(w), dma=sem, bar=False))

    def barrier(self):
        self.ops.append(dict(eng=None, fn=None, r=(), w=(), dma=None, bar=True))

    def emit(self, stack, final_keys=()):
        nc = self.nc
        ops = self.ops
        n = len(ops)
        last_writer = {}
        readers = {}
        deps = [None] * n
        last_on_eng = {}
        dmas_since = []
        pend = {e: set() for e in self.eng}
        for i, op in enumerate(ops):
            if op["bar"]:
                d = set(last_on_eng.values()) | set(dmas_since)
                for e in self.eng:
                    pend[e] |= d
                dmas_since = []
                deps[i] = set()
                continue
            d = set()
            for k in op["r"]:
                if k in last_writer:
                    d.add(last_writer[k])
            for k in op["w"]:
                if k in last_writer:
                    d.add(last_writer[k])
                d.update(readers.get(k, ()))
            e = op["eng"]
            if pend[e]:
                d |= pend[e]
                pend[e] = set()
            d.discard(i)
            best = {}
            dd = set()
            for j in d:
                oj = ops[j]
                if oj["dma"] is not None:
                    dd.add(j)
                elif best.get(oj["eng"], -1) < j:
                    best[oj["eng"]] = j
            dd.update(best.values())
            deps[i] = dd
            for k in op["r"]:
                readers.setdefault(k, []).append(i)
            for k in op["w"]:
                last_writer[k] = i
                readers[k] = []
            last_on_eng[e] = i
            if op["dma"] is not None:
                dmas_since.append(i)
        need_sig = [False] * n
        for i, op in enumerate(ops):
            if op["bar"]:
                continue
            for j in deps[i]:
                oj = ops[j]
                if oj["dma"] is not None:
                    continue
                if oj["eng"] == "pe" and op["eng"] == "pe" and op["dma"] is None:
                    continue
                need_sig[j] = True
        final_deps = set(last_writer[k] for k in final_keys)
        esem = {e: stack.enter_context(nc.semaphore("s_" + e)) for e in self.eng}
        dsem = {}
        dcount = {}
        ecount = {e: 0 for e in self.eng}
        waited = {e: {} for e in self.eng}
        sig = [None] * n
        nwaits = 0
        for i, op in enumerate(ops):
            if op["bar"]:
                continue
            e = op["eng"]
            eng = self.eng[e]
            for j in sorted(deps[i]):
                oj = ops[j]
                if oj["dma"] is None and oj["eng"] == "pe" and e == "pe" and op["dma"] is None:
                    continue
                sname, val = sig[j]
                if waited[e].get(sname, 0) >= val:
                    continue
                semh = esem[sname] if sname in esem else dsem[sname]
                eng.wait_ge(semh, val)
                nwaits += 1
                waited[e][sname] = val
            inst = op["fn"]()
            if op["dma"] is not None:
                key = op["dma"]
                if key not in dsem:
                    dsem[key] = stack.enter_context(nc.semaphore("d_%d" % len(dsem)))
                    dcount[key] = 0
                dcount[key] += 16
                inst.then_inc(dsem[key], 16)
                sig[i] = (key, dcount[key])
            elif need_sig[i]:
                ecount[e] += 1
                inst.then_inc(esem[e], 1)
                sig[i] = (e, ecount[e])
        eng = self.eng["sp"]
        for j in sorted(final_deps):
            sname, val = sig[j]
            semh = esem[sname] if sname in esem else dsem[sname]
            eng.wait_ge(semh, val)
        self.stats = dict(nops=n, nwaits=nwaits, ecount=ecount, ndsem=len(dsem))


class SBAlloc:
    def __init__(self, big, nbytes):
        self.big = big
        self.cap = nbytes
        self.off = 0
        self.peak = 0

    def mark(self):
        return self.off

    def reset(self, m):
        self.off = m

    def __call__(self, free_shape, dt, parts=128):
        esz = {F32: 4, BF16: 2, U8: 1, mybir.dt.int32: 4, mybir.dt.uint32: 4}[dt]
        nel = int(np.prod(free_shape))
        nbytes = (nel * esz + 63) // 64 * 64
        assert self.off + nbytes <= self.cap, ("SBUF overflow", self.off, nbytes, self.cap)
        ap = self.big[0:parts, self.off:self.off + nel * esz].bitcast(dt)
        self.off += nbytes
        self.peak = max(self.peak, self.off)
        if len(free_shape) == 2:
            ap = ap.rearrange("p (a b) -> p a b", a=free_shape[0], b=free_shape[1])
        elif len(free_shape) == 3:
            ap = ap.rearrange("p (a b c) -> p a b c", a=free_shape[0], b=free_shape[1], c=free_shape[2])
        return ap


def build(stage=99, dbg=False):
    nc = bass.Bass("TRN2", target_bir_lowering=False)
    dk = "ExternalOutput" if dbg else "Internal"

    def din(name, shape):
        return nc.dram_tensor(name, list(shape), F32, kind="ExternalInput").ap()

    x_d = din("x", [T, D])
    c_d = din("c", [D])
    ada_w = din("ada_w", [2, D, 6 * D])
    ada_b = din("ada_b", [2, 6 * D])
    w_in = din("w_in", [2, D, 3072])
    b_in = din("b_in", [2, 3072])
    hgrn_lb = din("hgrn_lb", [2, 512])
    hgrn_nw = din("hgrn_norm_w", [2, 128])
    conv_w = din("conv_w", [2, 31, 512])
    conv_b = din("conv_b", [2, 512])
    conv_g = din("conv_ln_g", [2, 512])
    conv_bb = din("conv_ln_b", [2, 512])
    w_out = din("w_out", [2, D, D])
    b_out = din("b_out", [2, D])
    ln1_g = din("ln1_g", [2, D])
    ln1_b = din("ln1_b", [2, D])
    router_w = din("router_w", [D, NE])
    router_bias = din("router_bias", [NE])
    w_gate = din("w_gate", [2, NE, D, 512])
    w_up = din("w_up", [2, NE, D, 512])
    w_down = din("w_down", [2, NE, 512, D])
    ln2_g = din("ln2_g", [2, D])
    ln2_b = din("ln2_b", [2, D])
    out_d = nc.dram_tensor("out", [T, D], F32, kind="ExternalOutput").ap()
    hTd = nc.dram_tensor("hTd", [NB, 128, 8, 512], BF16, kind=dk).ap()
    x1d = nc.dram_tensor("x1d", [T, D], F32, kind=dk).ap()
    u2Td = nc.dram_tensor("u2Td", [NB, 128, 8, 512], BF16, kind=dk).ap()
    xmid = nc.dram_tensor("xmid", [T, D], F32, kind=dk).ap()
    logd = nc.dram_tensor("logd", [128, NT * NE], F32, kind=dk).ap()
    combd = nc.dram_tensor("combd", [128, NT * NE], F32, kind=dk).ap()
    modTd = nc.dram_tensor("modTd", [128, 32], F32, kind=dk).ap()

    SB_BYTES = 207 * 1024
    with ExitStack() as top:
        big = top.enter_context(nc.sbuf_tensor("big", [128, SB_BYTES], U8))
        ps = top.enter_context(nc.psum_tensor("ps", [128, 8, 512], F32))
        sb = SBAlloc(big, SB_BYTES)
        P = Prog(nc)

        def bank(b):
            return ps[:, b, :]

        def bankb(b):
            return ps[:, b, :].bitcast(BF16)

        def bk(b):
            return ("ps", b)

        def mm(out, lhsT, rhs, start, stop, r, w):
            P.add("pe", lambda: nc.tensor.matmul(out, lhsT=lhsT, rhs=rhs, start=start, stop=stop), r=r, w=w)

        def tr(out, in_, ident, r, w):
            P.add("pe", lambda: nc.tensor.transpose(out=out, in_=in_, identity=ident), r=r, w=w)

        def act(out, in_, func, r, w, bias=0.0, scale=1.0, accum=None):
            if accum is None:
                P.add("act", lambda: nc.scalar.activation(out=out, in_=in_, func=func, bias=bias, scale=scale), r=r, w=w)
            else:
                P.add("act", lambda: nc.scalar.activation(out=out, in_=in_, func=func, bias=bias, scale=scale, accum_out=accum), r=r, w=w)

        def tt(eng, out, in0, in1, op, r, w):
            e = nc.vector if eng == "dve" else nc.gpsimd
            P.add(eng, lambda: e.tensor_tensor(out=out, in0=in0, in1=in1, op=op), r=r, w=w)

        def ts(eng, out, in0, s1, s2, op0, op1, r, w):
            e = nc.vector if eng == "dve" else nc.gpsimd
            if op1 is None:
                P.add(eng, lambda: e.tensor_scalar(out=out, in0=in0, scalar1=s1, scalar2=None, op0=op0), r=r, w=w)
            else:
                P.add(eng, lambda: e.tensor_scalar(out=out, in0=in0, scalar1=s1, scalar2=s2, op0=op0, op1=op1), r=r, w=w)

        def stt(out, in0, scalar, in1, op0, op1, r, w):
            P.add("dve", lambda: nc.vector.scalar_tensor_tensor(out=out, in0=in0, scalar=scalar, in1=in1, op0=op0, op1=op1), r=r, w=w)

        def cp(eng, out, in_, r, w):
            if eng == "act":
                P.add("act", lambda: nc.scalar.copy(out=out, in_=in_), r=r, w=w)
            else:
                e = nc.vector if eng == "dve" else nc.gpsimd
                P.add(eng, lambda: e.tensor_copy(out=out, in_=in_), r=r, w=w)

        def memset(eng, ap, val, w):
            e = nc.vector if eng == "dve" else nc.gpsimd
            P.add(eng, lambda: e.memset(ap, val), w=w)

        def dma(q, out, in_, r, w, sem, slow=False):
            e = {"sp": nc.sync, "pool": nc.gpsimd, "act": nc.scalar}[q]
            if slow:
                def f():
                    with nc.allow_non_contiguous_dma(reason="small strided load"):
                        return e.dma_start(out=out, in_=in_)
                P.dma(q, f, r=r, w=w, sem=sem)
            else:
                P.dma(q, lambda: e.dma_start(out=out, in_=in_), r=r, w=w, sem=sem)

        def ln_stats(src, stats, mv, rstd, nmr, tag, rkeys):
            for h in range(2):
                P.add("dve", (lambda h=h: nc.vector.bn_stats(out=stats[:, h, :], in_=src[:, h * 512:(h + 1) * 512])),
                      r=rkeys, w=[(tag, "st", h)])
            P.add("dve", lambda: nc.vector.bn_aggr(out=mv, in_=stats.rearrange("p a b -> p (a b)")),
                  r=[(tag, "st", 0), (tag, "st", 1)], w=[(tag, "mv")])
            act(rstd, mv[:, 1:2], AF.Sqrt, r=[(tag, "mv")], w=[(tag, "rstd")], bias=EPS)
            P.add("dve", lambda: nc.vector.reciprocal(out=rstd, in_=rstd), r=[(tag, "rstd")], w=[(tag, "rstd")])
            if nmr is not None:
                ts("dve", nmr, mv[:, 0:1], rstd, -1.0, ALU.mult, ALU.mult, r=[(tag, "mv"), (tag, "rstd")], w=[(tag, "nmr")])

        identF = sb([128], F32)
        identB = sb([128], BF16)
        Lincl = sb([128], F32)
        M1 = sb([128], F32)
        maskB = sb([128], BF16)
        onesM = sb([128], F32)
        ones_row = sb([128], BF16, parts=1)
        condT = sb([8], F32)
        cond_bc = sb([8, 128], F32)
        biasB = sb([NE], F32)
        eps_t = sb([1], F32)

        memset("pool", identF, 1.0, w=["identF"])
        P.add("pool", lambda: nc.gpsimd.affine_select(out=identF, in_=identF, pattern=[[-1, 128]], compare_op=ALU.is_equal,
                                                      fill=0.0, base=0, channel_multiplier=1), r=["identF"], w=["identF"])
        cp("pool", identB, identF, r=["identF"], w=["identB"])
        memset("pool", Lincl, 1.0, w=["Lincl"])
        P.add("pool", lambda: nc.gpsimd.affine_select(out=Lincl, in_=Lincl, pattern=[[1, 128]], compare_op=ALU.is_ge,
                                                      fill=0.0, base=0, channel_multiplier=-1), r=["Lincl"], w=["Lincl"])
        memset("pool", Lincl[0:64, 64:128], 0.0, w=["Lincl"])
        cp("pool", maskB, Lincl, r=["Lincl"], w=["maskB"])
        memset("pool", M1, 1.0, w=["M1"])
        P.add("pool", lambda: nc.gpsimd.affine_select(out=M1, in_=M1, pattern=[[-1, 128]], compare_op=ALU.is_ge,
                                                      fill=0.0, base=-1, channel_multiplier=1), r=["M1"], w=["M1"])
        memset("pool", M1[64:128, 0:64], 0.0, w=["M1"])
        memset("pool", onesM, 1.0 / 512.0, w=["onesM"])
        memset("pool", ones_row, 1.0, w=["ones_row"])
        dma("sp", condT, c_d.rearrange("(k p) -> p k", p=128), r=[], w=["condT"], sem="condT", slow=True)
        act(condT, condT, AF.Silu, r=["condT"], w=["condT"])
        for k in range(8):
            ts("pool", cond_bc[:, k, :], onesM, condT[:, k:k + 1], 512.0, ALU.mult, ALU.mult, r=["onesM", "condT"], w=["cond_bc"])
        dma("sp", biasB, router_bias.partition_broadcast(128), r=[], w=["biasB"], sem="biasB")
        perm_mark = sb.mark()

        final_keys = []
        nlayers = 2
        for l in range(nlayers):
            sb.reset(perm_mark)
            xsrc = x_d if l == 0 else xmid
            xdst = xmid if l == 0 else out_d
            modT = sb([4, 8], F32)
            g1B = sb([D], F32)
            g2B = sb([D], F32)
            logits = sb([NT * NE], F32)
            comb = sb([NT * NE], F32)
            layer_mark = sb.mark()

            adaw = [sb([8, 512], F32) for _ in range(2)]
            adab = [sb([512], F32) for _ in range(2)]
            piece = [sb([512], F32) for _ in range(2)]
            kind_of = {0: 0, 1: 0, 2: 1, 3: 1, 6: 2, 7: 2, 8: 3, 9: 3}
            for j in range(12):
                s2 = j % 2
                dma("sp", adaw[s2], ada_w[l, :, j * 512:(j + 1) * 512].rearrange("(k p) n -> p k n", p=128),
                    r=[], w=[("adaw", s2)], sem=("adaw", s2))
                dma("sp", adab[s2], ada_b[l, j * 512:(j + 1) * 512].partition_broadcast(128), r=[], w=[("adab", s2)], sem=("adab", s2))
                for kc in range(8):
                    mm(bank(s2), cond_bc[:, kc, :], adaw[s2][:, kc, :], kc == 0, kc == 7,
                       r=["cond_bc", ("adaw", s2)], w=[bk(s2)])
                if j in (4, 5):
                    dst = g1B[:, (j - 4) * 512:(j - 3) * 512]
                    tt("dve", dst, bank(s2), adab[s2], ALU.add, r=[bk(s2), ("adab", s2)], w=["g1B"])
                elif j in (10, 11):
                    dst = g2B[:, (j - 10) * 512:(j - 9) * 512]
                    tt("dve", dst, bank(s2), adab[s2], ALU.add, r=[bk(s2), ("adab", s2)], w=["g2B"])
                else:
                    tt("dve", piece[s2], bank(s2), adab[s2], ALU.add, r=[bk(s2), ("adab", s2)], w=[("piece", s2)])
                    for b in range(4):
                        tr(bank(2 + s2)[:, b * 128:(b + 1) * 128], piece[s2][:, b * 128:(b + 1) * 128], identF,
                           r=[("piece", s2), "identF"], w=[bk(2 + s2)])
                    kd = kind_of[j]
                    half = j % 2
                    dstm = modT[:, kd, half * 4:half * 4 + 4]
                    src = bank(2 + s2).rearrange("p (b c) -> p b c", c=128)[:, :, 0]
                    if kd in (1, 3):
                        ts("dve", dstm, src, 1.0, None, ALU.add, None, r=[bk(2 + s2)], w=["modT"])
                    else:
                        cp("dve", dstm, src, r=[bk(2 + s2)], w=["modT"])
            if dbg and l == 0:
                dma("sp", modTd, modT.rearrange("p a b -> p (a b)"), r=["modT"], w=["modTd"], sem="modTd")
                final_keys.append("modTd")
            P.barrier()
            if stage == 0:
                break

            sb.reset(layer_mark)
            win = sb([8, 3072], BF16)
            dg = sb([124, 128], BF16)
            lbB = sb([512], F32)
            omlbB = sb([512], F32)
            nwB = sb([512], F32)
            b_inT = sb([12], F32)
            brow = sb([1536], BF16, parts=1)
            cwT = sb([4, 31], F32)
            cbT = sb([4], F32)
            cgT = sb([4], F32)
            cbbT = sb([4], F32)
            xt = [sb([D], F32) for _ in range(2)]
            xn1 = sb([D], F32)
            xn = [xn1, xn1]
            uT = sb([8, 512], BF16)
            qT = sb([4, 512], F32)
            zs = sb([512], F32)
            tmp = sb([512], F32)
            gt = sb([512], F32)
            kk = sb([512], F32)
            ec = sb([512], F32)
            sog = sb([512], F32)
            gate = sb([512], F32)
            khat = sb([512], BF16)
            vt = sb([512], BF16)
            eb = sb([4, 128], F32)
            enc = sb([4, 128], F32)
            qtA = sb([4, 128], BF16)
            qtB = sb([4, 128], BF16)
            qp = sb([4, 128], BF16)
            khT = sb([4, 128], BF16)
            AT = sb([4, 128], BF16)
            S = sb([4, 128], F32)
            Sb0 = sb([4, 128], BF16)
            Sb1 = sb([4, 128], BF16)
            ha = sb([512], BF16)
            abuf = sb([4, 544], BF16)
            sg = sb([512], F32)
            ac = sb([4, 512], F32)
            meanS = sb([512], F32)
            varS = sb([512], F32)
            rstdB = sb([512], F32)
            t1 = sb([512], F32)
            ac2 = t1
            sq = ec
            cw_tm = sg[0:31, :]
            hT = sb([8, 512], BF16)
            stats = [sb([2, 6], F32) for _ in range(2)]
            mv = [sb([2], F32) for _ in range(2)]
            rstd = [sb([1], F32) for _ in range(2)]
            ss = sb([4], F32)
            rs = sb([4], F32)
            junk = sb([128], F32)
            lb2 = ac[:, 0:2, :]

            for kc in range(8):
                dma("pool", win[:, kc, :], w_in[l, kc * 128:(kc + 1) * 128, :], r=[], w=[("win", kc)], sem=("win", kc))
            dma("sp", b_inT[:, 0:4], b_in[l, 0:512].rearrange("(c p) -> p c", p=128), r=[], w=["b_inT"], sem="b_inT0", slow=True)
            dma("sp", b_inT[:, 4:12], b_in[l, 2048:3072].rearrange("(c p) -> p c", p=128), r=[], w=["b_inT"], sem="b_inT1", slow=True)
            dma("pool", brow, b_in[l:l + 1, 512:2048], r=[], w=["brow"], sem="brow")
            if l == 0:
                memset("pool", lbB, 0.0, w=["lbB"])
                memset("pool", omlbB, 1.0, w=["omlbB"])
            else:
                dma("sp", lb2.rearrange("p a b -> p (a b)"), hgrn_lb.rearrange("a b -> (a b)").partition_broadcast(128),
                    r=[], w=[("ac", 0), ("ac", 1)], sem="lb2")
                act(lb2, lb2, AF.Exp, r=[("ac", 0), ("ac", 1)], w=[("ac", 0), ("ac", 1)])
                tt("dve", tmp, lb2[:, 0, :], lb2[:, 1, :], ALU.add, r=[("ac", 0), ("ac", 1)], w=["tmp"])
                P.add("dve", lambda: nc.vector.reciprocal(out=tmp, in_=tmp), r=["tmp"], w=["tmp"])
                tt("dve", lbB, lb2[:, 1, :], tmp, ALU.mult, r=[("ac", 1), "tmp"], w=["lbB"])
                tt("dve", omlbB, lb2[:, 0, :], tmp, ALU.mult, r=[("ac", 0), "tmp"], w=["omlbB"])
            for h in range(4):
                dma("sp", nwB[:, h * 128:(h + 1) * 128], hgrn_nw[l].partition_broadcast(128), r=[], w=["nwB"], sem=("nwB", h))
            dma("sp", cw_tm, conv_w[l], r=[], w=["sg"], sem="cw_tm")
            for ch in range(4):
                tr(bank(0)[:, ch * 32:ch * 32 + 31], cw_tm[0:31, ch * 128:(ch + 1) * 128], identF[0:31, 0:31],
                   r=["sg", "identF"], w=[bk(0)])
                cp("dve", cwT[:, ch, :], bank(0)[:, ch * 32:ch * 32 + 31], r=[bk(0)], w=["cwT"])
            for ch in range(4):
                for j in range(31):
                    ts("pool", dg[:, ch * 31 + j, :], identF, cwT[:, ch, j:j + 1], None, ALU.mult, None,
                       r=["identF", "cwT"], w=["dg"])
            dma("sp", cbT, conv_b[l].rearrange("(c p) -> p c", p=128), r=[], w=["cbT"], sem="cbT", slow=True)
            dma("sp", cgT, conv_g[l].rearrange("(c p) -> p c", p=128), r=[], w=["cgT"], sem="cgT", slow=True)
            dma("sp", cbbT, conv_bb[l].rearrange("(c p) -> p c", p=128), r=[], w=["cbbT"], sem="cbbT", slow=True)
            memset("pool", abuf, 0.0, w=[("abuf", ch) for ch in range(4)])
            memset("pool", S, 0.0, w=["S"])
            memset("pool", Sb0, 0.0, w=["Sb0"])
            memset("pool", qtA, 0.0, w=["qtA"])
            memset("pool", qtB, 0.0, w=["qtB"])

            for blk in range(NB):
                for t4 in range(4):
                    t = blk * 4 + t4
                    s2 = t % 2
                    dma("sp", xt[s2], xsrc[t * 128:(t + 1) * 128, :], r=[], w=[("xt", s2)], sem=("xt", s2))
                    ln_stats(xt[s2], stats[s2], mv[s2], rstd[s2], None, ("ln1", s2), [("xt", s2)])
                    ts("dve", xn[s2], xt[s2], mv[s2][:, 0:1], rstd[s2], ALU.subtract, ALU.mult,
                       r=[("xt", s2), (("ln1", s2), "mv"), (("ln1", s2), "rstd")], w=["xn"])
                    for kc in range(8):
                        tr(bank(kc // 4)[:, (kc % 4) * 128:(kc % 4 + 1) * 128], xn[s2][:, kc * 128:(kc + 1) * 128], identF,
                           r=["xn", "identF"], w=[bk(kc // 4)])
                    for kc in range(8):
                        act(uT[:, kc, t4 * 128:(t4 + 1) * 128], bank(kc // 4)[:, (kc % 4) * 128:(kc % 4 + 1) * 128], AF.Identity,
                            r=[bk(kc // 4), "modT"], w=[("uT", t4)], bias=modT[:, 0, kc:kc + 1], scale=modT[:, 1, kc:kc + 1])
                uTk = [("uT", i) for i in range(4)]
                for h in range(4):
                    b_ = 2 + h % 2
                    for kc in range(8):
                        mm(bank(b_), win[:, kc, h * 128:(h + 1) * 128], uT[:, kc, :], kc == 0, kc == 7,
                           r=uTk + [("win", kc)], w=[bk(b_)])
                    act(qT[:, h, :], bank(b_), AF.Silu, r=[bk(b_), "b_inT"], w=[("qT", h)], bias=b_inT[:, h:h + 1])
                for ch in range(4):
                    b_ca, b_cg = 2, 3
                    for kc in range(8):
                        mm(bank(b_ca), win[:, kc, 2048 + ch * 128:2048 + (ch + 1) * 128], uT[:, kc, :], kc == 0, kc == 7,
                           r=uTk + [("win", kc)], w=[bk(b_ca)])
                    for kc in range(8):
                        mm(bank(b_cg), win[:, kc, 2560 + ch * 128:2560 + (ch + 1) * 128], uT[:, kc, :], kc == 0, kc == 7,
                           r=uTk + [("win", kc)], w=[bk(b_cg)])
                    act(sg, bank(b_cg), AF.Sigmoid, r=[bk(b_cg), "b_inT"], w=["sg"], bias=b_inT[:, 8 + ch:9 + ch])
                    stt(abuf[:, ch, 30:542], bank(b_ca), b_inT[:, 4 + ch:5 + ch], sg, ALU.add, ALU.mult,
                        r=[bk(b_ca), "sg", "b_inT"], w=[("abuf", ch)])
                    for j in range(31):
                        mm(bank(4), dg[:, ch * 31 + j, :], abuf[:, ch, j:j + 512], j == 0, j == 30,
                           r=["dg", ("abuf", ch)], w=[bk(4)])
                    act(ac[:, ch, :], bank(4), AF.Identity, r=[bk(4), "cbT"], w=[("ac", ch)], bias=cbT[:, ch:ch + 1])
                    act(ac2, bank(4), AF.Square, r=[bk(4), "cbT"], w=["t1"], bias=cbT[:, ch:ch + 1])
                    mm(bank(5), onesM, ac[:, ch, :], ch == 0, ch == 3, r=["onesM", ("ac", ch)], w=[bk(5)])
                    mm(bank(6), onesM, ac2, ch == 0, ch == 3, r=["onesM", "t1"], w=[bk(6)])
                    cp("pool", abuf[:, ch, 0:30], abuf[:, ch, 512:542], r=[("abuf", ch)], w=[("abuf", ch)])
                cp("act", meanS, bank(5), r=[bk(5)], w=["meanS"])
                tt("dve", varS, meanS, meanS, ALU.mult, r=["meanS"], w=["varS"])
                tt("dve", varS, bank(6), varS, ALU.subtract, r=[bk(6), "varS"], w=["varS"])
                act(rstdB, varS, AF.Sqrt, r=["varS"], w=["rstdB"], bias=EPS)
                P.add("dve", lambda: nc.vector.reciprocal(out=rstdB, in_=rstdB), r=["rstdB"], w=["rstdB"])
                for ch in range(4):
                    tt("dve", t1, ac[:, ch, :], meanS, ALU.subtract, r=[("ac", ch), "meanS"], w=["t1"])
                    tt("dve", t1, t1, rstdB, ALU.mult, r=["t1", "rstdB"], w=["t1"])
                    act(hT[:, 4 + ch, :], t1, AF.Silu, r=["t1", "cgT", "cbbT"], w=[("hT", 4 + ch)],
                        bias=cbbT[:, ch:ch + 1], scale=cgT[:, ch:ch + 1])
                for t4 in range(4):
                    tsl = slice(t4 * 128, (t4 + 1) * 128)
                    for pi, (pb, col0) in enumerate(((2, 512), (3, 1024), (4, 1536))):
                        for kc in range(8):
                            mm(bank(pb), uT[:, kc, tsl], win[:, kc, col0:col0 + 512], kc == 0, False,
                               r=[("uT", t4), ("win", kc)], w=[bk(pb)])
                        mm(bank(pb), ones_row, brow[:, pi * 512:(pi + 1) * 512], False, True, r=["ones_row", "brow"], w=[bk(pb)])
                    act(zs, bank(2), AF.Sigmoid, r=[bk(2)], w=["zs"])
                    cp("dve", vt, bank(3), r=[bk(3)], w=["vt"])
                    act(sog, bank(4), AF.Silu, r=[bk(4)], w=["sog"])
                    tt("pool", gate, sog, nwB, ALU.mult, r=["sog", "nwB"], w=["gate"])
                    tt("dve", tmp, zs, omlbB, ALU.mult, r=["zs", "omlbB"], w=["tmp"])
                    tt("dve", kk, omlbB, tmp, ALU.subtract, r=["omlbB", "tmp"], w=["kk"])
                    tt("dve", tmp, tmp, lbB, ALU.add, r=["tmp", "lbB"], w=["tmp"])
                    act(gt, tmp, AF.Ln, r=["tmp"], w=["gt"])
                    mm(bank(5), M1, gt, True, True, r=["M1", "gt"], w=[bk(5)])
                    for h in range(4):
                        mm(bank(6)[:, h * 128:(h + 1) * 128], gt[:, h * 128:(h + 1) * 128], Lincl, True, True,
                           r=["gt", "Lincl"], w=[bk(6)])
                    for h in range(4):
                        mm(bank(7)[:, h * 128:(h + 1) * 128], gt[:, h * 128:(h + 1) * 128], M1, True, True,
                           r=["gt", "M1"], w=[bk(7)])
                    act(ec, bank(5), AF.Exp, r=[bk(5)], w=["ec"])
                    tt("dve", khat, kk, ec, ALU.mult, r=["kk", "ec"], w=["khat"])
                    act(eb.rearrange("p a b -> p (a b)"), bank(6), AF.Exp, r=[bk(6)], w=["eb"])
                    ts("dve", enc.rearrange("p a b -> p (a b)"), bank(7), -1.0, 75.0, ALU.mult, ALU.min, r=[bk(7)], w=["enc"])
                    act(enc, enc, AF.Exp, r=["enc"], w=["enc"])
                    qk = [("qT", h) for h in range(4)]
                    tt("dve", qp, qT[:, :, tsl], enc, ALU.mult, r=qk + ["enc"], w=["qp"])
                    tt("dve", qtA[:, :, 0:64], qT[:, :, t4 * 128:t4 * 128 + 64], eb[:, :, 0:64], ALU.mult, r=qk + ["eb"], w=["qtA"])
                    tt("dve", qtB[:, :, 64:128], qT[:, :, t4 * 128 + 64:t4 * 128 + 128], eb[:, :, 64:128], ALU.mult, r=qk + ["eb"], w=["qtB"])
                    for h in range(4):
                        tr(bankb(5)[:, h * 128:(h + 1) * 128], khat[:, h * 128:(h + 1) * 128], identB, r=["khat", "identB"], w=[bk(5)])
                    cp("act", khT.rearrange("p a b -> p (a b)"), bankb(5)[:, 0:512], r=[bk(5)], w=["khT"])
                    for h in range(4):
                        mm(bank(6)[:, h * 128:(h + 1) * 128], khT[:, h, :], qp[:, h, :], True, True, r=["khT", "qp"], w=[bk(6)])
                    tt("dve", AT, bank(6).rearrange("p (a b) -> p a b", a=4), maskB.unsqueeze(1).to_broadcast([128, 4, 128]), ALU.mult,
                       r=[bk(6), "maskB"], w=["AT"])
                    for h in range(4):
                        mm(bank(7)[:, h * 128:(h + 1) * 128], khat[0:64, h * 128:(h + 1) * 128], vt[0:64, h * 128:(h + 1) * 128],
                           True, True, r=["khat", "vt"], w=[bk(7)])
                    for h in range(4):
                        stt(S[:, h, :], S[:, h, :], eb[:, h, 63:64], bank(7)[:, h * 128:(h + 1) * 128], ALU.mult, ALU.add,
                            r=["S", "eb", bk(7)], w=["S"])
                    cp("pool", Sb1, S, r=["S"], w=["Sb1"])
                    for h in range(4):
                        hs = slice(h * 128, (h + 1) * 128)
                        mm(bank(5)[:, hs], AT[:, h, :], vt[:, hs], True, False, r=["AT", "vt"], w=[bk(5)])
                        mm(bank(5)[:, hs], qtA[:, h, :], Sb0[:, h, :], False, False, r=["qtA", "Sb0"], w=[bk(5)])
                        mm(bank(5)[:, hs], qtB[:, h, :], Sb1[:, h, :], False, True, r=["qtB", "Sb1"], w=[bk(5)])
                    for h in range(4):
                        mm(bank(7)[:, h * 128:(h + 1) * 128], khat[64:128, h * 128:(h + 1) * 128], vt[64:128, h * 128:(h + 1) * 128],
                           True, True, r=["khat", "vt"], w=[bk(7)])
                    for h in range(4):
                        stt(S[:, h, :], S[:, h, :], eb[:, h, 127:128], bank(7)[:, h * 128:(h + 1) * 128], ALU.mult, ALU.add,
                            r=["S", "eb", bk(7)], w=["S"])
                    cp("pool", Sb0, S, r=["S"], w=["Sb0"])
                    act(sq, bank(5), AF.Square, r=[bk(5)], w=["ec"])
                    P.add("dve", lambda: nc.vector.tensor_reduce(out=ss, in_=sq.rearrange("p (a b) -> p a b", a=4), axis=AX.X, op=ALU.add),
                          r=["ec"], w=["ss"])
                    act(rs, ss, AF.Sqrt, r=["ss"], w=["rs"], bias=EPS, scale=1.0 / 128.0)
                    P.add("dve", lambda: nc.vector.reciprocal(out=rs, in_=rs), r=["rs"], w=["rs"])
                    for h in range(4):
                        hs = slice(h * 128, (h + 1) * 128)
                        stt(ha[:, hs], bank(5)[:, hs], rs[:, h:h + 1], gate[:, hs], ALU.mult, ALU.mult,
                            r=[bk(5), "rs", "gate"], w=["ha"])
                    for h in range(4):
                        tr(bankb(6)[:, h * 128:(h + 1) * 128], ha[:, h * 128:(h + 1) * 128], identB, r=["ha", "identB"], w=[bk(6)])
                    for h in range(4):
                        cp("act", hT[:, h, tsl], bankb(6)[:, h * 128:(h + 1) * 128], r=[bk(6)], w=[("hT", h)])
                dma("sp", hTd[blk], hT, r=[("hT", i) for i in range(8)], w=[("hTd", blk)], sem="hTd")
            P.barrier()
            if stage == 1:
                final_keys.append(("hTd", NB - 1))
                break

            sb.reset(layer_mark)
            wout = sb([8, D], BF16)
            brow_o = sb([D], BF16, parts=1)
            ln1gB = sb([D], F32)
            ln1bB = sb([D], F32)
            rw = sb([8, NE], F32)
            hTb = [sb([8, 512], BF16) for _ in range(2)]
            xtb = [sb([D], F32) for _ in range(2)]
            xa = sb([D], F32)
            zt = sb([D], F32)
            x1 = [sb([D], F32) for _ in range(2)]
            xn2 = sb([D], F32)
            u2T = sb([8, 128], F32)
            u2Tb = [sb([8, 512], BF16) for _ in range(2)]
            statsb = sb([2, 6], F32)
            mvb = sb([2], F32)
            rstdb = sb([1], F32)
            nmrb = sb([1], F32)
            stats2 = sb([2, 6], F32)
            mv2 = sb([2], F32)
            rstd2 = sb([1], F32)
            nmr2 = sb([1], F32)
            for kc in range(8):
                dma("pool", wout[:, kc, :], w_out[l, kc * 128:(kc + 1) * 128, :], r=[], w=["wout"], sem=("wout", kc))
            dma("pool", brow_o, b_out[l:l + 1, :], r=[], w=["brow_o"], sem="brow_o")
            dma("sp", ln1gB, ln1_g[l].partition_broadcast(128), r=[], w=["ln1gB"], sem="ln1gB")
            dma("sp", ln1bB, ln1_b[l].partition_broadcast(128), r=[], w=["ln1bB"], sem="ln1bB")
            dma("sp", rw, router_w.rearrange("(k p) e -> p k e", p=128), r=[], w=["rw"], sem="rw", slow=True)
            for blk in range(NB):
                hb = blk % 2
                dma("sp", hTb[hb], hTd[blk], r=[("hTd", blk)], w=[("hTb", hb)], sem=("hTb", hb))
                for t4 in range(4):
                    t = blk * 4 + t4
                    s2 = t % 2
                    tsl = slice(t4 * 128, (t4 + 1) * 128)
                    dma("sp", xtb[s2], xsrc[t * 128:(t + 1) * 128, :], r=[], w=[("xtb", s2)], sem=("xtb", s2))
                    for hf in range(2):
                        for cc in range(8):
                            mm(bank(hf), hTb[hb][:, cc, tsl], wout[:, cc, hf * 512:(hf + 1) * 512], cc == 0, False,
                               r=[("hTb", hb), "wout"], w=[bk(hf)])
                        mm(bank(hf), ones_row, brow_o[:, hf * 512:(hf + 1) * 512], False, True, r=["ones_row", "brow_o"], w=[bk(hf)])
                    ts("pool", xa, xtb[s2], ALPHA, None, ALU.mult, None, r=[("xtb", s2)], w=["xa"])
                    tt("dve", zt, ps[:, 0:2, :].rearrange("p a b -> p (a b)"), g1B, ALU.mult, r=[bk(0), bk(1), "g1B"], w=["zt"])
                    tt("dve", zt, zt, xa, ALU.add, r=["zt", "xa"], w=["zt"])
                    ln_stats(zt, statsb, mvb, rstdb, nmrb, "pl1", ["zt"])
                    act(x1[s2], zt, AF.Identity, r=["zt", ("pl1", "rstd"), ("pl1", "nmr")], w=[("x1", s2)], bias=nmrb, scale=rstdb)
                    tt("dve", x1[s2], x1[s2], ln1gB, ALU.mult, r=[("x1", s2), "ln1gB"], w=[("x1", s2)])
                    tt("pool", x1[s2], x1[s2], ln1bB, ALU.add, r=[("x1", s2), "ln1bB"], w=[("x1", s2)])
                    dma("sp", x1d[t * 128:(t + 1) * 128, :], x1[s2], r=[("x1", s2)], w=[("x1d", t)], sem=("x1st", s2))
                    ln_stats(x1[s2], stats2, mv2, rstd2, nmr2, "ln2", [("x1", s2)])
                    act(xn2, x1[s2], AF.Identity, r=[("x1", s2), ("ln2", "rstd"), ("ln2", "nmr")], w=["xn2"], bias=nmr2, scale=rstd2)
                    for kc in range(8):
                        tr(bank(2 + kc // 4)[:, (kc % 4) * 128:(kc % 4 + 1) * 128], xn2[:, kc * 128:(kc + 1) * 128], identF,
                           r=["xn2", "identF"], w=[bk(2 + kc // 4)])
                    for kc in range(8):
                        act(u2T[:, kc, :], bank(2 + kc // 4)[:, (kc % 4) * 128:(kc % 4 + 1) * 128], AF.Identity,
                            r=[bk(2 + kc // 4), "modT"], w=["u2T"], bias=modT[:, 2, kc:kc + 1], scale=modT[:, 3, kc:kc + 1])
                    for kc in range(8):
                        mm(bank(4)[:, 0:NE], u2T[:, kc, :], rw[:, kc, :], kc == 0, kc == 7, r=["u2T", "rw"], w=[bk(4)])
                    cp("dve", logits[:, t * NE:(t + 1) * NE], bank(4)[:, 0:NE], r=[bk(4)], w=["logits"])
                    cp("pool", u2Tb[hb][:, :, tsl], u2T, r=["u2T"], w=[("u2Tb", hb)])
                dma("sp", u2Td[blk], u2Tb[hb], r=[("u2Tb", hb)], w=[("u2Td", blk)], sem=("u2Tst", hb))
            if dbg:
                dma("sp", logd, logits, r=["logits"], w=["logd"], sem="logd")
                final_keys.append("logd")
            P.barrier()
            if stage == 2:
                final_keys += [("x1d", NT - 1), ("u2Td", NB - 1)]
                break

            sb.reset(layer_mark)
            NG = NT * 4
            s_t = sb([NT * NE], F32)
            sbv = sb([NT * NE], F32)
            m1 = sb([NG], F32)
            is1 = sb([NT * NE], F32)
            G2 = sb([NT * NE], F32)
            m2 = sb([NG], F32)
            gs = sb([NG], F32)
            gmax = sb([NT], F32)
            gsel = sb([NG], F32)
            top2 = sb([NT * NE], F32)
            den = sb([NT], F32)
            g3 = lambda a: a.rearrange("p (g e) -> p g e", e=4)
            t3 = lambda a: a.rearrange("p (t e) -> p t e", t=NT)
            act(s_t, logits, AF.Sigmoid, r=["logits"], w=["s_t"])
            tt("dve", t3(sbv), t3(s_t), biasB.unsqueeze(1).to_broadcast([128, NT, NE]), ALU.add, r=["s_t", "biasB"], w=["sbv"])
            P.add("dve", lambda: nc.vector.tensor_reduce(out=m1, in_=g3(sbv), axis=AX.X, op=ALU.max), r=["sbv"], w=["m1"])
            tt("dve", g3(is1), g3(sbv), m1.unsqueeze(2).to_broadcast([128, NG, 4]), ALU.is_ge, r=["sbv", "m1"], w=["is1"])
            stt(G2, is1, -1.0e9, sbv, ALU.mult, ALU.add, r=["is1", "sbv"], w=["G2"])
            P.add("dve", lambda: nc.vector.tensor_reduce(out=m2, in_=g3(G2), axis=AX.X, op=ALU.max), r=["G2"], w=["m2"])
            tt("dve", gs, m1, m2, ALU.add, r=["m1", "m2"], w=["gs"])
            P.add("dve", lambda: nc.vector.tensor_reduce(out=gmax, in_=gs.rearrange("p (t g) -> p t g", g=4), axis=AX.X, op=ALU.max),
                  r=["gs"], w=["gmax"])
            tt("dve", gsel.rearrange("p (t g) -> p t g", g=4), gs.rearrange("p (t g) -> p t g", g=4),
               gmax.unsqueeze(2).to_broadcast([128, NT, 4]), ALU.is_ge, r=["gs", "gmax"], w=["gsel"])
            tt("dve", g3(top2), g3(sbv), m2.unsqueeze(2).to_broadcast([128, NG, 4]), ALU.is_ge, r=["sbv", "m2"], w=["top2"])
            tt("dve", g3(top2), g3(top2), gsel.unsqueeze(2).to_broadcast([128, NG, 4]), ALU.mult, r=["top2", "gsel"], w=["top2"])
            tt("dve", top2, top2, s_t, ALU.mult, r=["top2", "s_t"], w=["top2"])
            P.add("dve", lambda: nc.vector.tensor_reduce(out=den, in_=t3(top2), axis=AX.X, op=ALU.add), r=["top2"], w=["den"])
            P.add("dve", lambda: nc.vector.reciprocal(out=den, in_=den), r=["den"], w=["den"])
            tt("dve", t3(comb), t3(top2), den.unsqueeze(2).to_broadcast([128, NT, NE]), ALU.mult, r=["top2", "den"], w=["comb"])
            if dbg:
                dma("sp", combd, comb, r=["comb"], w=["combd"], sem="combd")
                final_keys.append("combd")
            P.barrier()
            if stage == 3:
                break

            sb.reset(layer_mark)
            NSB = 2
            TPS = NT // NSB
            wg = [sb([8, 512], BF16) for _ in range(2)]
            wu = [sb([8, 512], BF16) for _ in range(2)]
            wd = [sb([4, D], BF16) for _ in range(2)]
            u2s = sb([4, 8, 512], BF16)
            yacc = sb([TPS, D], F32)
            hTm = [sb([4, 512], BF16) for _ in range(2)]
            sgt = [sb([512], F32) for _ in range(2)]
            ln2gB = sb([D], F32)
            ln2bB = sb([D], F32)
            xe = [sb([D], F32) for _ in range(2)]
            ze = sb([D], F32)
            xo = [sb([D], F32) for _ in range(2)]
            statse = sb([2, 6], F32)
            mve = sb([2], F32)
            rstde = sb([1], F32)
            nmre = sb([1], F32)
            dma("sp", ln2gB, ln2_g[l].partition_broadcast(128), r=[], w=["ln2gB"], sem="ln2gB")
            dma("sp", ln2bB, ln2_b[l].partition_broadcast(128), r=[], w=["ln2bB"], sem="ln2bB")
            wcnt = 0
            hcnt = 0
            for sbk in range(NSB):
                for b4 in range(4):
                    dma("sp", u2s[:, b4, :, :], u2Td[sbk * 4 + b4], r=[("u2Td", sbk * 4 + b4)], w=[("u2s", b4)], sem=("u2s", b4))
                for e in range(NE):
                    ws = wcnt % 2
                    wcnt += 1
                    dma("pool", wg[ws], w_gate[l, e].rearrange("(k p) f -> p k f", p=128), r=[], w=[("wg", ws)], sem=("wg", ws))
                    dma("pool", wu[ws], w_up[l, e].rearrange("(k p) f -> p k f", p=128), r=[], w=[("wu", ws)], sem=("wu", ws))
                    dma("pool", wd[ws], w_down[l, e].rearrange("(k p) n -> p k n", p=128), r=[], w=[("wd", ws)], sem=("wd", ws))
                    for b4 in range(4):
                        hs_ = hcnt % 2
                        hcnt += 1
                        for fc in range(4):
                            bg = fc % 2
                            bu = 2 + fc % 2
                            for kc in range(8):
                                mm(bank(bg), wg[ws][:, kc, fc * 128:(fc + 1) * 128], u2s[:, b4, kc, :], kc == 0, kc == 7,
                                   r=[("wg", ws), ("u2s", b4)], w=[bk(bg)])
                            for kc in range(8):
                                mm(bank(bu), wu[ws][:, kc, fc * 128:(fc + 1) * 128], u2s[:, b4, kc, :], kc == 0, kc == 7,
                                   r=[("wu", ws), ("u2s", b4)], w=[bk(bu)])
                            act(sgt[fc % 2], bank(bg), AF.Silu, r=[bk(bg)], w=[("sgt", fc % 2)])
                            tt("dve", hTm[hs_][:, fc, :], sgt[fc % 2], bank(bu), ALU.mult, r=[("sgt", fc % 2), bk(bu)], w=[("hTm", hs_, fc)])
                        for t4 in range(4):
                            tl = b4 * 4 + t4
                            tg = sbk * TPS + tl
                            yb = 4 + 2 * (t4 % 2)
                            for hf in range(2):
                                for fc in range(4):
                                    mm(bank(yb + hf), hTm[hs_][:, fc, t4 * 128:(t4 + 1) * 128], wd[ws][:, fc, hf * 512:(hf + 1) * 512],
                                       fc == 0, fc == 3, r=[("hTm", hs_, fc), ("wd", ws)], w=[bk(yb + hf)])
                            ysrc = ps[:, yb:yb + 2, :].rearrange("p a b -> p (a b)")
                            cw = comb[:, tg * NE + e:tg * NE + e + 1]
                            if e == 0:
                                ts("dve", yacc[:, tl, :], ysrc, cw, None, ALU.mult, None, r=[bk(yb), bk(yb + 1), "comb"], w=[("yacc", tl)])
                            else:
                                stt(yacc[:, tl, :], ysrc, cw, yacc[:, tl, :], ALU.mult, ALU.add,
                                    r=[bk(yb), bk(yb + 1), "comb", ("yacc", tl)], w=[("yacc", tl)])
                for tl in range(TPS):
                    tg = sbk * TPS + tl
                    s2 = tg % 2
                    dma("sp", xe[s2], x1d[tg * 128:(tg + 1) * 128, :], r=[("x1d", tg)], w=[("xe", s2)], sem=("xe", s2))
                    tt("dve", ze, yacc[:, tl, :], g2B, ALU.mult, r=[("yacc", tl), "g2B"], w=["ze"])
                    stt(ze, xe[s2], ALPHA, ze, ALU.mult, ALU.add, r=[("xe", s2), "ze"], w=["ze"])
                    ln_stats(ze, statse, mve, rstde, nmre, "pl2", ["ze"])
                    act(xo[s2], ze, AF.Identity, r=["ze", ("pl2", "rstd"), ("pl2", "nmr")], w=[("xo", s2)], bias=nmre, scale=rstde)
                    tt("dve", xo[s2], xo[s2], ln2gB, ALU.mult, r=[("xo", s2), "ln2gB"], w=[("xo", s2)])
                    tt("pool", xo[s2], xo[s2], ln2bB, ALU.add, r=[("xo", s2), "ln2bB"], w=[("xo", s2)])
                    dma("sp", xdst[tg * 128:(tg + 1) * 128, :], xo[s2], r=[("xo", s2)], w=[("xdst", l, tg)], sem=("xost", s2))
                    if l == nlayers - 1 or stage == 4:
                        final_keys.append(("xdst", l, tg))
            P.barrier()
            if stage == 4:
                break
        P.emit(top, final_keys=final_keys)
        build.stats = dict(P.stats, sb_peak=sb.peak)
    return nc


_IN_NAMES = ["ada_w", "ada_b", "w_in", "b_in", "hgrn_lb", "hgrn_norm_w", "conv_w", "conv_b", "conv_ln_g", "conv_ln_b",
             "w_out", "b_out", "ln1_g", "ln1_b", "router_w", "router_bias", "w_gate", "w_up", "w_down", "ln2_g", "ln2_b"]


def make_in_maps(inputs, ncores=NCORES):
    shared = {k: np.ascontiguousarray(np.asarray(inputs[k], dtype=np.float32)) for k in _IN_NAMES}
    x = np.asarray(inputs["x"], dtype=np.float32)
    c = np.asarray(inputs["c"], dtype=np.float32)
    maps = []
    for i in range(ncores):
        m = dict(shared)
        m["x"] = np.ascontiguousarray(x[i])
        m["c"] = np.ascontiguousarray(c[i])
        maps.append(m)
    return maps


def kernel(**inputs):
    nc = build()
    in_maps = make_in_maps(inputs)
    res = run_bass_kernel_spmd(nc, in_maps, core_ids=list(range(NCORES)))
    return np.stack([np.asarray(r["out"], dtype=np.float32) for r in res.results], axis=0)
```

```python
import numpy as np
from contextlib import ExitStack
import concourse.bass as bass
import concourse.mybir as mybir
from concourse.bass_utils import run_bass_kernel_spmd

F32 = mybir.dt.float32
BF16 = mybir.dt.bfloat16
U8 = mybir.dt.uint8
AF = mybir.ActivationFunctionType
ALU = mybir.AluOpType
AX = mybir.AxisListType

T = 4096
D = 1024
NT = 32
NB = 8
NE = 16
ALPHA = 4.0 ** 0.25
EPS = 1e-5
NCORES = 8
SAME_ENG_SYNC = True


class Prog:
    DMA_BW = 300e3

    def __init__(self, nc):
        self.nc = nc
        self.ops = []
        self.eng = {"pe": nc.tensor, "act": nc.scalar, "dve": nc.vector,
                    "pool": nc.gpsimd, "sp": nc.sync}

    def add(self, eng, fn, r=(), w=(), cost=0.2):
        self.ops.append(dict(eng=eng, fn=fn, r=tuple(r), w=tuple(w), dma=None, bar=False, cost=cost, nbytes=0))

    def dma(self, q, fn, r=(), w=(), sem=None, nbytes=0):
        assert sem is not None
        self.ops.append(dict(eng=q, fn=fn, r=tuple(r), w=tuple(w), dma=sem, bar=False,
                             cost=(1.0 if q == "pool" else 0.1), nbytes=nbytes))

    def barrier(self):
        self.ops.append(dict(eng=None, fn=None, r=(), w=(), dma=None, bar=True, cost=0.0, nbytes=0))

    def _raw_deps(self):
        ops = self.ops
        n = len(ops)
        last_writer = {}
        readers = {}
        deps = [None] * n
        for i, op in enumerate(ops):
            if op["bar"]:
                deps[i] = set()
                continue
            d = set()
            for k in op["r"]:
                if k in last_writer:
                    d.add(last_writer[k])
            for k in op["w"]:
                if k in last_writer:
                    d.add(last_writer[k])
                d.update(readers.get(k, ()))
            d.discard(i)
            deps[i] = d
            for k in op["r"]:
                readers.setdefault(k, []).append(i)
            for k in op["w"]:
                last_writer[k] = i
                readers[k] = []
        return deps, last_writer

    def _schedule_segment(self, idxs, deps):
        ops = self.ops
        inseg = set(idxs)
        nrem = {}
        users = {}
        for i in idxs:
            dl = [j for j in deps[i] if j in inseg]
            nrem[i] = len(dl)
            for j in dl:
                users.setdefault(j, []).append(i)
        ready = {e: [] for e in self.eng}
        ready_time = {}
        finish = {}
        import heapq
        for i in idxs:
            if nrem[i] == 0:
                ready_time[i] = 0.0
                heapq.heappush(ready[ops[i]["eng"]], (0.0, i))
        etime = {e: 0.0 for e in self.eng}
        dma_free = 0.0
        order = []
        remaining = len(idxs)
        while remaining:
            best = None
            for e in self.eng:
                if not ready[e]:
                    continue
                rt, i = ready[e][0]
                st = max(etime[e], rt)
                cand = (st, i, e)
                cands = [(max(etime[e], r_), i_) for (r_, i_) in ready[e] if r_ <= st]
                if cands:
                    i2 = min(c[1] for c in cands)
                    cand = (st, i2, e)
                if best is None or cand < best:
                    best = cand
            st, i, e = best
            lst = ready[e]
            for k_, (r_, i_) in enumerate(lst):
                if i_ == i:
                    lst[k_] = lst[-1]
                    lst.pop()
                    heapq.heapify(lst)
                    break
            op = ops[i]
            if op["dma"] is not None:
                etime[e] = st + op["cost"]
                t0 = max(st + op["cost"], dma_free)
                dma_free = t0 + op["nbytes"] / self.DMA_BW
                fin = dma_free + 2.0
            else:
                fin = st + op["cost"]
                etime[e] = fin
            finish[i] = fin
            order.append(i)
            remaining -= 1
            for u in users.get(i, ()):
                nrem[u] -= 1
                rt_u = max(ready_time.get(u, 0.0), fin + 0.1)
                ready_time[u] = rt_u
                if nrem[u] == 0:
                    heapq.heappush(ready[ops[u]["eng"]], (rt_u, u))
        mk = max(finish.values()) if finish else 0.0
        return order, mk

    def emit(self, stack, final_keys=(), schedule=True):
        nc = self.nc
        ops = self.ops
        n = len(ops)
        deps, last_writer = self._raw_deps()
        order = []
        seg = []
        seg_times = []
        for i, op in enumerate(ops):
            if op["bar"]:
                if seg:
                    if schedule:
                        o, mk = self._schedule_segment(seg, deps)
                    else:
                        o, mk = list(seg), 0.0
                    order += o
                    seg_times.append(mk)
                order.append(i)
                seg = []
            else:
                seg.append(i)
        if seg:
            if schedule:
                o, mk = self._schedule_segment(seg, deps)
            else:
                o, mk = list(seg), 0.0
            order += o
            seg_times.append(mk)
        pos = {i: p for p, i in enumerate(order)}
        last_on_eng = {}
        dmas_since = []
        pend = {e: set() for e in self.eng}
        fdeps = [None] * n
        for i in order:
            op = ops[i]
            if op["bar"]:
                d = set(last_on_eng.values()) | set(dmas_since)
                for e in self.eng:
                    pend[e] |= d
                dmas_since = []
                fdeps[i] = set()
                continue
            d = set(deps[i])
            e = op["eng"]
            if pend[e]:
                d |= pend[e]
                pend[e] = set()
            best = {}
            dd = set()
            for j in d:
                oj = ops[j]
                if oj["dma"] is not None:
                    dd.add(j)
                elif oj["eng"] not in best or pos[best[oj["eng"]]] < pos[j]:
                    best[oj["eng"]] = j
            dd.update(best.values())
            fdeps[i] = dd
            last_on_eng[e] = i
            if op["dma"] is not None:
                dmas_since.append(i)
        need_sig = [False] * n
        for i in order:
            op = ops[i]
            if op["bar"]:
                continue
            for j in fdeps[i]:
                oj = ops[j]
                if oj["dma"] is not None:
                    continue
                if oj["eng"] == op["eng"] and op["dma"] is None and (oj["eng"] == "pe" or not SAME_ENG_SYNC):
                    continue
                need_sig[j] = True
        final_deps = set(last_writer[k] for k in final_keys)
        esem = {e: stack.enter_context(nc.semaphore("s_" + e)) for e in self.eng}
        dsem = {}
        dcount = {}
        ecount = {e: 0 for e in self.eng}
        waited = {e: {} for e in self.eng}
        sig = [None] * n
        nwaits = 0
        for i in order:
            op = ops[i]
            if op["bar"]:
                continue
            e = op["eng"]
            eng = self.eng[e]
            for j in sorted(fdeps[i], key=lambda j: pos[j]):
                oj = ops[j]
                if oj["dma"] is None and oj["eng"] == e and op["dma"] is None and (e == "pe" or not SAME_ENG_SYNC):
                    continue
                assert pos[j] < pos[i], (i, j)
                sname, val = sig[j]
                if waited[e].get(sname, 0) >= val:
                    continue
                semh = esem[sname] if sname in esem else dsem[sname]
                eng.wait_ge(semh, val)
                nwaits += 1
                waited[e][sname] = val
            inst = op["fn"]()
            if op["dma"] is not None:
                key = op["dma"]
                if key not in dsem:
                    dsem[key] = stack.enter_context(nc.semaphore("d_%d" % len(dsem)))
                    dcount[key] = 0
                dcount[key] += 16
                inst.then_inc(dsem[key], 16)
                sig[i] = (key, dcount[key])
            elif need_sig[i]:
                ecount[e] += 1
                inst.then_inc(esem[e], 1)
                sig[i] = (e, ecount[e])
        eng = self.eng["sp"]
        for j in sorted(final_deps):
            sname, val = sig[j]
            semh = esem[sname] if sname in esem else dsem[sname]
            eng.wait_ge(semh, val)
        self.stats = dict(nops=n, nwaits=nwaits, ecount=ecount, ndsem=len(dsem),
                          seg_us=[round(t) for t in seg_times])


class SBAlloc:
    def __init__(self, big, nbytes):
        self.big = big
        self.cap = nbytes
        self.off = 0
        self.peak = 0

    def mark(self):
        return self.off

    def reset(self, m):
        self.off = m

    def __call__(self, free_shape, dt, parts=128):
        esz = {F32: 4, BF16: 2, U8: 1, mybir.dt.int32: 4, mybir.dt.uint32: 4}[dt]
        nel = int(np.prod(free_shape))
        nbytes = (nel * esz + 63) // 64 * 64
        assert self.off + nbytes <= self.cap, ("SBUF overflow", self.off, nbytes, self.cap)
        ap = self.big[0:parts, self.off:self.off + nel * esz].bitcast(dt)
        self.off += nbytes
        self.peak = max(self.peak, self.off)
        if len(free_shape) == 2:
            ap = ap.rearrange("p (a b) -> p a b", a=free_shape[0], b=free_shape[1])
        elif len(free_shape) == 3:
            ap = ap.rearrange("p (a b c) -> p a b c", a=free_shape[0], b=free_shape[1], c=free_shape[2])
        return ap


def build(stage=99, dbg=False):
    nc = bass.Bass("TRN2", target_bir_lowering=False)
    dk = "ExternalOutput" if dbg else "Internal"

    def din(name, shape):
        return nc.dram_tensor(name, list(shape), F32, kind="ExternalInput").ap()

    x_d = din("x", [T, D])
    c_d = din("c", [D])
    ada_w = din("ada_w", [2, D, 6 * D])
    ada_b = din("ada_b", [2, 6 * D])
    w_in = din("w_in", [2, D, 3072])
    b_in = din("b_in", [2, 3072])
    hgrn_lb = din("hgrn_lb", [2, 512])
    hgrn_nw = din("hgrn_norm_w", [2, 128])
    conv_w = din("conv_w", [2, 31, 512])
    conv_b = din("conv_b", [2, 512])
    conv_g = din("conv_ln_g", [2, 512])
    conv_bb = din("conv_ln_b", [2, 512])
    w_out = din("w_out", [2, D, D])
    b_out = din("b_out", [2, D])
    ln1_g = din("ln1_g", [2, D])
    ln1_b = din("ln1_b", [2, D])
    router_w = din("router_w", [D, NE])
    router_bias = din("router_bias", [NE])
    w_gate = din("w_gate", [2, NE, D, 512])
    w_up = din("w_up", [2, NE, D, 512])
    w_down = din("w_down", [2, NE, 512, D])
    ln2_g = din("ln2_g", [2, D])
    ln2_b = din("ln2_b", [2, D])
    out_d = nc.dram_tensor("out", [T, D], F32, kind="ExternalOutput").ap()
    hTd = nc.dram_tensor("hTd", [NB, 128, 8, 512], BF16, kind=dk).ap()
    uTd = nc.dram_tensor("uTd", [NB, 128, 8, 512], BF16, kind="Internal").ap()
    winb_d = nc.dram_tensor("winb_d", [D, 3072], BF16, kind="Internal").ap()
    woutb_d = nc.dram_tensor("woutb_d", [D, D], BF16, kind="Internal").ap()
    x1d = nc.dram_tensor("x1d", [T, D], F32, kind=dk).ap()
    u2Td = nc.dram_tensor("u2Td", [NB, 128, 8, 512], BF16, kind=dk).ap()
    xmid = nc.dram_tensor("xmid", [T, D], F32, kind=dk).ap()
    logd = nc.dram_tensor("logd", [128, NT * NE], F32, kind=dk).ap()
    combd = nc.dram_tensor("combd", [128, NT * NE], F32, kind=dk).ap()
    modTd = nc.dram_tensor("modTd", [128, 32], F32, kind=dk).ap()

    SB_BYTES = 207 * 1024
    with ExitStack() as top:
        big = top.enter_context(nc.sbuf_tensor("big", [128, SB_BYTES], U8))
        ps = top.enter_context(nc.psum_tensor("ps", [128, 8, 512], F32))
        sb = SBAlloc(big, SB_BYTES)
        P = Prog(nc)

        def bank(b):
            return ps[:, b, :]

        def bankb(b):
            return ps[:, b, :].bitcast(BF16)

        def bk(b):
            return ("ps", b)

        def fsz(ap):
            return int(np.prod(ap.shape[1:]))

        def mm(out, lhsT, rhs, start, stop, r, w):
            c = max(fsz(out), 64) * (4 if rhs.dtype == F32 else 1) / 2000.0 + 0.02
            P.add("pe", lambda: nc.tensor.matmul(out, lhsT=lhsT, rhs=rhs, start=start, stop=stop), r=r, w=w, cost=c)

        def tr(out, in_, ident, r, w):
            P.add("pe", lambda: nc.tensor.transpose(out=out, in_=in_, identity=ident), r=r, w=w, cost=0.1)

        def act(out, in_, func, r, w, bias=0.0, scale=1.0, accum=None):
            c = 0.2 + fsz(out) * 0.00085
            if accum is None:
                P.add("act", lambda: nc.scalar.activation(out=out, in_=in_, func=func, bias=bias, scale=scale), r=r, w=w, cost=c)
            else:
                P.add("act", lambda: nc.scalar.activation(out=out, in_=in_, func=func, bias=bias, scale=scale, accum_out=accum), r=r, w=w, cost=c)

        def tt(eng, out, in0, in1, op, r, w):
            e = nc.vector if eng == "dve" else nc.gpsimd
            c = (0.1 + fsz(out) * 0.00105) if eng == "dve" else (0.2 + fsz(out) * 0.0021)
            P.add(eng, lambda: e.tensor_tensor(out=out, in0=in0, in1=in1, op=op), r=r, w=w, cost=c)

        def ts(eng, out, in0, s1, s2, op0, op1, r, w):
            e = nc.vector if eng == "dve" else nc.gpsimd
            c = (0.1 + fsz(out) * 0.00105) if eng == "dve" else (0.2 + fsz(out) * 0.0021)
            if op1 is None:
                P.add(eng, lambda: e.tensor_scalar(out=out, in0=in0, scalar1=s1, scalar2=None, op0=op0), r=r, w=w, cost=c)
            else:
                P.add(eng, lambda: e.tensor_scalar(out=out, in0=in0, scalar1=s1, scalar2=s2, op0=op0, op1=op1), r=r, w=w, cost=c)

        def stt(out, in0, scalar, in1, op0, op1, r, w):
            P.add("dve", lambda: nc.vector.scalar_tensor_tensor(out=out, in0=in0, scalar=scalar, in1=in1, op0=op0, op1=op1), r=r, w=w,
                  cost=0.1 + fsz(out) * 0.00105)

        def cp(eng, out, in_, r, w):
            if eng == "act":
                P.add("act", lambda: nc.scalar.copy(out=out, in_=in_), r=r, w=w, cost=0.2 + fsz(out) * 0.00085)
            else:
                e = nc.vector if eng == "dve" else nc.gpsimd
                c = (0.1 + fsz(out) * 0.00105) if eng == "dve" else (0.2 + fsz(out) * 0.0021)
                P.add(eng, lambda: e.tensor_copy(out=out, in_=in_), r=r, w=w, cost=c)

        def memset(eng, ap, val, w):
            e = nc.vector if eng == "dve" else nc.gpsimd
            P.add(eng, lambda: e.memset(ap, val), w=w, cost=0.1 + fsz(ap) * 0.001)

        def dma(q, out, in_, r, w, sem, slow=False):
            e = {"sp": nc.sync, "pool": nc.gpsimd, "act": nc.scalar}[q]
            nb = int(np.prod(out.shape)) * (2 if out.dtype == BF16 else 4)
            if in_.dtype == F32 and out.dtype == BF16:
                nb *= 2
            if slow:
                def f():
                    with nc.allow_non_contiguous_dma(reason="small strided load"):
                        return e.dma_start(out=out, in_=in_)
                P.dma(q, f, r=r, w=w, sem=sem, nbytes=nb)
            else:
                P.dma(q, lambda: e.dma_start(out=out, in_=in_), r=r, w=w, sem=sem, nbytes=nb)

        def ln_stats(src, stats, mv, rstd, nmr, tag, rkeys):
            for h in range(2):
                P.add("dve", (lambda h=h: nc.vector.bn_stats(out=stats[:, h, :], in_=src[:, h * 512:(h + 1) * 512])),
                      r=rkeys, w=[(tag, "st", h)], cost=0.65)
            P.add("dve", lambda: nc.vector.bn_aggr(out=mv, in_=stats.rearrange("p a b -> p (a b)")),
                  r=[(tag, "st", 0), (tag, "st", 1)], w=[(tag, "mv")])
            act(rstd, mv[:, 1:2], AF.Ln, r=[(tag, "mv")], w=[(tag, "rstd")], bias=eps_t)
            act(rstd, rstd, AF.Exp, r=[(tag, "rstd")], w=[(tag, "rstd")], scale=-0.5)
            if nmr is not None:
                ts("dve", nmr, mv[:, 0:1], rstd, -1.0, ALU.mult, ALU.mult, r=[(tag, "mv"), (tag, "rstd")], w=[(tag, "nmr")])

        identF = sb([128], F32)
        identB = sb([128], BF16)
        Lincl = sb([128], F32)
        M1 = sb([128], F32)
        maskB = sb([128], BF16)
        onesM = sb([128], F32)
        ones_row = sb([128], BF16, parts=1)
        condT = sb([8], F32)
        cond_bc = sb([8, 128], F32)
        biasB = sb([NE], F32)
        eps_t = sb([1], F32)
        mhalf = sb([512], F32)
        one_t = sb([1], F32)

        memset("pool", identF, 1.0, w=["identF"])
        P.add("pool", lambda: nc.gpsimd.affine_select(out=identF, in_=identF, pattern=[[-1, 128]], compare_op=ALU.is_equal,
                                                      fill=0.0, base=0, channel_multiplier=1), r=["identF"], w=["identF"])
        cp("pool", identB, identF, r=["identF"], w=["identB"])
        memset("pool", Lincl, 1.0, w=["Lincl"])
        P.add("pool", lambda: nc.gpsimd.affine_select(out=Lincl, in_=Lincl, pattern=[[1, 128]], compare_op=ALU.is_ge,
                                                      fill=0.0, base=0, channel_multiplier=-1), r=["Lincl"], w=["Lincl"])
        memset("pool", Lincl[0:64, 64:128], 0.0, w=["Lincl"])
        cp("pool", maskB, Lincl, r=["Lincl"], w=["maskB"])
        memset("pool", M1, 1.0, w=["M1"])
        P.add("pool", lambda: nc.gpsimd.affine_select(out=M1, in_=M1, pattern=[[-1, 128]], compare_op=ALU.is_ge,
                                                      fill=0.0, base=-1, channel_multiplier=1), r=["M1"], w=["M1"])
        memset("pool", M1[64:128, 0:64], 0.0, w=["M1"])
        memset("pool", onesM, 1.0 / 512.0, w=["onesM"])
        memset("pool", ones_row, 1.0, w=["ones_row"])
        memset("pool", mhalf, -0.5, w=["mhalf"])
        memset("pool", eps_t, EPS, w=["eps_t"])
        memset("pool", one_t, 1.0, w=["one_t"])
        dma("sp", condT, c_d.rearrange("(k p) -> p k", p=128), r=[], w=["condT"], sem="condT", slow=True)
        act(condT, condT, AF.Silu, r=["condT"], w=["condT"])
        for k in range(8):
            ts("dve", cond_bc[:, k, :], onesM, condT[:, k:k + 1], 512.0, ALU.mult, ALU.mult, r=["onesM", "condT"], w=["cond_bc"])
        dma("sp", biasB, router_bias.partition_broadcast(128), r=[], w=["biasB"], sem="biasB")
        perm_mark = sb.mark()

        final_keys = []
        nlayers = 2
        for l in range(nlayers):
            sb.reset(perm_mark)
            xsrc = x_d if l == 0 else xmid
            xdst = xmid if l == 0 else out_d
            modT = sb([4, 8], F32)
            g1B = sb([D], F32)
            g2B = sb([D], F32)
            logits = sb([NT * NE], F32)
            comb = sb([NT * NE], F32)
            layer_mark = sb.mark()

            adaw = [sb([8, 512], F32) for _ in range(2)]
            adab = [sb([512], F32) for _ in range(2)]
            piece = [sb([512], F32) for _ in range(2)]
            kind_of = {0: 0, 1: 0, 2: 1, 3: 1, 6: 2, 7: 2, 8: 3, 9: 3}
            for j in range(12):
                s2 = j % 2
                dma("sp", adaw[s2], ada_w[l, :, j * 512:(j + 1) * 512].rearrange("(k p) n -> p k n", p=128),
                    r=[], w=[("adaw", s2)], sem=("adaw", s2))
                dma("sp", adab[s2], ada_b[l, j * 512:(j + 1) * 512].partition_broadcast(128), r=[], w=[("adab", s2)], sem=("adab", s2))
                for kc in range(8):
                    mm(bank(s2), cond_bc[:, kc, :], adaw[s2][:, kc, :], kc == 0, kc == 7,
                       r=["cond_bc", ("adaw", s2)], w=[bk(s2)])
                if j in (4, 5):
                    dst = g1B[:, (j - 4) * 512:(j - 3) * 512]
                    tt("dve", dst, bank(s2), adab[s2], ALU.add, r=[bk(s2), ("adab", s2)], w=["g1B"])
                elif j in (10, 11):
                    dst = g2B[:, (j - 10) * 512:(j - 9) * 512]
                    tt("dve", dst, bank(s2), adab[s2], ALU.add, r=[bk(s2), ("adab", s2)], w=["g2B"])
                else:
                    tt("dve", piece[s2], bank(s2), adab[s2], ALU.add, r=[bk(s2), ("adab", s2)], w=[("piece", s2)])
                    for b in range(4):
                        tr(bank(2 + s2)[:, b * 128:(b + 1) * 128], piece[s2][:, b * 128:(b + 1) * 128], identF,
                           r=[("piece", s2), "identF"], w=[bk(2 + s2)])
                    kd = kind_of[j]
                    half = j % 2
                    dstm = modT[:, kd, half * 4:half * 4 + 4]
                    src = bank(2 + s2).rearrange("p (b c) -> p b c", c=128)[:, :, 0]
                    if kd in (1, 3):
                        ts("dve", dstm, src, 1.0, None, ALU.add, None, r=[bk(2 + s2)], w=["modT"])
                    else:
                        cp("dve", dstm, src, r=[bk(2 + s2)], w=["modT"])
            wst = [sb([1536], F32) for _ in range(2)]
            wbf = [sb([1536], BF16) for _ in range(2)]
            pc = 0
            for kc in range(8):
                for (c0, c1) in ((0, 1536), (1536, 3072)):
                    q2 = pc % 2
                    pc += 1
                    dma("sp", wst[q2], w_in[l, kc * 128:(kc + 1) * 128, c0:c1], r=[], w=[("wst", q2)], sem=("wst", q2))
                    cp("dve" if q2 == 0 else "act", wbf[q2], wst[q2], r=[("wst", q2)], w=[("wbf", q2)])
                    dma("sp", winb_d[kc * 128:(kc + 1) * 128, c0:c1], wbf[q2], r=[("wbf", q2)], w=[("winb_d", kc)], sem=("wbfst", q2))
            for kc in range(8):
                q2 = pc % 2
                pc += 1
                dma("sp", wst[q2][:, 0:1024], w_out[l, kc * 128:(kc + 1) * 128, :], r=[], w=[("wst", q2)], sem=("wst", q2))
                cp("dve" if q2 == 0 else "act", wbf[q2][:, 0:1024], wst[q2][:, 0:1024], r=[("wst", q2)], w=[("wbf", q2)])
                dma("sp", woutb_d[kc * 128:(kc + 1) * 128, :], wbf[q2][:, 0:1024], r=[("wbf", q2)], w=[("woutb_d", kc)], sem=("wbfst", q2))
            if dbg and l == 0:
                dma("sp", modTd, modT.rearrange("p a b -> p (a b)"), r=["modT"], w=["modTd"], sem="modTd")
                final_keys.append("modTd")
            P.barrier()
            if stage == 0:
                break

            sb.reset(layer_mark)
            winc = sb([8, 1024], BF16)
            dg = sb([124, 128], BF16)
            b_inT = sb([12], F32)
            b_inH = sb([12], F32)
            cwT = sb([4, 31], F32)
            cbT = sb([4], F32)
            cgT = sb([4], F32)
            cbbT = sb([4], F32)
            abuf = sb([4, 30 + T], BF16)
            xt = [sb([D], F32) for _ in range(2)]
            xn = [sb([D], F32) for _ in range(2)]
            uT = [sb([8, 512], BF16) for _ in range(2)]
            sg = [sb([512], F32) for _ in range(2)]
            ac = [sb([4, 512], F32) for _ in range(2)]
            sq = [sb([512], F32) for _ in range(2)]
            t1 = [sb([512], F32) for _ in range(2)]
            meanS = [sb([512], F32) for _ in range(2)]
            varS = [sb([512], F32) for _ in range(2)]
            rstdB = [sb([512], F32) for _ in range(2)]
            hTc = [sb([4, 512], BF16) for _ in range(2)]
            stats = [sb([2, 6], F32) for _ in range(2)]
            mv = [sb([2], F32) for _ in range(2)]
            rstd = [sb([1], F32) for _ in range(2)]
            cw_tm = sg[0][0:31, :]
            yv = [sb([512], F32) for _ in range(2)]
            ncgT = sb([4], F32)
            ncbbT = sb([4], F32)
            print("A1 sbuf", sb.off)

            for kc in range(8):
                dma("sp", winc[:, kc, :], winb_d[kc * 128:(kc + 1) * 128, 2048:3072], r=[("winb_d", kc)], w=[("winc", kc)], sem=("winc", kc))
            dma("sp", b_inT[:, 0:4], b_in[l, 0:512].rearrange("(c p) -> p c", p=128), r=[], w=["b_inT"], sem="b_inT0", slow=True)
            dma("sp", b_inT[:, 4:12], b_in[l, 2048:3072].rearrange("(c p) -> p c", p=128), r=[], w=["b_inT"], sem="b_inT1", slow=True)
            ts("dve", b_inH, b_inT, -1.0, None, ALU.mult, None, r=["b_inT"], w=["b_inH"])
            dma("sp", cw_tm, conv_w[l], r=[], w=[("sg", 0)], sem="cw_tm")
            for ch in range(4):
                tr(bank(0)[:, ch * 32:ch * 32 + 31], cw_tm[0:31, ch * 128:(ch + 1) * 128], identF[0:31, 0:31],
                   r=[("sg", 0), "identF"], w=[bk(0)])
                cp("dve", cwT[:, ch, :], bank(0)[:, ch * 32:ch * 32 + 31], r=[bk(0)], w=["cwT"])
            for ch in range(4):
                for j in range(31):
                    ts("dve", dg[:, ch * 31 + j, :], identF, cwT[:, ch, j:j + 1], None, ALU.mult, None,
                       r=["identF", "cwT"], w=[("dg", ch)])
            dma("sp", cbT, conv_b[l].rearrange("(c p) -> p c", p=128), r=[], w=["cbT"], sem="cbT", slow=True)
            dma("sp", cgT, conv_g[l].rearrange("(c p) -> p c", p=128), r=[], w=["cgT"], sem="cgT", slow=True)
            dma("sp", cbbT, conv_bb[l].rearrange("(c p) -> p c", p=128), r=[], w=["cbbT"], sem="cbbT", slow=True)
            ts("dve", ncgT, cgT, -1.0, None, ALU.mult, None, r=["cgT"], w=["ncgT"])
            ts("dve", ncbbT, cbbT, -1.0, None, ALU.mult, None, r=["cbbT"], w=["ncbbT"])
            memset("pool", abuf[:, :, 0:30], 0.0, w=[("abuf", ch, -1) for ch in range(4)])

            for blk in range(NB):
                pb = blk % 2
                for t4 in range(4):
                    t = blk * 4 + t4
                    s2 = t % 2
                    dma("sp", xt[s2], xsrc[t * 128:(t + 1) * 128, :], r=[], w=[("xt", s2)], sem=("xt", s2))
                    ln_stats(xt[s2], stats[s2], mv[s2], rstd[s2], None, ("ln1", s2), [("xt", s2)])
                    ts("dve", xn[s2], xt[s2], mv[s2][:, 0:1], rstd[s2], ALU.subtract, ALU.mult,
                       r=[("xt", s2), (("ln1", s2), "mv"), (("ln1", s2), "rstd")], w=[("xn", s2)])
                    for kc in range(8):
                        bb = 2 * s2 + kc // 4
                        tr(bank(bb)[:, (kc % 4) * 128:(kc % 4 + 1) * 128], xn[s2][:, kc * 128:(kc + 1) * 128], identF,
                           r=[("xn", s2), "identF"], w=[bk(bb)])
                    for kc in range(8):
                        bb = 2 * s2 + kc // 4
                        act(uT[pb][:, kc, t4 * 128:(t4 + 1) * 128], bank(bb)[:, (kc % 4) * 128:(kc % 4 + 1) * 128], AF.Identity,
                            r=[bk(bb), "modT"], w=[("uT", pb, t4)], bias=modT[:, 0, kc:kc + 1], scale=modT[:, 1, kc:kc + 1])
                uTk = [("uT", pb, i) for i in range(4)]
                dma("sp", uTd[blk], uT[pb], r=uTk, w=[("uTd", blk)], sem=("uTst", pb))
                for ch in range(4):
                    c2 = ch % 2
                    for kc in range(8):
                        mm(bank(4), winc[:, kc, ch * 128:(ch + 1) * 128], uT[pb][:, kc, :], kc == 0, kc == 7,
                           r=uTk + [("winc", kc)], w=[bk(4)])
                    for kc in range(8):
                        mm(bank(5), winc[:, kc, 512 + ch * 128:512 + (ch + 1) * 128], uT[pb][:, kc, :], kc == 0, kc == 7,
                           r=uTk + [("winc", kc)], w=[bk(5)])
                    act(sg[c2], bank(5), AF.Exp, r=[bk(5), "b_inH"], w=[("sg", c2)], bias=b_inH[:, 8 + ch:9 + ch], scale=-1.0)
                    act(sg[c2], sg[c2], AF.Ln, r=[("sg", c2)], w=[("sg", c2)], bias=one_t)
                    act(sg[c2], sg[c2], AF.Exp, r=[("sg", c2)], w=[("sg", c2)], scale=-1.0)
                    stt(abuf[:, ch, 30 + blk * 512:30 + (blk + 1) * 512], bank(4), b_inT[:, 4 + ch:5 + ch], sg[c2], ALU.add, ALU.mult,
                        r=[bk(4), ("sg", c2), "b_inT"], w=[("abuf", ch, blk)])
                    for j in range(31):
                        mm(bank(6 + c2), dg[:, ch * 31 + j, :], abuf[:, ch, blk * 512 + j:blk * 512 + j + 512], j == 0, j == 30,
                           r=[("dg", ch), ("abuf", ch, blk), ("abuf", ch, blk - 1)], w=[bk(6 + c2)])
                    act(ac[pb][:, ch, :], bank(6 + c2), AF.Identity, r=[bk(6 + c2), "cbT"], w=[("ac", pb, ch)], bias=cbT[:, ch:ch + 1])
                    act(sq[c2], bank(6 + c2), AF.Square, r=[bk(6 + c2), "cbT"], w=[("sq", c2)], bias=cbT[:, ch:ch + 1])
                    mm(bank(2 * pb), onesM, ac[pb][:, ch, :], ch == 0, ch == 3, r=["onesM", ("ac", pb, ch)], w=[bk(2 * pb)])
                    mm(bank(2 * pb + 1), onesM, sq[c2], ch == 0, ch == 3, r=["onesM", ("sq", c2)], w=[bk(2 * pb + 1)])
                cp("act", meanS[pb], bank(2 * pb), r=[bk(2 * pb)], w=[("meanS", pb)])
                tt("dve", varS[pb], meanS[pb], meanS[pb], ALU.mult, r=[("meanS", pb)], w=[("varS", pb)])
                tt("dve", varS[pb], bank(2 * pb + 1), varS[pb], ALU.subtract, r=[bk(2 * pb + 1), ("varS", pb)], w=[("varS", pb)])
                act(rstdB[pb], varS[pb], AF.Ln, r=[("varS", pb)], w=[("rstdB", pb)], bias=eps_t)
                act(rstdB[pb], rstdB[pb], AF.Exp, r=[("rstdB", pb)], w=[("rstdB", pb)], scale=-0.5)
                for ch in range(4):
                    c2 = ch % 2
                    tt("dve", t1[c2], ac[pb][:, ch, :], meanS[pb], ALU.subtract, r=[("ac", pb, ch), ("meanS", pb)], w=[("t1", c2)])
                    tt("pool", t1[c2], t1[c2], rstdB[pb], ALU.mult, r=[("t1", c2), ("rstdB", pb)], w=[("t1", c2)])
                    act(yv[c2], t1[c2], AF.Identity, r=[("t1", c2), "cgT", "cbbT"], w=[("yv", c2)],
                        bias=cbbT[:, ch:ch + 1], scale=cgT[:, ch:ch + 1])
                    act(t1[c2], t1[c2], AF.Exp, r=[("t1", c2), "ncgT", "ncbbT"], w=[("t1", c2)],
                        bias=ncbbT[:, ch:ch + 1], scale=ncgT[:, ch:ch + 1])
                    act(t1[c2], t1[c2], AF.Ln, r=[("t1", c2)], w=[("t1", c2)], bias=one_t)
                    act(t1[c2], t1[c2], AF.Exp, r=[("t1", c2)], w=[("t1", c2)], scale=-1.0)
                    tt("dve", hTc[pb][:, ch, :], yv[c2], t1[c2], ALU.mult, r=[("yv", c2), ("t1", c2)], w=[("hTc", pb, ch)])
                dma("sp", hTd[blk, :, 4:8, :], hTc[pb], r=[("hTc", pb, ch) for ch in range(4)], w=[("hTd", blk, 1)], sem=("hTcst", pb))
            P.barrier()

            sb.reset(layer_mark)
            winh = sb([8, 2048], BF16)
            lbB = sb([512], F32)
            omlbB = sb([512], F32)
            nwB = sb([512], F32)
            b_inT = sb([4], F32)
            b_inH = sb([4], F32)
            omlbH = sb([512], F32)
            nwBH = sb([512], F32)
            brow = sb([1536], BF16, parts=1)
            uT = [sb([8, 512], BF16) for _ in range(2)]
            qT = [sb([4, 512], F32) for _ in range(2)]
            qb = [sb([512], F32) for _ in range(2)]
            zs = [sb([512], F32) for _ in range(2)]
            tmp = [sb([512], F32) for _ in range(2)]
            gt4 = [sb([4, 512], F32) for _ in range(2)]
            kk4 = [sb([4, 512], F32) for _ in range(2)]
            ec = [sb([512], F32) for _ in range(2)]
            sog = [sb([512], F32) for _ in range(2)]
            gate = [sb([512], F32) for _ in range(2)]
            sqo = [sb([512], F32) for _ in range(2)]
            khat = [sb([512], BF16) for _ in range(2)]
            vt = [sb([512], BF16) for _ in range(2)]
            eb = [sb([4, 128], F32) for _ in range(2)]
            enc = [sb([4, 128], F32) for _ in range(2)]
            qtA = [sb([4, 128], BF16) for _ in range(2)]
            qtB = [sb([4, 128], BF16) for _ in range(2)]
            qp = [sb([4, 128], BF16) for _ in range(2)]
            khT = [sb([4, 128], BF16) for _ in range(2)]
            AT = [sb([4, 128], BF16) for _ in range(2)]
            S = sb([4, 128], F32)
            Sb0 = [sb([4, 128], BF16) for _ in range(2)]
            Sb1 = [sb([4, 128], BF16) for _ in range(2)]
            ha = [sb([512], BF16) for _ in range(2)]
            hTa = [sb([4, 512], BF16) for _ in range(2)]
            ss = [sb([4], F32) for _ in range(2)]
            rs = [sb([4], F32) for _ in range(2)]
            lb2 = sb([2, 512], F32)
            print("A2 sbuf", sb.off)

            for kc in range(8):
                dma("sp", winh[:, kc, :], winb_d[kc * 128:(kc + 1) * 128, 0:2048], r=[("winb_d", kc)], w=[("winh", kc)], sem=("winh", kc))
            dma("sp", b_inT, b_in[l, 0:512].rearrange("(c p) -> p c", p=128), r=[], w=["b_inT"], sem="b_inTq", slow=True)
            ts("dve", b_inH, b_inT, -1.0, None, ALU.mult, None, r=["b_inT"], w=["b_inH"])
            dma("pool", brow, b_in[l:l + 1, 512:2048], r=[], w=["brow"], sem="brow")
            if l == 0:
                memset("pool", lbB, 0.0, w=["lbB"])
                memset("pool", omlbB, 1.0, w=["omlbB"])
            else:
                dma("sp", lb2.rearrange("p a b -> p (a b)"), hgrn_lb.rearrange("a b -> (a b)").partition_broadcast(128),
                    r=[], w=["lb2"], sem="lb2")
                act(lb2, lb2, AF.Exp, r=["lb2"], w=["lb2"])
                tt("dve", tmp[0], lb2[:, 0, :], lb2[:, 1, :], ALU.add, r=["lb2"], w=[("tmp", 0)])
                P.add("dve", lambda: nc.vector.reciprocal(out=tmp[0], in_=tmp[0]), r=[("tmp", 0)], w=[("tmp", 0)], cost=4.2)
                tt("dve", lbB, lb2[:, 1, :], tmp[0], ALU.mult, r=["lb2", ("tmp", 0)], w=["lbB"])
                tt("dve", omlbB, lb2[:, 0, :], tmp[0], ALU.mult, r=["lb2", ("tmp", 0)], w=["omlbB"])
            for h in range(4):
                dma("sp", nwB[:, h * 128:(h + 1) * 128], hgrn_nw[l].partition_broadcast(128), r=[], w=["nwB"], sem=("nwB", h))
            ts("dve", omlbH, omlbB, 0.5, None, ALU.mult, None, r=["omlbB"], w=["omlbH"])
            ts("dve", nwBH, nwB, 0.5, None, ALU.mult, None, r=["nwB"], w=["nwBH"])
            memset("pool", S, 0.0, w=["S"])
            memset("pool", Sb0[0], 0.0, w=[("Sb0", 0)])
            for p in range(2):
                memset("pool", qtA[p], 0.0, w=[("qtA", p)])
                memset("pool", qtB[p], 0.0, w=[("qtB", p)])

            for blk in range(NB):
                pb = blk % 2
                dma("sp", uT[pb], uTd[blk], r=[("uTd", blk)], w=[("uT", pb)], sem=("uTld", pb))
                for h in range(4):
                    b_ = 3 + 4 * (h % 2)
                    for kc in range(8):
                        mm(bank(b_), winh[:, kc, h * 128:(h + 1) * 128], uT[pb][:, kc, :], kc == 0, kc == 7,
                           r=[("uT", pb), ("winh", kc)], w=[bk(b_)])
                    h2 = h % 2
                    act(zs[h2], bank(b_), AF.Exp, r=[bk(b_), "b_inH"], w=[("zs", h2)], bias=b_inH[:, h:h + 1], scale=-1.0)
                    act(qb[h2], bank(b_), AF.Identity, r=[bk(b_), "b_inT"], w=[("qb", h2)], bias=b_inT[:, h:h + 1])
                    act(zs[h2], zs[h2], AF.Ln, r=[("zs", h2)], w=[("zs", h2)], bias=one_t)
                    act(zs[h2], zs[h2], AF.Exp, r=[("zs", h2)], w=[("zs", h2)], scale=-1.0)
                    tt("dve", qT[pb][:, h, :], zs[h2], qb[h2], ALU.mult, r=[("zs", h2), ("qb", h2)], w=[("qT", pb, h)])
                qk = [("qT", pb, h) for h in range(4)]
                for t4 in range(4):
                    p = (blk * 4 + t4) % 2
                    fb = 4 * p + 2
                    tsl = slice(t4 * 128, (t4 + 1) * 128)
                    for kc in range(8):
                        mm(bank(fb), uT[pb][:, kc, tsl], winh[:, kc, 512:1024], kc == 0, False,
                           r=[("uT", pb), ("winh", kc)], w=[bk(fb)])
                    mm(bank(fb), ones_row, brow[:, 0:512], False, True, r=["ones_row", "brow"], w=[bk(fb)])
                    act(zs[p], bank(fb), AF.Exp, r=[bk(fb)], w=[("zs", p)], scale=-1.0)
                    act(zs[p], zs[p], AF.Ln, r=[("zs", p)], w=[("zs", p)], bias=one_t)
                    act(zs[p], zs[p], AF.Exp, r=[("zs", p)], w=[("zs", p)], scale=-1.0)
                    tt("dve", tmp[p], zs[p], omlbB, ALU.mult, r=[("zs", p), "omlbB"], w=[("tmp", p)])
                    tt("pool", kk4[pb][:, t4, :], omlbB, tmp[p], ALU.subtract, r=["omlbB", ("tmp", p)], w=[("kk4", pb, t4)])
                    tt("dve", gt4[pb][:, t4, :], tmp[p], lbB, ALU.add, r=[("tmp", p), "lbB"], w=[("gt4", pb, t4)])
                g4k = [("gt4", pb, i) for i in range(4)]
                act(gt4[pb], gt4[pb], AF.Ln, r=g4k, w=g4k)
                for t4 in range(4):
                    t = blk * 4 + t4
                    p = t % 2
                    pn = 1 - p
                    B0 = 4 * p
                    tsl = slice(t4 * 128, (t4 + 1) * 128)
                    gt = [gt4[pb][:, t4, :]] * 2
                    kk = [kk4[pb][:, t4, :]] * 2
                    for pi, (pbk, col0) in ((1, (B0 + 1, 1024)), (2, (B0 + 2, 1536))):
                        for kc in range(8):
                            mm(bank(pbk), uT[pb][:, kc, tsl], winh[:, kc, col0:col0 + 512], kc == 0, False,
                               r=[("uT", pb), ("winh", kc)], w=[bk(pbk)])
                        mm(bank(pbk), ones_row, brow[:, pi * 512:(pi + 1) * 512], False, True, r=["ones_row", "brow"], w=[bk(pbk)])
                    cp("dve", vt[p], bank(B0 + 1), r=[bk(B0 + 1)], w=[("vt", p)])
                    act(sog[p], bank(B0 + 2), AF.Exp, r=[bk(B0 + 2)], w=[("sog", p)], scale=-1.0)
                    act(sog[p], sog[p], AF.Ln, r=[("sog", p)], w=[("sog", p)], bias=one_t)
                    act(sog[p], sog[p], AF.Exp, r=[("sog", p)], w=[("sog", p)], scale=-1.0)
                    tt("dve", sog[p], sog[p], bank(B0 + 2), ALU.mult, r=[("sog", p), bk(B0 + 2)], w=[("sog", p)])
                    tt("pool", gate[p], sog[p], nwB, ALU.mult, r=[("sog", p), "nwB"], w=[("gate", p)])
                    mm(bank(B0 + 3), M1, gt[p], True, True, r=["M1", ("gt4", pb, t4)], w=[bk(B0 + 3)])
                    for h in range(4):
                        mm(bank(B0 + 0)[:, h * 128:(h + 1) * 128], gt[p][:, h * 128:(h + 1) * 128], Lincl, True, True,
                           r=[("gt4", pb, t4), "Lincl"], w=[bk(B0 + 0)])
                    for h in range(4):
                        mm(bank(B0 + 1)[:, h * 128:(h + 1) * 128], gt[p][:, h * 128:(h + 1) * 128], M1, True, True,
                           r=[("gt4", pb, t4), "M1"], w=[bk(B0 + 1)])
                    act(ec[p], bank(B0 + 3), AF.Exp, r=[bk(B0 + 3)], w=[("ec", p)])
                    tt("dve", khat[p], kk[p], ec[p], ALU.mult, r=[("kk4", pb, t4), ("ec", p)], w=[("khat", p)])
                    act(eb[p].rearrange("p a b -> p (a b)"), bank(B0 + 0), AF.Exp, r=[bk(B0 + 0)], w=[("eb", p)])
                    ts("dve", enc[p].rearrange("p a b -> p (a b)"), bank(B0 + 1), -1.0, 75.0, ALU.mult, ALU.min, r=[bk(B0 + 1)], w=[("enc", p)])
                    act(enc[p], enc[p], AF.Exp, r=[("enc", p)], w=[("enc", p)])
                    tt("dve", qp[p], qT[pb][:, :, tsl], enc[p], ALU.mult, r=qk + [("enc", p)], w=[("qp", p)])
                    tt("pool", qtA[p][:, :, 0:64], qT[pb][:, :, t4 * 128:t4 * 128 + 64], eb[p][:, :, 0:64], ALU.mult,
                       r=qk + [("eb", p)], w=[("qtA", p)])
                    tt("pool", qtB[p][:, :, 64:128], qT[pb][:, :, t4 * 128 + 64:t4 * 128 + 128], eb[p][:, :, 64:128], ALU.mult,
                       r=qk + [("eb", p)], w=[("qtB", p)])
                    for h in range(4):
                        tr(bankb(B0 + 2)[:, h * 128:(h + 1) * 128], khat[p][:, h * 128:(h + 1) * 128], identB,
                           r=[("khat", p), "identB"], w=[bk(B0 + 2)])
                    cp("act", khT[p].rearrange("p a b -> p (a b)"), bankb(B0 + 2)[:, 0:512], r=[bk(B0 + 2)], w=[("khT", p)])
                    for h in range(4):
                        mm(bank(B0 + 3)[:, h * 128:(h + 1) * 128], khT[p][:, h, :], qp[p][:, h, :], True, True,
                           r=[("khT", p), ("qp", p)], w=[bk(B0 + 3)])
                    tt("dve", AT[p], bank(B0 + 3).rearrange("p (a b) -> p a b", a=4), maskB.unsqueeze(1).to_broadcast([128, 4, 128]), ALU.mult,
                       r=[bk(B0 + 3), "maskB"], w=[("AT", p)])
                    for h in range(4):
                        mm(bank(B0 + 0)[:, h * 128:(h + 1) * 128], khat[p][0:64, h * 128:(h + 1) * 128], vt[p][0:64, h * 128:(h + 1) * 128],
                           True, True, r=[("khat", p), ("vt", p)], w=[bk(B0 + 0)])
                    for h in range(4):
                        stt(S[:, h, :], S[:, h, :], eb[p][:, h, 63:64], bank(B0 + 0)[:, h * 128:(h + 1) * 128], ALU.mult, ALU.add,
                            r=["S", ("eb", p), bk(B0 + 0)], w=["S"])
                    cp("pool", Sb1[p], S, r=["S"], w=[("Sb1", p)])
                    for h in range(4):
                        hs = slice(h * 128, (h + 1) * 128)
                        mm(bank(B0 + 1)[:, hs], AT[p][:, h, :], vt[p][:, hs], True, False, r=[("AT", p), ("vt", p)], w=[bk(B0 + 1)])
                        mm(bank(B0 + 1)[:, hs], qtA[p][:, h, :], Sb0[p][:, h, :], False, False, r=[("qtA", p), ("Sb0", p)], w=[bk(B0 + 1)])
                        mm(bank(B0 + 1)[:, hs], qtB[p][:, h, :], Sb1[p][:, h, :], False, True, r=[("qtB", p), ("Sb1", p)], w=[bk(B0 + 1)])
                    for h in range(4):
                        mm(bank(B0 + 2)[:, h * 128:(h + 1) * 128], khat[p][64:128, h * 128:(h + 1) * 128], vt[p][64:128, h * 128:(h + 1) * 128],
                           True, True, r=[("khat", p), ("vt", p)], w=[bk(B0 + 2)])
                    for h in range(4):
                        stt(S[:, h, :], S[:, h, :], eb[p][:, h, 127:128], bank(B0 + 2)[:, h * 128:(h + 1) * 128], ALU.mult, ALU.add,
                            r=["S", ("eb", p), bk(B0 + 2)], w=["S"])
                    cp("pool", Sb0[pn], S, r=["S"], w=[("Sb0", pn)])
                    act(sqo[p], bank(B0 + 1), AF.Square, r=[bk(B0 + 1)], w=[("sqo", p)])
                    P.add("dve", (lambda p=p: nc.vector.tensor_reduce(out=ss[p], in_=sqo[p].rearrange("p (a b) -> p a b", a=4), axis=AX.X, op=ALU.add)),
                          r=[("sqo", p)], w=[("ss", p)], cost=0.65)
                    act(rs[p], ss[p], AF.Ln, r=[("ss", p)], w=[("rs", p)], bias=eps_t, scale=1.0 / 128.0)
                    act(rs[p], rs[p], AF.Exp, r=[("rs", p)], w=[("rs", p)], scale=-0.5)
                    for h in range(4):
                        hs = slice(h * 128, (h + 1) * 128)
                        stt(ha[p][:, hs], bank(B0 + 1)[:, hs], rs[p][:, h:h + 1], gate[p][:, hs], ALU.mult, ALU.mult,
                            r=[bk(B0 + 1), ("rs", p), ("gate", p)], w=[("ha", p)])
                    for h in range(4):
                        tr(bankb(B0 + 3)[:, h * 128:(h + 1) * 128], ha[p][:, h * 128:(h + 1) * 128], identB, r=[("ha", p), "identB"], w=[bk(B0 + 3)])
                    for h in range(4):
                        cp("act", hTa[pb][:, h, tsl], bankb(B0 + 3)[:, h * 128:(h + 1) * 128], r=[bk(B0 + 3)], w=[("hTa", pb, t4)])
                dma("sp", hTd[blk, :, 0:4, :], hTa[pb], r=[("hTa", pb, i) for i in range(4)], w=[("hTd", blk, 0)], sem=("hTast", pb))
            P.barrier()
            if stage == 1:
                final_keys.append(("hTd", NB - 1, 0))
                break

            sb.reset(layer_mark)
            wout = sb([8, D], BF16)
            brow_o = sb([D], BF16, parts=1)
            ln1gB = sb([D], F32)
            ln1bB = sb([D], F32)
            rw = sb([8, NE], F32)
            hTb = [sb([8, 512], BF16) for _ in range(2)]
            xtb = [sb([D], F32) for _ in range(2)]
            zt = [sb([D], F32) for _ in range(2)]
            x1 = [sb([D], F32) for _ in range(2)]
            xn2 = [sb([D], F32) for _ in range(2)]
            u2T = [sb([8, 128], F32) for _ in range(2)]
            u2Tb = [sb([8, 512], BF16) for _ in range(2)]
            statsb = [sb([2, 6], F32) for _ in range(2)]
            mvb = [sb([2], F32) for _ in range(2)]
            rstdb = [sb([1], F32) for _ in range(2)]
            nmrb = [sb([1], F32) for _ in range(2)]
            stats2 = [sb([2, 6], F32) for _ in range(2)]
            mv2 = [sb([2], F32) for _ in range(2)]
            rstd2 = [sb([1], F32) for _ in range(2)]
            nmr2 = [sb([1], F32) for _ in range(2)]
            for kc in range(8):
                dma("sp", wout[:, kc, :], woutb_d[kc * 128:(kc + 1) * 128, :], r=[("woutb_d", kc)], w=["wout"], sem=("wout", kc))
            dma("pool", brow_o, b_out[l:l + 1, :], r=[], w=["brow_o"], sem="brow_o")
            dma("sp", ln1gB, ln1_g[l].partition_broadcast(128), r=[], w=["ln1gB"], sem="ln1gB")
            dma("sp", ln1bB, ln1_b[l].partition_broadcast(128), r=[], w=["ln1bB"], sem="ln1bB")
            dma("sp", rw, router_w.rearrange("(k p) e -> p k e", p=128), r=[], w=["rw"], sem="rw", slow=True)
            for blk in range(NB):
                hb = blk % 2
                dma("sp", hTb[hb], hTd[blk], r=[("hTd", blk, 0), ("hTd", blk, 1)], w=[("hTb", hb)], sem=("hTb", hb))
                for t4 in range(4):
                    t = blk * 4 + t4
                    s2 = t % 2
                    Y0 = 2 * s2
                    X0 = 4 + 2 * s2
                    tsl = slice(t4 * 128, (t4 + 1) * 128)
                    dma("sp", xtb[s2], xsrc[t * 128:(t + 1) * 128, :], r=[], w=[("xtb", s2)], sem=("xtb", s2))
                    for hf in range(2):
                        for cc in range(8):
                            mm(bank(Y0 + hf), hTb[hb][:, cc, tsl], wout[:, cc, hf * 512:(hf + 1) * 512], cc == 0, False,
                               r=[("hTb", hb), "wout"], w=[bk(Y0 + hf)])
                        mm(bank(Y0 + hf), ones_row, brow_o[:, hf * 512:(hf + 1) * 512], False, True, r=["ones_row", "brow_o"], w=[bk(Y0 + hf)])
                    tt("dve", zt[s2], ps[:, Y0:Y0 + 2, :].rearrange("p a b -> p (a b)"), g1B, ALU.mult, r=[bk(Y0), bk(Y0 + 1), "g1B"], w=[("zt", s2)])
                    stt(zt[s2], xtb[s2], ALPHA, zt[s2], ALU.mult, ALU.add, r=[("xtb", s2), ("zt", s2)], w=[("zt", s2)])
                    ln_stats(zt[s2], statsb[s2], mvb[s2], rstdb[s2], nmrb[s2], ("pl1", s2), [("zt", s2)])
                    act(x1[s2], zt[s2], AF.Identity, r=[("zt", s2), (("pl1", s2), "rstd"), (("pl1", s2), "nmr")], w=[("x1", s2)],
                        bias=nmrb[s2], scale=rstdb[s2])
                    tt("dve", x1[s2], x1[s2], ln1gB, ALU.mult, r=[("x1", s2), "ln1gB"], w=[("x1", s2)])
                    tt("pool", x1[s2], x1[s2], ln1bB, ALU.add, r=[("x1", s2), "ln1bB"], w=[("x1", s2)])
                    dma("sp", x1d[t * 128:(t + 1) * 128, :], x1[s2], r=[("x1", s2)], w=[("x1d", t)], sem=("x1st", s2))
                    ln_stats(x1[s2], stats2[s2], mv2[s2], rstd2[s2], nmr2[s2], ("ln2", s2), [("x1", s2)])
                    act(xn2[s2], x1[s2], AF.Identity, r=[("x1", s2), (("ln2", s2), "rstd"), (("ln2", s2), "nmr")], w=[("xn2", s2)],
                        bias=nmr2[s2], scale=rstd2[s2])
                    for kc in range(8):
                        tr(bank(X0 + kc // 4)[:, (kc % 4) * 128:(kc % 4 + 1) * 128], xn2[s2][:, kc * 128:(kc + 1) * 128], identF,
                           r=[("xn2", s2), "identF"], w=[bk(X0 + kc // 4)])
                    for kc in range(8):
                        act(u2T[s2][:, kc, :], bank(X0 + kc // 4)[:, (kc % 4) * 128:(kc % 4 + 1) * 128], AF.Identity,
                            r=[bk(X0 + kc // 4), "modT"], w=[("u2T", s2)], bias=modT[:, 2, kc:kc + 1], scale=modT[:, 3, kc:kc + 1])
                    for kc in range(8):
                        mm(bank(X0)[:, 0:NE], u2T[s2][:, kc, :], rw[:, kc, :], kc == 0, kc == 7, r=[("u2T", s2), "rw"], w=[bk(X0)])
                    cp("dve", logits[:, t * NE:(t + 1) * NE], bank(X0)[:, 0:NE], r=[bk(X0)], w=["logits"])
                    cp("pool", u2Tb[hb][:, :, tsl], u2T[s2], r=[("u2T", s2)], w=[("u2Tb", hb)])
                dma("sp", u2Td[blk], u2Tb[hb], r=[("u2Tb", hb)], w=[("u2Td", blk)], sem=("u2Tst", hb))
            if dbg:
                dma("sp", logd, logits, r=["logits"], w=["logd"], sem="logd")
                final_keys.append("logd")
            P.barrier()
            if stage == 2:
                final_keys += [("x1d", NT - 1), ("u2Td", NB - 1)]
                break


            sb.reset(layer_mark)
            NG = NT * 4
            s_t = sb([NT * NE], F32)
            sbv = sb([NT * NE], F32)
            m1 = sb([NG], F32)
            is1 = sb([NT * NE], F32)
            G2 = sb([NT * NE], F32)
            m2 = sb([NG], F32)
            gs = sb([NG], F32)
            gmax = sb([NT], F32)
            gsel = sb([NG], F32)
            top2 = sb([NT * NE], F32)
            den = sb([NT], F32)
            g3 = lambda a: a.rearrange("p (g e) -> p g e", e=4)
            t3 = lambda a: a.rearrange("p (t e) -> p t e", t=NT)
            act(s_t, logits, AF.Sigmoid, r=["logits"], w=["s_t"])
            tt("dve", t3(sbv), t3(s_t), biasB.unsqueeze(1).to_broadcast([128, NT, NE]), ALU.add, r=["s_t", "biasB"], w=["sbv"])
            P.add("dve", lambda: nc.vector.tensor_reduce(out=m1, in_=g3(sbv), axis=AX.X, op=ALU.max), r=["sbv"], w=["m1"])
            tt("dve", g3(is1), g3(sbv), m1.unsqueeze(2).to_broadcast([128, NG, 4]), ALU.is_ge, r=["sbv", "m1"], w=["is1"])
            stt(G2, is1, -1.0e9, sbv, ALU.mult, ALU.add, r=["is1", "sbv"], w=["G2"])
            P.add("dve", lambda: nc.vector.tensor_reduce(out=m2, in_=g3(G2), axis=AX.X, op=ALU.max), r=["G2"], w=["m2"])
            tt("dve", gs, m1, m2, ALU.add, r=["m1", "m2"], w=["gs"])
            P.add("dve", lambda: nc.vector.tensor_reduce(out=gmax, in_=gs.rearrange("p (t g) -> p t g", g=4), axis=AX.X, op=ALU.max),
                  r=["gs"], w=["gmax"])
            tt("dve", gsel.rearrange("p (t g) -> p t g", g=4), gs.rearrange("p (t g) -> p t g", g=4),
               gmax.unsqueeze(2).to_broadcast([128, NT, 4]), ALU.is_ge, r=["gs", "gmax"], w=["gsel"])
            tt("dve", g3(top2), g3(sbv), m2.unsqueeze(2).to_broadcast([128, NG, 4]), ALU.is_ge, r=["sbv", "m2"], w=["top2"])
            tt("dve", g3(top2), g3(top2), gsel.unsqueeze(2).to_broadcast([128, NG, 4]), ALU.mult, r=["top2", "gsel"], w=["top2"])
            tt("dve", top2, top2, s_t, ALU.mult, r=["top2", "s_t"], w=["top2"])
            P.add("dve", lambda: nc.vector.tensor_reduce(out=den, in_=t3(top2), axis=AX.X, op=ALU.add), r=["top2"], w=["den"])
            P.add("dve", lambda: nc.vector.reciprocal(out=den, in_=den), r=["den"], w=["den"])
            tt("dve", t3(comb), t3(top2), den.unsqueeze(2).to_broadcast([128, NT, NE]), ALU.mult, r=["top2", "den"], w=["comb"])
            if dbg:
                dma("sp", combd, comb, r=["comb"], w=["combd"], sem="combd")
                final_keys.append("combd")
            P.barrier()
            if stage == 3:
                break

            sb.reset(layer_mark)
            NSB = 2
            TPS = NT // NSB
            wg = [sb([8, 512], BF16) for _ in range(2)]
            wu = [sb([8, 512], BF16) for _ in range(2)]
            wd = [sb([4, D], BF16) for _ in range(2)]
            u2s = sb([4, 8, 512], BF16)
            yacc = sb([TPS, D], F32)
            hTm = [sb([4, 512], BF16) for _ in range(2)]
            sgt = [sb([512], F32) for _ in range(2)]
            ln2gB = sb([D], F32)
            ln2bB = sb([D], F32)
            xe = [sb([D], F32) for _ in range(2)]
            ze = sb([D], F32)
            xo = [sb([D], F32) for _ in range(2)]
            statse = sb([2, 6], F32)
            mve = sb([2], F32)
            rstde = sb([1], F32)
            nmre = sb([1], F32)
            dma("sp", ln2gB, ln2_g[l].partition_broadcast(128), r=[], w=["ln2gB"], sem="ln2gB")
            dma("sp", ln2bB, ln2_b[l].partition_broadcast(128), r=[], w=["ln2bB"], sem="ln2bB")
            wcnt = 0
            hcnt = 0
            for sbk in range(NSB):
                for b4 in range(4):
                    dma("sp", u2s[:, b4, :, :], u2Td[sbk * 4 + b4], r=[("u2Td", sbk * 4 + b4)], w=[("u2s", b4)], sem=("u2s", b4))
                for e in range(NE):
                    ws = wcnt % 2
                    wcnt += 1
                    dma("pool", wg[ws], w_gate[l, e].rearrange("(k p) f -> p k f", p=128), r=[], w=[("wg", ws)], sem=("wg", ws))
                    dma("pool", wu[ws], w_up[l, e].rearrange("(k p) f -> p k f", p=128), r=[], w=[("wu", ws)], sem=("wu", ws))
                    dma("pool", wd[ws], w_down[l, e].rearrange("(k p) n -> p k n", p=128), r=[], w=[("wd", ws)], sem=("wd", ws))
                    for b4 in range(4):
                        hs_ = hcnt % 2
                        hcnt += 1
                        for fc in range(4):
                            bg = fc % 2
                            bu = 2 + fc % 2
                            for kc in range(8):
                                mm(bank(bg), wg[ws][:, kc, fc * 128:(fc + 1) * 128], u2s[:, b4, kc, :], kc == 0, kc == 7,
                                   r=[("wg", ws), ("u2s", b4)], w=[bk(bg)])
                            for kc in range(8):
                                mm(bank(bu), wu[ws][:, kc, fc * 128:(fc + 1) * 128], u2s[:, b4, kc, :], kc == 0, kc == 7,
                                   r=[("wu", ws), ("u2s", b4)], w=[bk(bu)])
                            act(sgt[fc % 2], bank(bg), AF.Silu, r=[bk(bg)], w=[("sgt", fc % 2)])
                            tt("dve", hTm[hs_][:, fc, :], sgt[fc % 2], bank(bu), ALU.mult, r=[("sgt", fc % 2), bk(bu)], w=[("hTm", hs_, fc)])
                        for t4 in range(4):
                            tl = b4 * 4 + t4
                            tg = sbk * TPS + tl
                            yb = 4 + 2 * (t4 % 2)
                            for hf in range(2):
                                for fc in range(4):
                                    mm(bank(yb + hf), hTm[hs_][:, fc, t4 * 128:(t4 + 1) * 128], wd[ws][:, fc, hf * 512:(hf + 1) * 512],
                                       fc == 0, fc == 3, r=[("hTm", hs_, fc), ("wd", ws)], w=[bk(yb + hf)])
                            ysrc = ps[:, yb:yb + 2, :].rearrange("p a b -> p (a b)")
                            cw = comb[:, tg * NE + e:tg * NE + e + 1]
                            if e == 0:
                                ts("dve", yacc[:, tl, :], ysrc, cw, None, ALU.mult, None, r=[bk(yb), bk(yb + 1), "comb"], w=[("yacc", tl)])
                            else:
                                stt(yacc[:, tl, :], ysrc, cw, yacc[:, tl, :], ALU.mult, ALU.add,
                                    r=[bk(yb), bk(yb + 1), "comb", ("yacc", tl)], w=[("yacc", tl)])
                for tl in range(TPS):
                    tg = sbk * TPS + tl
                    s2 = tg % 2
                    dma("sp", xe[s2], x1d[tg * 128:(tg + 1) * 128, :], r=[("x1d", tg)], w=[("xe", s2)], sem=("xe", s2))
                    tt("dve", ze, yacc[:, tl, :], g2B, ALU.mult, r=[("yacc", tl), "g2B"], w=["ze"])
                    stt(ze, xe[s2], ALPHA, ze, ALU.mult, ALU.add, r=[("xe", s2), "ze"], w=["ze"])
                    ln_stats(ze, statse, mve, rstde, nmre, "pl2", ["ze"])
                    act(xo[s2], ze, AF.Identity, r=["ze", ("pl2", "rstd"), ("pl2", "nmr")], w=[("xo", s2)], bias=nmre, scale=rstde)
                    tt("dve", xo[s2], xo[s2], ln2gB, ALU.mult, r=[("xo", s2), "ln2gB"], w=[("xo", s2)])
                    tt("pool", xo[s2], xo[s2], ln2bB, ALU.add, r=[("xo", s2), "ln2bB"], w=[("xo", s2)])
                    dma("sp", xdst[tg * 128:(tg + 1) * 128, :], xo[s2], r=[("xo", s2)], w=[("xdst", l, tg)], sem=("xost", s2))
                    if l == nlayers - 1 or stage == 4:
                        final_keys.append(("xdst", l, tg))
            P.barrier()
            if stage == 4:
                break
        P.emit(top, final_keys=final_keys)
        build.stats = dict(P.stats, sb_peak=sb.peak)
    return nc


_IN_NAMES = ["ada_w", "ada_b", "w_in", "b_in", "hgrn_lb", "hgrn_norm_w", "conv_w", "conv_b", "conv_ln_g", "conv_ln_b",
             "w_out", "b_out", "ln1_g", "ln1_b", "router_w", "router_bias", "w_gate", "w_up", "w_down", "ln2_g", "ln2_b"]


def make_in_maps(inputs, ncores=NCORES):
    shared = {k: np.ascontiguousarray(np.asarray(inputs[k], dtype=np.float32)) for k in _IN_NAMES}
    x = np.asarray(inputs["x"], dtype=np.float32)
    c = np.asarray(inputs["c"], dtype=np.float32)
    maps = []
    for i in range(ncores):
        m = dict(shared)
        m["x"] = np.ascontiguousarray(x[i])
        m["c"] = np.ascontiguousarray(c[i])
        maps.append(m)
    return maps


def kernel(**inputs):
    nc = build()
    in_maps = make_in_maps(inputs)
    res = run_bass_kernel_spmd(nc, in_maps, core_ids=list(range(NCORES)))
    return np.stack([np.asarray(r["out"], dtype=np.float32) for r in res.results], axis=0)
```

```python
import numpy as np
from contextlib import ExitStack
import concourse.bass as bass
import concourse.mybir as mybir
from concourse.bass_utils import run_bass_kernel_spmd

F32 = mybir.dt.float32
BF16 = mybir.dt.bfloat16
U8 = mybir.dt.uint8
AF = mybir.ActivationFunctionType
ALU = mybir.AluOpType
AX = mybir.AxisListType

T = 4096
D = 1024
NT = 32
NB = 8
NE = 16
ALPHA = 4.0 ** 0.25
EPS = 1e-5
NCORES = 8
SAME_ENG_SYNC = True


class Prog:
    DMA_BW = 300e3

    def __init__(self, nc):
        self.nc = nc
        self.ops = []
        self.eng = {"pe": nc.tensor, "act": nc.scalar, "dve": nc.vector,
                    "pool": nc.gpsimd, "sp": nc.sync}

    def add(self, eng, fn, r=(), w=(), cost=0.2):
        self.ops.append(dict(eng=eng, fn=fn, r=tuple(r), w=tuple(w), dma=None, bar=False, cost=cost, nbytes=0))

    def dma(self, q, fn, r=(), w=(), sem=None, nbytes=0):
        assert sem is not None
        self.ops.append(dict(eng=q, fn=fn, r=tuple(r), w=tuple(w), dma=sem, bar=False,
                             cost=(1.0 if q == "pool" else 0.1), nbytes=nbytes))

    def barrier(self):
        self.ops.append(dict(eng=None, fn=None, r=(), w=(), dma=None, bar=True, cost=0.0, nbytes=0))

    def _raw_deps(self):
        ops = self.ops
        n = len(ops)
        last_writer = {}
        readers = {}
        deps = [None] * n
        for i, op in enumerate(ops):
            if op["bar"]:
                deps[i] = set()
                continue
            d = set()
            for k in op["r"]:
                if k in last_writer:
                    d.add(last_writer[k])
            for k in op["w"]:
                if k in last_writer:
                    d.add(last_writer[k])
                d.update(readers.get(k, ()))
            d.discard(i)
            deps[i] = d
            for k in op["r"]:
                readers.setdefault(k, []).append(i)
            for k in op["w"]:
                last_writer[k] = i
                readers[k] = []
        return deps, last_writer

    def _schedule_segment(self, idxs, deps):
        ops = self.ops
        inseg = set(idxs)
        nrem = {}
        users = {}
        for i in idxs:
            dl = [j for j in deps[i] if j in inseg]
            nrem[i] = len(dl)
            for j in dl:
                users.setdefault(j, []).append(i)
        ready = {e: [] for e in self.eng}
        ready_time = {}
        finish = {}
        import heapq
        for i in idxs:
            if nrem[i] == 0:
                ready_time[i] = 0.0
                heapq.heappush(ready[ops[i]["eng"]], (0.0, i))
        etime = {e: 0.0 for e in self.eng}
        dma_free = 0.0
        order = []
        remaining = len(idxs)
        while remaining:
            best = None
            for e in self.eng:
                if not ready[e]:
                    continue
                rt, i = ready[e][0]
                st = max(etime[e], rt)
                cand = (st, i, e)
                cands = [(max(etime[e], r_), i_) for (r_, i_) in ready[e] if r_ <= st]
                if cands:
                    i2 = min(c[1] for c in cands)
                    cand = (st, i2, e)
                if best is None or cand < best:
                    best = cand
            st, i, e = best
            lst = ready[e]
            for k_, (r_, i_) in enumerate(lst):
                if i_ == i:
                    lst[k_] = lst[-1]
                    lst.pop()
                    heapq.heapify(lst)
                    break
            op = ops[i]
            if op["dma"] is not None:
                etime[e] = st + op["cost"]
                t0 = max(st + op["cost"], dma_free)
                dma_free = t0 + op["nbytes"] / self.DMA_BW
                fin = dma_free + 2.0
            else:
                fin = st + op["cost"]
                etime[e] = fin
            finish[i] = fin
            order.append(i)
            remaining -= 1
            for u in users.get(i, ()):
                nrem[u] -= 1
                rt_u = max(ready_time.get(u, 0.0), fin + 0.1)
                ready_time[u] = rt_u
                if nrem[u] == 0:
                    heapq.heappush(ready[ops[u]["eng"]], (rt_u, u))
        mk = max(finish.values()) if finish else 0.0
        return order, mk

    def emit(self, stack, final_keys=(), schedule=True):
        nc = self.nc
        ops = self.ops
        n = len(ops)
        deps, last_writer = self._raw_deps()
        order = []
        seg = []
        seg_times = []
        for i, op in enumerate(ops):
            if op["bar"]:
                if seg:
                    if schedule:
                        o, mk = self._schedule_segment(seg, deps)
                    else:
                        o, mk = list(seg), 0.0
                    order += o
                    seg_times.append(mk)
                order.append(i)
                seg = []
            else:
                seg.append(i)
        if seg:
            if schedule:
                o, mk = self._schedule_segment(seg, deps)
            else:
                o, mk = list(seg), 0.0
            order += o
            seg_times.append(mk)
        pos = {i: p for p, i in enumerate(order)}
        last_on_eng = {}
        dmas_since = []
        pend = {e: set() for e in self.eng}
        fdeps = [None] * n
        for i in order:
            op = ops[i]
            if op["bar"]:
                d = set(last_on_eng.values()) | set(dmas_since)
                for e in self.eng:
                    pend[e] |= d
                dmas_since = []
                fdeps[i] = set()
                continue
            d = set(deps[i])
            e = op["eng"]
            if pend[e]:
                d |= pend[e]
                pend[e] = set()
            best = {}
            dd = set()
            for j in d:
                oj = ops[j]
                if oj["dma"] is not None:
                    dd.add(j)
                elif oj["eng"] not in best or pos[best[oj["eng"]]] < pos[j]:
                    best[oj["eng"]] = j
            dd.update(best.values())
            fdeps[i] = dd
            last_on_eng[e] = i
            if op["dma"] is not None:
                dmas_since.append(i)
        need_sig = [False] * n
        for i in order:
            op = ops[i]
            if op["bar"]:
                continue
            for j in fdeps[i]:
                oj = ops[j]
                if oj["dma"] is not None:
                    continue
                if oj["eng"] == op["eng"] and op["dma"] is None and (oj["eng"] == "pe" or not SAME_ENG_SYNC):
                    continue
                need_sig[j] = True
        final_deps = set(last_writer[k] for k in final_keys)
        esem = {e: stack.enter_context(nc.semaphore("s_" + e)) for e in self.eng}
        dsem = {}
        dcount = {}
        ecount = {e: 0 for e in self.eng}
        waited = {e: {} for e in self.eng}
        sig = [None] * n
        nwaits = 0
        phys = []
        pcount = []
        free_phys = []
        for i in order:
            op = ops[i]
            if op["bar"]:
                free_phys = list(range(len(phys)))
                dsem = {}
                continue
            e = op["eng"]
            eng = self.eng[e]
            for j in sorted(fdeps[i], key=lambda j: pos[j]):
                oj = ops[j]
                if oj["dma"] is None and oj["eng"] == e and op["dma"] is None and (e == "pe" or not SAME_ENG_SYNC):
                    continue
                assert pos[j] < pos[i], (i, j)
                sname, val = sig[j]
                if waited[e].get(sname, 0) >= val:
                    continue
                semh = esem[sname] if sname in esem else phys[sname]
                eng.wait_ge(semh, val)
                nwaits += 1
                waited[e][sname] = val
            inst = op["fn"]()
            if op["dma"] is not None:
                key = op["dma"]
                if key not in dsem:
                    if free_phys:
                        dsem[key] = free_phys.pop()
                    else:
                        phys.append(stack.enter_context(nc.semaphore("d_%d" % len(phys))))
                        pcount.append(0)
                        dsem[key] = len(phys) - 1
                pid = dsem[key]
                pcount[pid] += 16
                inst.then_inc(phys[pid], 16)
                sig[i] = (pid, pcount[pid])
            elif need_sig[i]:
                ecount[e] += 1
                inst.then_inc(esem[e], 1)
                sig[i] = (e, ecount[e])
        eng = self.eng["sp"]
        for j in sorted(final_deps):
            sname, val = sig[j]
            semh = esem[sname] if sname in esem else phys[sname]
            eng.wait_ge(semh, val)
        self.stats = dict(nops=n, nwaits=nwaits, ecount=ecount, ndsem=len(phys),
                          seg_us=[round(t) for t in seg_times])


class SBAlloc:
    def __init__(self, big, nbytes):
        self.big = big
        self.cap = nbytes
        self.off = 0
        self.peak = 0

    def mark(self):
        return self.off

    def reset(self, m):
        self.off = m

    def __call__(self, free_shape, dt, parts=128):
        esz = {F32: 4, BF16: 2, U8: 1, mybir.dt.int32: 4, mybir.dt.uint32: 4}[dt]
        nel = int(np.prod(free_shape))
        nbytes = (nel * esz + 63) // 64 * 64
        assert self.off + nbytes <= self.cap, ("SBUF overflow", self.off, nbytes, self.cap)
        ap = self.big[0:parts, self.off:self.off + nel * esz].bitcast(dt)
        self.off += nbytes
        self.peak = max(self.peak, self.off)
        if len(free_shape) == 2:
            ap = ap.rearrange("p (a b) -> p a b", a=free_shape[0], b=free_shape[1])
        elif len(free_shape) == 3:
            ap = ap.rearrange("p (a b c) -> p a b c", a=free_shape[0], b=free_shape[1], c=free_shape[2])
        return ap


def build(stage=99, dbg=False):
    nc = bass.Bass("TRN2", target_bir_lowering=False)
    dk = "ExternalOutput" if dbg else "Internal"

    def din(name, shape):
        return nc.dram_tensor(name, list(shape), F32, kind="ExternalInput").ap()

    x_d = din("x", [T, D])
    c_d = din("c", [D])
    ada_w = din("ada_w", [2, D, 6 * D])
    ada_b = din("ada_b", [2, 6 * D])
    w_in = din("w_in", [2, D, 3072])
    b_in = din("b_in", [2, 3072])
    hgrn_lb = din("hgrn_lb", [2, 512])
    hgrn_nw = din("hgrn_norm_w", [2, 128])
    conv_w = din("conv_w", [2, 31, 512])
    conv_b = din("conv_b", [2, 512])
    conv_g = din("conv_ln_g", [2, 512])
    conv_bb = din("conv_ln_b", [2, 512])
    w_out = din("w_out", [2, D, D])
    b_out = din("b_out", [2, D])
    ln1_g = din("ln1_g", [2, D])
    ln1_b = din("ln1_b", [2, D])
    router_w = din("router_w", [D, NE])
    router_bias = din("router_bias", [NE])
    w_gate = din("w_gate", [2, NE, D, 512])
    w_up = din("w_up", [2, NE, D, 512])
    w_down = din("w_down", [2, NE, 512, D])
    ln2_g = din("ln2_g", [2, D])
    ln2_b = din("ln2_b", [2, D])
    out_d = nc.dram_tensor("out", [T, D], F32, kind="ExternalOutput").ap()
    hTd = nc.dram_tensor("hTd", [NB, 128, 8, 512], BF16, kind=dk).ap()
    uTd = nc.dram_tensor("uTd", [NB, 128, 8, 512], BF16, kind="Internal").ap()
    winb_d = nc.dram_tensor("winb_d", [D, 3072], BF16, kind="Internal").ap()
    woutb_d = nc.dram_tensor("woutb_d", [D, D], BF16, kind="Internal").ap()
    x1d = nc.dram_tensor("x1d", [T, D], F32, kind=dk).ap()
    xn2d = nc.dram_tensor("xn2d", [T, D], BF16, kind=dk).ap()
    NSLOT = 32 * 512
    Ud = nc.dram_tensor("Ud", [NSLOT, D], BF16, kind="Internal").ap()
    Yd = nc.dram_tensor("Yd", [NSLOT, D], F32, kind="Internal").ap()
    modrow_d = nc.dram_tensor("modrow_d", [2, D], F32, kind="Internal").ap()
    slotd = nc.dram_tensor("slotd", [128, 4 * NT], F32, kind=dk).ap()
    xmid = nc.dram_tensor("xmid", [T, D], F32, kind=dk).ap()
    logd = nc.dram_tensor("logd", [128, NT * NE], F32, kind=dk).ap()
    combd = nc.dram_tensor("combd", [128, NT * NE], F32, kind=dk).ap()
    modTd = nc.dram_tensor("modTd", [128, 32], F32, kind=dk).ap()

    SB_BYTES = 207 * 1024
    with ExitStack() as top:
        big = top.enter_context(nc.sbuf_tensor("big", [128, SB_BYTES], U8))
        ps = top.enter_context(nc.psum_tensor("ps", [128, 8, 512], F32))
        sb = SBAlloc(big, SB_BYTES)
        P = Prog(nc)

        def bank(b):
            return ps[:, b, :]

        def bankb(b):
            return ps[:, b, :].bitcast(BF16)

        def bk(b):
            return ("ps", b)

        def fsz(ap):
            return int(np.prod(ap.shape[1:]))

        def mm(out, lhsT, rhs, start, stop, r, w):
            c = max(fsz(out), 64) * (4 if rhs.dtype == F32 else 1) / 2000.0 + 0.02
            P.add("pe", lambda: nc.tensor.matmul(out, lhsT=lhsT, rhs=rhs, start=start, stop=stop), r=r, w=w, cost=c)

        def tr(out, in_, ident, r, w):
            P.add("pe", lambda: nc.tensor.transpose(out=out, in_=in_, identity=ident), r=r, w=w, cost=0.1)

        def act(out, in_, func, r, w, bias=0.0, scale=1.0, accum=None):
            c = 0.2 + fsz(out) * 0.00085
            if accum is None:
                P.add("act", lambda: nc.scalar.activation(out=out, in_=in_, func=func, bias=bias, scale=scale), r=r, w=w, cost=c)
            else:
                P.add("act", lambda: nc.scalar.activation(out=out, in_=in_, func=func, bias=bias, scale=scale, accum_out=accum), r=r, w=w, cost=c)

        def tt(eng, out, in0, in1, op, r, w):
            e = nc.vector if eng == "dve" else nc.gpsimd
            c = (0.1 + fsz(out) * 0.00105) if eng == "dve" else (0.2 + fsz(out) * 0.0021)
            P.add(eng, lambda: e.tensor_tensor(out=out, in0=in0, in1=in1, op=op), r=r, w=w, cost=c)

        def ts(eng, out, in0, s1, s2, op0, op1, r, w):
            e = nc.vector if eng == "dve" else nc.gpsimd
            c = (0.1 + fsz(out) * 0.00105) if eng == "dve" else (0.2 + fsz(out) * 0.0021)
            if op1 is None:
                P.add(eng, lambda: e.tensor_scalar(out=out, in0=in0, scalar1=s1, scalar2=None, op0=op0), r=r, w=w, cost=c)
            else:
                P.add(eng, lambda: e.tensor_scalar(out=out, in0=in0, scalar1=s1, scalar2=s2, op0=op0, op1=op1), r=r, w=w, cost=c)

        def stt(out, in0, scalar, in1, op0, op1, r, w):
            P.add("dve", lambda: nc.vector.scalar_tensor_tensor(out=out, in0=in0, scalar=scalar, in1=in1, op0=op0, op1=op1), r=r, w=w,
                  cost=0.1 + fsz(out) * 0.00105)

        def cp(eng, out, in_, r, w):
            if eng == "act":
                P.add("act", lambda: nc.scalar.copy(out=out, in_=in_), r=r, w=w, cost=0.2 + fsz(out) * 0.00085)
            else:
                e = nc.vector if eng == "dve" else nc.gpsimd
                c = (0.1 + fsz(out) * 0.00105) if eng == "dve" else (0.2 + fsz(out) * 0.0021)
                P.add(eng, lambda: e.tensor_copy(out=out, in_=in_), r=r, w=w, cost=c)

        def memset(eng, ap, val, w):
            e = nc.vector if eng == "dve" else nc.gpsimd
            P.add(eng, lambda: e.memset(ap, val), w=w, cost=0.1 + fsz(ap) * 0.001)

        def dma(q, out, in_, r, w, sem, slow=False):
            e = {"sp": nc.sync, "pool": nc.gpsimd, "act": nc.scalar}[q]
            nb = int(np.prod(out.shape)) * (2 if out.dtype == BF16 else 4)
            if in_.dtype == F32 and out.dtype == BF16:
                nb *= 2
            if slow:
                def f():
                    with nc.allow_non_contiguous_dma(reason="small strided load"):
                        return e.dma_start(out=out, in_=in_)
                P.dma(q, f, r=r, w=w, sem=sem, nbytes=nb)
            else:
                P.dma(q, lambda: e.dma_start(out=out, in_=in_), r=r, w=w, sem=sem, nbytes=nb)

        def ln_stats(src, stats, mv, rstd, nmr, tag, rkeys):
            for h in range(2):
                P.add("dve", (lambda h=h: nc.vector.bn_stats(out=stats[:, h, :], in_=src[:, h * 512:(h + 1) * 512])),
                      r=rkeys, w=[(tag, "st", h)], cost=0.65)
            P.add("dve", lambda: nc.vector.bn_aggr(out=mv, in_=stats.rearrange("p a b -> p (a b)")),
                  r=[(tag, "st", 0), (tag, "st", 1)], w=[(tag, "mv")])
            act(rstd, mv[:, 1:2], AF.Ln, r=[(tag, "mv")], w=[(tag, "rstd")], bias=eps_t)
            act(rstd, rstd, AF.Exp, r=[(tag, "rstd")], w=[(tag, "rstd")], scale=-0.5)
            if nmr is not None:
                ts("dve", nmr, mv[:, 0:1], rstd, -1.0, ALU.mult, ALU.mult, r=[(tag, "mv"), (tag, "rstd")], w=[(tag, "nmr")])

        identF = sb([128], F32)
        identB = sb([128], BF16)
        Lincl = sb([128], F32)
        M1 = sb([128], F32)
        maskB = sb([128], BF16)
        onesM = sb([128], F32)
        ones_row = sb([128], BF16, parts=1)
        condT = sb([8], F32)
        cond_bc = sb([8, 128], F32)
        biasB = sb([NE], F32)
        eps_t = sb([1], F32)
        mhalf = sb([512], F32)
        one_t = sb([1], F32)

        memset("pool", identF, 1.0, w=["identF"])
        P.add("pool", lambda: nc.gpsimd.affine_select(out=identF, in_=identF, pattern=[[-1, 128]], compare_op=ALU.is_equal,
                                                      fill=0.0, base=0, channel_multiplier=1), r=["identF"], w=["identF"])
        cp("pool", identB, identF, r=["identF"], w=["identB"])
        memset("pool", Lincl, 1.0, w=["Lincl"])
        P.add("pool", lambda: nc.gpsimd.affine_select(out=Lincl, in_=Lincl, pattern=[[1, 128]], compare_op=ALU.is_ge,
                                                      fill=0.0, base=0, channel_multiplier=-1), r=["Lincl"], w=["Lincl"])
        memset("pool", Lincl[0:64, 64:128], 0.0, w=["Lincl"])
        cp("pool", maskB, Lincl, r=["Lincl"], w=["maskB"])
        memset("pool", M1, 1.0, w=["M1"])
        P.add("pool", lambda: nc.gpsimd.affine_select(out=M1, in_=M1, pattern=[[-1, 128]], compare_op=ALU.is_ge,
                                                      fill=0.0, base=-1, channel_multiplier=1), r=["M1"], w=["M1"])
        memset("pool", M1[64:128, 0:64], 0.0, w=["M1"])
        memset("pool", onesM, 1.0 / 512.0, w=["onesM"])
        memset("pool", ones_row, 1.0, w=["ones_row"])
        memset("pool", mhalf, -0.5, w=["mhalf"])
        memset("pool", eps_t, EPS, w=["eps_t"])
        memset("pool", one_t, 1.0, w=["one_t"])
        dma("sp", condT, c_d.rearrange("(k p) -> p k", p=128), r=[], w=["condT"], sem="condT", slow=True)
        act(condT, condT, AF.Silu, r=["condT"], w=["condT"])
        for k in range(8):
            ts("dve", cond_bc[:, k, :], onesM, condT[:, k:k + 1], 512.0, ALU.mult, ALU.mult, r=["onesM", "condT"], w=["cond_bc"])
        dma("sp", biasB, router_bias.partition_broadcast(128), r=[], w=["biasB"], sem="biasB")
        perm_mark = sb.mark()

        final_keys = []
        nlayers = 2
        for l in range(nlayers):
            sb.reset(perm_mark)
            xsrc = x_d if l == 0 else xmid
            xdst = xmid if l == 0 else out_d
            modT = sb([4, 8], F32)
            g1B = sb([D], F32)
            g2B = sb([D], F32)
            logits = sb([NT * NE], F32)
            comb = sb([NT * NE], F32)
            modP = sb([2, 8], F32)
            layer_mark = sb.mark()

            adaw = [sb([8, 512], F32) for _ in range(2)]
            adab = [sb([512], F32) for _ in range(2)]
            piece = [sb([512], F32) for _ in range(2)]
            kind_of = {0: 0, 1: 0, 2: 1, 3: 1, 6: 2, 7: 2, 8: 3, 9: 3}
            for j in range(12):
                s2 = j % 2
                dma("sp", adaw[s2], ada_w[l, :, j * 512:(j + 1) * 512].rearrange("(k p) n -> p k n", p=128),
                    r=[], w=[("adaw", s2)], sem=("adaw", s2))
                dma("sp", adab[s2], ada_b[l, j * 512:(j + 1) * 512].partition_broadcast(128), r=[], w=[("adab", s2)], sem=("adab", s2))
                for kc in range(8):
                    mm(bank(s2), cond_bc[:, kc, :], adaw[s2][:, kc, :], kc == 0, kc == 7,
                       r=["cond_bc", ("adaw", s2)], w=[bk(s2)])
                if j in (4, 5):
                    dst = g1B[:, (j - 4) * 512:(j - 3) * 512]
                    tt("dve", dst, bank(s2), adab[s2], ALU.add, r=[bk(s2), ("adab", s2)], w=["g1B"])
                elif j in (10, 11):
                    dst = g2B[:, (j - 10) * 512:(j - 9) * 512]
                    tt("dve", dst, bank(s2), adab[s2], ALU.add, r=[bk(s2), ("adab", s2)], w=["g2B"])
                else:
                    tt("dve", piece[s2], bank(s2), adab[s2], ALU.add, r=[bk(s2), ("adab", s2)], w=[("piece", s2)])
                    if j in (6, 7, 8, 9):
                        rr = 0 if j in (6, 7) else 1
                        dma("sp", modrow_d[rr:rr + 1, (j % 2) * 512:(j % 2 + 1) * 512], piece[s2][0:1, :], r=[("piece", s2)],
                            w=[("modrow", rr, j % 2)], sem=("modrow", s2))
                    for b in range(4):
                        tr(bank(2 + s2)[:, b * 128:(b + 1) * 128], piece[s2][:, b * 128:(b + 1) * 128], identF,
                           r=[("piece", s2), "identF"], w=[bk(2 + s2)])
                    kd = kind_of[j]
                    half = j % 2
                    dstm = modT[:, kd, half * 4:half * 4 + 4]
                    src = bank(2 + s2).rearrange("p (b c) -> p b c", c=128)[:, :, 0]
                    if kd in (1, 3):
                        ts("dve", dstm, src, 1.0, None, ALU.add, None, r=[bk(2 + s2)], w=["modT"])
                    else:
                        cp("dve", dstm, src, r=[bk(2 + s2)], w=["modT"])
            for rr in range(2):
                dma("sp", modP[:, rr, :], modrow_d[rr].rearrange("(p j) -> p j", j=8), r=[("modrow", rr, 0), ("modrow", rr, 1)],
                    w=["modP"], sem=("modP", rr))
            ts("dve", modP[:, 1, :], modP[:, 1, :], 1.0, None, ALU.add, None, r=["modP"], w=["modP"])
            wst = [sb([1536], F32) for _ in range(2)]
            wbf = [sb([1536], BF16) for _ in range(2)]
            pc = 0
            for kc in range(8):
                for (c0, c1) in ((0, 1536), (1536, 3072)):
                    q2 = pc % 2
                    pc += 1
                    dma("sp", wst[q2], w_in[l, kc * 128:(kc + 1) * 128, c0:c1], r=[], w=[("wst", q2)], sem=("wst", q2))
                    cp("dve" if q2 == 0 else "act", wbf[q2], wst[q2], r=[("wst", q2)], w=[("wbf", q2)])
                    dma("sp", winb_d[kc * 128:(kc + 1) * 128, c0:c1], wbf[q2], r=[("wbf", q2)], w=[("winb_d", kc)], sem=("wbfst", q2))
            for kc in range(8):
                q2 = pc % 2
                pc += 1
                dma("sp", wst[q2][:, 0:1024], w_out[l, kc * 128:(kc + 1) * 128, :], r=[], w=[("wst", q2)], sem=("wst", q2))
                cp("dve" if q2 == 0 else "act", wbf[q2][:, 0:1024], wst[q2][:, 0:1024], r=[("wst", q2)], w=[("wbf", q2)])
                dma("sp", woutb_d[kc * 128:(kc + 1) * 128, :], wbf[q2][:, 0:1024], r=[("wbf", q2)], w=[("woutb_d", kc)], sem=("wbfst", q2))
            if dbg and l == 0:
                dma("sp", modTd, modT.rearrange("p a b -> p (a b)"), r=["modT"], w=["modTd"], sem="modTd")
                final_keys.append("modTd")
            P.barrier()
            if stage == 0:
                break

            sb.reset(layer_mark)
            winc = sb([8, 1024], BF16)
            dg = sb([124, 128], BF16)
            b_inT = sb([12], F32)
            b_inH = sb([12], F32)
            cwT = sb([4, 31], F32)
            cbT = sb([4], F32)
            cgT = sb([4], F32)
            cbbT = sb([4], F32)
            abuf = sb([4, 30 + T], BF16)
            xt = [sb([D], F32) for _ in range(2)]
            xn = [sb([D], F32) for _ in range(2)]
            uT = [sb([8, 512], BF16) for _ in range(2)]
            sg = [sb([512], F32) for _ in range(2)]
            ac = [sb([4, 512], F32) for _ in range(2)]
            sq = [sb([512], F32) for _ in range(2)]
            t1 = [sb([512], F32) for _ in range(2)]
            meanS = [sb([512], F32) for _ in range(2)]
            varS = [sb([512], F32) for _ in range(2)]
            rstdB = [sb([512], F32) for _ in range(2)]
            hTc = [sb([4, 512], BF16) for _ in range(2)]
            stats = [sb([2, 6], F32) for _ in range(2)]
            mv = [sb([2], F32) for _ in range(2)]
            rstd = [sb([1], F32) for _ in range(2)]
            cw_tm = sg[0][0:31, :]
            yv = [sb([512], F32) for _ in range(2)]
            ncgT = sb([4], F32)
            ncbbT = sb([4], F32)
            print("A1 sbuf", sb.off)

            for kc in range(8):
                dma("sp", winc[:, kc, :], winb_d[kc * 128:(kc + 1) * 128, 2048:3072], r=[("winb_d", kc)], w=[("winc", kc)], sem=("winc", kc))
            dma("sp", b_inT[:, 0:4], b_in[l, 0:512].rearrange("(c p) -> p c", p=128), r=[], w=["b_inT"], sem="b_inT0", slow=True)
            dma("sp", b_inT[:, 4:12], b_in[l, 2048:3072].rearrange("(c p) -> p c", p=128), r=[], w=["b_inT"], sem="b_inT1", slow=True)
            ts("dve", b_inH, b_inT, -1.0, None, ALU.mult, None, r=["b_inT"], w=["b_inH"])
            dma("sp", cw_tm, conv_w[l], r=[], w=[("sg", 0)], sem="cw_tm")
            for ch in range(4):
                tr(bank(0)[:, ch * 32:ch * 32 + 31], cw_tm[0:31, ch * 128:(ch + 1) * 128], identF[0:31, 0:31],
                   r=[("sg", 0), "identF"], w=[bk(0)])
                cp("dve", cwT[:, ch, :], bank(0)[:, ch * 32:ch * 32 + 31], r=[bk(0)], w=["cwT"])
            for ch in range(4):
                for j in range(31):
                    ts("dve", dg[:, ch * 31 + j, :], identF, cwT[:, ch, j:j + 1], None, ALU.mult, None,
                       r=["identF", "cwT"], w=[("dg", ch)])
            dma("sp", cbT, conv_b[l].rearrange("(c p) -> p c", p=128), r=[], w=["cbT"], sem="cbT", slow=True)
            dma("sp", cgT, conv_g[l].rearrange("(c p) -> p c", p=128), r=[], w=["cgT"], sem="cgT", slow=True)
            dma("sp", cbbT, conv_bb[l].rearrange("(c p) -> p c", p=128), r=[], w=["cbbT"], sem="cbbT", slow=True)
            ts("dve", ncgT, cgT, -1.0, None, ALU.mult, None, r=["cgT"], w=["ncgT"])
            ts("dve", ncbbT, cbbT, -1.0, None, ALU.mult, None, r=["cbbT"], w=["ncbbT"])
            memset("pool", abuf[:, :, 0:30], 0.0, w=[("abuf", ch, -1) for ch in range(4)])

            for blk in range(NB):
                pb = blk % 2
                for t4 in range(4):
                    t = blk * 4 + t4
                    s2 = t % 2
                    dma("sp", xt[s2], xsrc[t * 128:(t + 1) * 128, :], r=[], w=[("xt", s2)], sem=("xt", s2))
                    ln_stats(xt[s2], stats[s2], mv[s2], rstd[s2], None, ("ln1", s2), [("xt", s2)])
                    ts("dve", xn[s2], xt[s2], mv[s2][:, 0:1], rstd[s2], ALU.subtract, ALU.mult,
                       r=[("xt", s2), (("ln1", s2), "mv"), (("ln1", s2), "rstd")], w=[("xn", s2)])
                    for kc in range(8):
                        bb = 2 * s2 + kc // 4
                        tr(bank(bb)[:, (kc % 4) * 128:(kc % 4 + 1) * 128], xn[s2][:, kc * 128:(kc + 1) * 128], identF,
                           r=[("xn", s2), "identF"], w=[bk(bb)])
                    for kc in range(8):
                        bb = 2 * s2 + kc // 4
                        act(uT[pb][:, kc, t4 * 128:(t4 + 1) * 128], bank(bb)[:, (kc % 4) * 128:(kc % 4 + 1) * 128], AF.Identity,
                            r=[bk(bb), "modT"], w=[("uT", pb, t4)], bias=modT[:, 0, kc:kc + 1], scale=modT[:, 1, kc:kc + 1])
                uTk = [("uT", pb, i) for i in range(4)]
                dma("sp", uTd[blk], uT[pb], r=uTk, w=[("uTd", blk)], sem=("uTst", pb))
                for ch in range(4):
                    c2 = ch % 2
                    for kc in range(8):
                        mm(bank(4), winc[:, kc, ch * 128:(ch + 1) * 128], uT[pb][:, kc, :], kc == 0, kc == 7,
                           r=uTk + [("winc", kc)], w=[bk(4)])
                    for kc in range(8):
                        mm(bank(5), winc[:, kc, 512 + ch * 128:512 + (ch + 1) * 128], uT[pb][:, kc, :], kc == 0, kc == 7,
                           r=uTk + [("winc", kc)], w=[bk(5)])
                    act(sg[c2], bank(5), AF.Exp, r=[bk(5), "b_inH"], w=[("sg", c2)], bias=b_inH[:, 8 + ch:9 + ch], scale=-1.0)
                    act(sg[c2], sg[c2], AF.Ln, r=[("sg", c2)], w=[("sg", c2)], bias=one_t)
                    act(sg[c2], sg[c2], AF.Exp, r=[("sg", c2)], w=[("sg", c2)], scale=-1.0)
                    stt(abuf[:, ch, 30 + blk * 512:30 + (blk + 1) * 512], bank(4), b_inT[:, 4 + ch:5 + ch], sg[c2], ALU.add, ALU.mult,
                        r=[bk(4), ("sg", c2), "b_inT"], w=[("abuf", ch, blk)])
                    for j in range(31):
                        mm(bank(6 + c2), dg[:, ch * 31 + j, :], abuf[:, ch, blk * 512 + j:blk * 512 + j + 512], j == 0, j == 30,
                           r=[("dg", ch), ("abuf", ch, blk), ("abuf", ch, blk - 1)], w=[bk(6 + c2)])
                    act(ac[pb][:, ch, :], bank(6 + c2), AF.Identity, r=[bk(6 + c2), "cbT"], w=[("ac", pb, ch)], bias=cbT[:, ch:ch + 1])
                    act(sq[c2], bank(6 + c2), AF.Square, r=[bk(6 + c2), "cbT"], w=[("sq", c2)], bias=cbT[:, ch:ch + 1])
                    mm(bank(2 * pb), onesM, ac[pb][:, ch, :], ch == 0, ch == 3, r=["onesM", ("ac", pb, ch)], w=[bk(2 * pb)])
                    mm(bank(2 * pb + 1), onesM, sq[c2], ch == 0, ch == 3, r=["onesM", ("sq", c2)], w=[bk(2 * pb + 1)])
                cp("act", meanS[pb], bank(2 * pb), r=[bk(2 * pb)], w=[("meanS", pb)])
                tt("dve", varS[pb], meanS[pb], meanS[pb], ALU.mult, r=[("meanS", pb)], w=[("varS", pb)])
                tt("dve", varS[pb], bank(2 * pb + 1), varS[pb], ALU.subtract, r=[bk(2 * pb + 1), ("varS", pb)], w=[("varS", pb)])
                act(rstdB[pb], varS[pb], AF.Ln, r=[("varS", pb)], w=[("rstdB", pb)], bias=eps_t)
                act(rstdB[pb], rstdB[pb], AF.Exp, r=[("rstdB", pb)], w=[("rstdB", pb)], scale=-0.5)
                for ch in range(4):
                    c2 = ch % 2
                    tt("dve", t1[c2], ac[pb][:, ch, :], meanS[pb], ALU.subtract, r=[("ac", pb, ch), ("meanS", pb)], w=[("t1", c2)])
                    tt("pool", t1[c2], t1[c2], rstdB[pb], ALU.mult, r=[("t1", c2), ("rstdB", pb)], w=[("t1", c2)])
                    act(yv[c2], t1[c2], AF.Identity, r=[("t1", c2), "cgT", "cbbT"], w=[("yv", c2)],
                        bias=cbbT[:, ch:ch + 1], scale=cgT[:, ch:ch + 1])
                    act(t1[c2], t1[c2], AF.Exp, r=[("t1", c2), "ncgT", "ncbbT"], w=[("t1", c2)],
                        bias=ncbbT[:, ch:ch + 1], scale=ncgT[:, ch:ch + 1])
                    act(t1[c2], t1[c2], AF.Ln, r=[("t1", c2)], w=[("t1", c2)], bias=one_t)
                    act(t1[c2], t1[c2], AF.Exp, r=[("t1", c2)], w=[("t1", c2)], scale=-1.0)
                    tt("dve", hTc[pb][:, ch, :], yv[c2], t1[c2], ALU.mult, r=[("yv", c2), ("t1", c2)], w=[("hTc", pb, ch)])
                dma("sp", hTd[blk, :, 4:8, :], hTc[pb], r=[("hTc", pb, ch) for ch in range(4)], w=[("hTd", blk, 1)], sem=("hTcst", pb))
            P.barrier()

            sb.reset(layer_mark)
            winh = sb([8, 2048], BF16)
            lbB = sb([512], F32)
            omlbB = sb([512], F32)
            nwB = sb([512], F32)
            b_inT = sb([4], F32)
            b_inH = sb([4], F32)
            omlbH = sb([512], F32)
            nwBH = sb([512], F32)
            brow = sb([1536], BF16, parts=1)
            uT = [sb([8, 512], BF16) for _ in range(2)]
            qT = [sb([4, 512], F32) for _ in range(2)]
            qb = [sb([512], F32) for _ in range(2)]
            zs = [sb([512], F32) for _ in range(2)]
            tmp = [sb([512], F32) for _ in range(2)]
            gt4 = [sb([4, 512], F32) for _ in range(2)]
            kk4 = [sb([4, 512], F32) for _ in range(2)]
            ec = [sb([512], F32) for _ in range(2)]
            sog = [sb([512], F32) for _ in range(2)]
            gate = [sb([512], F32) for _ in range(2)]
            sqo = [sb([512], F32) for _ in range(2)]
            khat = [sb([512], BF16) for _ in range(2)]
            vt = [sb([512], BF16) for _ in range(2)]
            eb = [sb([4, 128], F32) for _ in range(2)]
            enc = [sb([4, 128], F32) for _ in range(2)]
            qtA = [sb([4, 128], BF16) for _ in range(2)]
            qtB = [sb([4, 128], BF16) for _ in range(2)]
            qp = [sb([4, 128], BF16) for _ in range(2)]
            khT = [sb([4, 128], BF16) for _ in range(2)]
            AT = [sb([4, 128], BF16) for _ in range(2)]
            S = sb([4, 128], F32)
            Sb0 = [sb([4, 128], BF16) for _ in range(2)]
            Sb1 = [sb([4, 128], BF16) for _ in range(2)]
            ha = [sb([512], BF16) for _ in range(2)]
            hTa = [sb([4, 512], BF16) for _ in range(2)]
            ss = [sb([4], F32) for _ in range(2)]
            rs = [sb([4], F32) for _ in range(2)]
            lb2 = sb([2, 512], F32)
            print("A2 sbuf", sb.off)

            for kc in range(8):
                dma("sp", winh[:, kc, :], winb_d[kc * 128:(kc + 1) * 128, 0:2048], r=[("winb_d", kc)], w=[("winh", kc)], sem=("winh", kc))
            dma("sp", b_inT, b_in[l, 0:512].rearrange("(c p) -> p c", p=128), r=[], w=["b_inT"], sem="b_inTq", slow=True)
            ts("dve", b_inH, b_inT, -1.0, None, ALU.mult, None, r=["b_inT"], w=["b_inH"])
            dma("pool", brow, b_in[l:l + 1, 512:2048], r=[], w=["brow"], sem="brow")
            if l == 0:
                memset("pool", lbB, 0.0, w=["lbB"])
                memset("pool", omlbB, 1.0, w=["omlbB"])
            else:
                dma("sp", lb2.rearrange("p a b -> p (a b)"), hgrn_lb.rearrange("a b -> (a b)").partition_broadcast(128),
                    r=[], w=["lb2"], sem="lb2")
                act(lb2, lb2, AF.Exp, r=["lb2"], w=["lb2"])
                tt("dve", tmp[0], lb2[:, 0, :], lb2[:, 1, :], ALU.add, r=["lb2"], w=[("tmp", 0)])
                P.add("dve", lambda: nc.vector.reciprocal(out=tmp[0], in_=tmp[0]), r=[("tmp", 0)], w=[("tmp", 0)], cost=4.2)
                tt("dve", lbB, lb2[:, 1, :], tmp[0], ALU.mult, r=["lb2", ("tmp", 0)], w=["lbB"])
                tt("dve", omlbB, lb2[:, 0, :], tmp[0], ALU.mult, r=["lb2", ("tmp", 0)], w=["omlbB"])
            for h in range(4):
                dma("sp", nwB[:, h * 128:(h + 1) * 128], hgrn_nw[l].partition_broadcast(128), r=[], w=["nwB"], sem=("nwB", h))
            ts("dve", omlbH, omlbB, 0.5, None, ALU.mult, None, r=["omlbB"], w=["omlbH"])
            ts("dve", nwBH, nwB, 0.5, None, ALU.mult, None, r=["nwB"], w=["nwBH"])
            memset("pool", S, 0.0, w=["S"])
            memset("pool", Sb0[0], 0.0, w=[("Sb0", 0)])
            for p in range(2):
                memset("pool", qtA[p], 0.0, w=[("qtA", p)])
                memset("pool", qtB[p], 0.0, w=[("qtB", p)])

            for blk in range(NB):
                pb = blk % 2
                dma("sp", uT[pb], uTd[blk], r=[("uTd", blk)], w=[("uT", pb)], sem=("uTld", pb))
                for h in range(4):
                    b_ = 3 + 4 * (h % 2)
                    for kc in range(8):
                        mm(bank(b_), winh[:, kc, h * 128:(h + 1) * 128], uT[pb][:, kc, :], kc == 0, kc == 7,
                           r=[("uT", pb), ("winh", kc)], w=[bk(b_)])
                    h2 = h % 2
                    act(zs[h2], bank(b_), AF.Exp, r=[bk(b_), "b_inH"], w=[("zs", h2)], bias=b_inH[:, h:h + 1], scale=-1.0)
                    act(qb[h2], bank(b_), AF.Identity, r=[bk(b_), "b_inT"], w=[("qb", h2)], bias=b_inT[:, h:h + 1])
                    act(zs[h2], zs[h2], AF.Ln, r=[("zs", h2)], w=[("zs", h2)], bias=one_t)
                    act(zs[h2], zs[h2], AF.Exp, r=[("zs", h2)], w=[("zs", h2)], scale=-1.0)
                    tt("dve", qT[pb][:, h, :], zs[h2], qb[h2], ALU.mult, r=[("zs", h2), ("qb", h2)], w=[("qT", pb, h)])
                qk = [("qT", pb, h) for h in range(4)]
                for t4 in range(4):
                    p = (blk * 4 + t4) % 2
                    fb = 4 * p + 2
                    tsl = slice(t4 * 128, (t4 + 1) * 128)
                    for kc in range(8):
                        mm(bank(fb), uT[pb][:, kc, tsl], winh[:, kc, 512:1024], kc == 0, False,
                           r=[("uT", pb), ("winh", kc)], w=[bk(fb)])
                    mm(bank(fb), ones_row, brow[:, 0:512], False, True, r=["ones_row", "brow"], w=[bk(fb)])
                    act(zs[p], bank(fb), AF.Exp, r=[bk(fb)], w=[("zs", p)], scale=-1.0)
                    act(zs[p], zs[p], AF.Ln, r=[("zs", p)], w=[("zs", p)], bias=one_t)
                    act(zs[p], zs[p], AF.Exp, r=[("zs", p)], w=[("zs", p)], scale=-1.0)
                    tt("dve", tmp[p], zs[p], omlbB, ALU.mult, r=[("zs", p), "omlbB"], w=[("tmp", p)])
                    tt("pool", kk4[pb][:, t4, :], omlbB, tmp[p], ALU.subtract, r=["omlbB", ("tmp", p)], w=[("kk4", pb, t4)])
                    tt("dve", gt4[pb][:, t4, :], tmp[p], lbB, ALU.add, r=[("tmp", p), "lbB"], w=[("gt4", pb, t4)])
                g4k = [("gt4", pb, i) for i in range(4)]
                act(gt4[pb], gt4[pb], AF.Ln, r=g4k, w=g4k)
                for t4 in range(4):
                    t = blk * 4 + t4
                    p = t % 2
                    pn = 1 - p
                    B0 = 4 * p
                    tsl = slice(t4 * 128, (t4 + 1) * 128)
                    gt = [gt4[pb][:, t4, :]] * 2
                    kk = [kk4[pb][:, t4, :]] * 2
                    for pi, (pbk, col0) in ((1, (B0 + 1, 1024)), (2, (B0 + 2, 1536))):
                        for kc in range(8):
                            mm(bank(pbk), uT[pb][:, kc, tsl], winh[:, kc, col0:col0 + 512], kc == 0, False,
                               r=[("uT", pb), ("winh", kc)], w=[bk(pbk)])
                        mm(bank(pbk), ones_row, brow[:, pi * 512:(pi + 1) * 512], False, True, r=["ones_row", "brow"], w=[bk(pbk)])
                    cp("dve", vt[p], bank(B0 + 1), r=[bk(B0 + 1)], w=[("vt", p)])
                    act(sog[p], bank(B0 + 2), AF.Exp, r=[bk(B0 + 2)], w=[("sog", p)], scale=-1.0)
                    act(sog[p], sog[p], AF.Ln, r=[("sog", p)], w=[("sog", p)], bias=one_t)
                    act(sog[p], sog[p], AF.Exp, r=[("sog", p)], w=[("sog", p)], scale=-1.0)
                    tt("dve", sog[p], sog[p], bank(B0 + 2), ALU.mult, r=[("sog", p), bk(B0 + 2)], w=[("sog", p)])
                    tt("pool", gate[p], sog[p], nwB, ALU.mult, r=[("sog", p), "nwB"], w=[("gate", p)])
                    mm(bank(B0 + 3), M1, gt[p], True, True, r=["M1", ("gt4", pb, t4)], w=[bk(B0 + 3)])
                    for h in range(4):
                        mm(bank(B0 + 0)[:, h * 128:(h + 1) * 128], gt[p][:, h * 128:(h + 1) * 128], Lincl, True, True,
                           r=[("gt4", pb, t4), "Lincl"], w=[bk(B0 + 0)])
                    for h in range(4):
                        mm(bank(B0 + 1)[:, h * 128:(h + 1) * 128], gt[p][:, h * 128:(h + 1) * 128], M1, True, True,
                           r=[("gt4", pb, t4), "M1"], w=[bk(B0 + 1)])
                    act(ec[p], bank(B0 + 3), AF.Exp, r=[bk(B0 + 3)], w=[("ec", p)])
                    tt("dve", khat[p], kk[p], ec[p], ALU.mult, r=[("kk4", pb, t4), ("ec", p)], w=[("khat", p)])
                    act(eb[p].rearrange("p a b -> p (a b)"), bank(B0 + 0), AF.Exp, r=[bk(B0 + 0)], w=[("eb", p)])
                    ts("dve", enc[p].rearrange("p a b -> p (a b)"), bank(B0 + 1), -1.0, 75.0, ALU.mult, ALU.min, r=[bk(B0 + 1)], w=[("enc", p)])
                    act(enc[p], enc[p], AF.Exp, r=[("enc", p)], w=[("enc", p)])
                    tt("dve", qp[p], qT[pb][:, :, tsl], enc[p], ALU.mult, r=qk + [("enc", p)], w=[("qp", p)])
                    tt("pool", qtA[p][:, :, 0:64], qT[pb][:, :, t4 * 128:t4 * 128 + 64], eb[p][:, :, 0:64], ALU.mult,
                       r=qk + [("eb", p)], w=[("qtA", p)])
                    tt("pool", qtB[p][:, :, 64:128], qT[pb][:, :, t4 * 128 + 64:t4 * 128 + 128], eb[p][:, :, 64:128], ALU.mult,
                       r=qk + [("eb", p)], w=[("qtB", p)])
                    for h in range(4):
                        tr(bankb(B0 + 2)[:, h * 128:(h + 1) * 128], khat[p][:, h * 128:(h + 1) * 128], identB,
                           r=[("khat", p), "identB"], w=[bk(B0 + 2)])
                    cp("act", khT[p].rearrange("p a b -> p (a b)"), bankb(B0 + 2)[:, 0:512], r=[bk(B0 + 2)], w=[("khT", p)])
                    for h in range(4):
                        mm(bank(B0 + 3)[:, h * 128:(h + 1) * 128], khT[p][:, h, :], qp[p][:, h, :], True, True,
                           r=[("khT", p), ("qp", p)], w=[bk(B0 + 3)])
                    tt("dve", AT[p], bank(B0 + 3).rearrange("p (a b) -> p a b", a=4), maskB.unsqueeze(1).to_broadcast([128, 4, 128]), ALU.mult,
                       r=[bk(B0 + 3), "maskB"], w=[("AT", p)])
                    for h in range(4):
                        mm(bank(B0 + 0)[:, h * 128:(h + 1) * 128], khat[p][0:64, h * 128:(h + 1) * 128], vt[p][0:64, h * 128:(h + 1) * 128],
                           True, True, r=[("khat", p), ("vt", p)], w=[bk(B0 + 0)])
                    for h in range(4):
                        stt(S[:, h, :], S[:, h, :], eb[p][:, h, 63:64], bank(B0 + 0)[:, h * 128:(h + 1) * 128], ALU.mult, ALU.add,
                            r=["S", ("eb", p), bk(B0 + 0)], w=["S"])
                    cp("pool", Sb1[p], S, r=["S"], w=[("Sb1", p)])
                    for h in range(4):
                        hs = slice(h * 128, (h + 1) * 128)
                        mm(bank(B0 + 1)[:, hs], AT[p][:, h, :], vt[p][:, hs], True, False, r=[("AT", p), ("vt", p)], w=[bk(B0 + 1)])
                        mm(bank(B0 + 1)[:, hs], qtA[p][:, h, :], Sb0[p][:, h, :], False, False, r=[("qtA", p), ("Sb0", p)], w=[bk(B0 + 1)])
                        mm(bank(B0 + 1)[:, hs], qtB[p][:, h, :], Sb1[p][:, h, :], False, True, r=[("qtB", p), ("Sb1", p)], w=[bk(B0 + 1)])
                    for h in range(4):
                        mm(bank(B0 + 2)[:, h * 128:(h + 1) * 128], khat[p][64:128, h * 128:(h + 1) * 128], vt[p][64:128, h * 128:(h + 1) * 128],
                           True, True, r=[("khat", p), ("vt", p)], w=[bk(B0 + 2)])
                    for h in range(4):
                        stt(S[:, h, :], S[:, h, :], eb[p][:, h, 127:128], bank(B0 + 2)[:, h * 128:(h + 1) * 128], ALU.mult, ALU.add,
                            r=["S", ("eb", p), bk(B0 + 2)], w=["S"])
                    cp("pool", Sb0[pn], S, r=["S"], w=[("Sb0", pn)])
                    act(sqo[p], bank(B0 + 1), AF.Square, r=[bk(B0 + 1)], w=[("sqo", p)])
                    P.add("dve", (lambda p=p: nc.vector.tensor_reduce(out=ss[p], in_=sqo[p].rearrange("p (a b) -> p a b", a=4), axis=AX.X, op=ALU.add)),
                          r=[("sqo", p)], w=[("ss", p)], cost=0.65)
                    act(rs[p], ss[p], AF.Ln, r=[("ss", p)], w=[("rs", p)], bias=eps_t, scale=1.0 / 128.0)
                    act(rs[p], rs[p], AF.Exp, r=[("rs", p)], w=[("rs", p)], scale=-0.5)
                    for h in range(4):
                        hs = slice(h * 128, (h + 1) * 128)
                        stt(ha[p][:, hs], bank(B0 + 1)[:, hs], rs[p][:, h:h + 1], gate[p][:, hs], ALU.mult, ALU.mult,
                            r=[bk(B0 + 1), ("rs", p), ("gate", p)], w=[("ha", p)])
                    for h in range(4):
                        tr(bankb(B0 + 3)[:, h * 128:(h + 1) * 128], ha[p][:, h * 128:(h + 1) * 128], identB, r=[("ha", p), "identB"], w=[bk(B0 + 3)])
                    for h in range(4):
                        cp("act", hTa[pb][:, h, tsl], bankb(B0 + 3)[:, h * 128:(h + 1) * 128], r=[bk(B0 + 3)], w=[("hTa", pb, t4)])
                dma("sp", hTd[blk, :, 0:4, :], hTa[pb], r=[("hTa", pb, i) for i in range(4)], w=[("hTd", blk, 0)], sem=("hTast", pb))
            P.barrier()
            if stage == 1:
                final_keys.append(("hTd", NB - 1, 0))
                break

            sb.reset(layer_mark)
            wout = sb([8, D], BF16)
            brow_o = sb([D], BF16, parts=1)
            ln1gB = sb([D], F32)
            ln1bB = sb([D], F32)
            rw = sb([8, NE], F32)
            hTb = [sb([8, 512], BF16) for _ in range(2)]
            xtb = [sb([D], F32) for _ in range(2)]
            zt = [sb([D], F32) for _ in range(2)]
            x1 = [sb([D], F32) for _ in range(2)]
            xn2 = [sb([D], F32) for _ in range(2)]
            u2T = [sb([8, 128], F32) for _ in range(2)]
            xn2b = [sb([D], BF16) for _ in range(2)]
            statsb = [sb([2, 6], F32) for _ in range(2)]
            mvb = [sb([2], F32) for _ in range(2)]
            rstdb = [sb([1], F32) for _ in range(2)]
            nmrb = [sb([1], F32) for _ in range(2)]
            stats2 = [sb([2, 6], F32) for _ in range(2)]
            mv2 = [sb([2], F32) for _ in range(2)]
            rstd2 = [sb([1], F32) for _ in range(2)]
            nmr2 = [sb([1], F32) for _ in range(2)]
            for kc in range(8):
                dma("sp", wout[:, kc, :], woutb_d[kc * 128:(kc + 1) * 128, :], r=[("woutb_d", kc)], w=["wout"], sem=("wout", kc))
            dma("pool", brow_o, b_out[l:l + 1, :], r=[], w=["brow_o"], sem="brow_o")
            dma("sp", ln1gB, ln1_g[l].partition_broadcast(128), r=[], w=["ln1gB"], sem="ln1gB")
            dma("sp", ln1bB, ln1_b[l].partition_broadcast(128), r=[], w=["ln1bB"], sem="ln1bB")
            dma("sp", rw, router_w.rearrange("(k p) e -> p k e", p=128), r=[], w=["rw"], sem="rw", slow=True)
            for blk in range(NB):
                hb = blk % 2
                dma("sp", hTb[hb], hTd[blk], r=[("hTd", blk, 0), ("hTd", blk, 1)], w=[("hTb", hb)], sem=("hTb", hb))
                for t4 in range(4):
                    t = blk * 4 + t4
                    s2 = t % 2
                    Y0 = 2 * s2
                    X0 = 4 + 2 * s2
                    tsl = slice(t4 * 128, (t4 + 1) * 128)
                    dma("sp", xtb[s2], xsrc[t * 128:(t + 1) * 128, :], r=[], w=[("xtb", s2)], sem=("xtb", s2))
                    for hf in range(2):
                        for cc in range(8):
                            mm(bank(Y0 + hf), hTb[hb][:, cc, tsl], wout[:, cc, hf * 512:(hf + 1) * 512], cc == 0, False,
                               r=[("hTb", hb), "wout"], w=[bk(Y0 + hf)])
                        mm(bank(Y0 + hf), ones_row, brow_o[:, hf * 512:(hf + 1) * 512], False, True, r=["ones_row", "brow_o"], w=[bk(Y0 + hf)])
                    tt("dve", zt[s2], ps[:, Y0:Y0 + 2, :].rearrange("p a b -> p (a b)"), g1B, ALU.mult, r=[bk(Y0), bk(Y0 + 1), "g1B"], w=[("zt", s2)])
                    stt(zt[s2], xtb[s2], ALPHA, zt[s2], ALU.mult, ALU.add, r=[("xtb", s2), ("zt", s2)], w=[("zt", s2)])
                    ln_stats(zt[s2], statsb[s2], mvb[s2], rstdb[s2], nmrb[s2], ("pl1", s2), [("zt", s2)])
                    act(x1[s2], zt[s2], AF.Identity, r=[("zt", s2), (("pl1", s2), "rstd"), (("pl1", s2), "nmr")], w=[("x1", s2)],
                        bias=nmrb[s2], scale=rstdb[s2])
                    tt("dve", x1[s2], x1[s2], ln1gB, ALU.mult, r=[("x1", s2), "ln1gB"], w=[("x1", s2)])
                    tt("pool", x1[s2], x1[s2], ln1bB, ALU.add, r=[("x1", s2), "ln1bB"], w=[("x1", s2)])
                    dma("sp", x1d[t * 128:(t + 1) * 128, :], x1[s2], r=[("x1", s2)], w=[("x1d", t)], sem=("x1st", s2))
                    ln_stats(x1[s2], stats2[s2], mv2[s2], rstd2[s2], nmr2[s2], ("ln2", s2), [("x1", s2)])
                    act(xn2[s2], x1[s2], AF.Identity, r=[("x1", s2), (("ln2", s2), "rstd"), (("ln2", s2), "nmr")], w=[("xn2", s2)],
                        bias=nmr2[s2], scale=rstd2[s2])
                    for kc in range(8):
                        tr(bank(X0 + kc // 4)[:, (kc % 4) * 128:(kc % 4 + 1) * 128], xn2[s2][:, kc * 128:(kc + 1) * 128], identF,
                           r=[("xn2", s2), "identF"], w=[bk(X0 + kc // 4)])
                    for kc in range(8):
                        act(u2T[s2][:, kc, :], bank(X0 + kc // 4)[:, (kc % 4) * 128:(kc % 4 + 1) * 128], AF.Identity,
                            r=[bk(X0 + kc // 4), "modT"], w=[("u2T", s2)], bias=modT[:, 2, kc:kc + 1], scale=modT[:, 3, kc:kc + 1])
                    for kc in range(8):
                        mm(bank(X0)[:, 0:NE], u2T[s2][:, kc, :], rw[:, kc, :], kc == 0, kc == 7, r=[("u2T", s2), "rw"], w=[bk(X0)])
                    cp("dve", logits[:, t * NE:(t + 1) * NE], bank(X0)[:, 0:NE], r=[bk(X0)], w=["logits"])
                    cp("pool", xn2b[s2], xn2[s2], r=[("xn2", s2)], w=[("xn2b", s2)])
                    dma("sp", xn2d[t * 128:(t + 1) * 128, :], xn2b[s2], r=[("xn2b", s2)], w=[("xn2d", t)], sem=("xn2st", s2))
            if dbg:
                dma("sp", logd, logits, r=["logits"], w=["logd"], sem="logd")
                final_keys.append("logd")
            P.barrier()
            if stage == 2:
                final_keys += [("x1d", NT - 1), ("xn2d", NT - 1)]
                break


            sb.reset(layer_mark)
            slAi = sb([NT], mybir.dt.int32)
            slBi = sb([NT], mybir.dt.int32)
            cwA = sb([NT], F32)
            cwB = sb([NT], F32)
            widx = sb([NT], mybir.dt.int32)
            moe_mark = sb.mark()
            NG = NT * 4
            s_t = sb([NT * NE], F32)
            sbv = sb([NT * NE], F32)
            m1 = sb([NG], F32)
            is1 = sb([NT * NE], F32)
            G2 = sb([NT * NE], F32)
            m2 = sb([NG], F32)
            gs = sb([NG], F32)
            gmax = sb([NT], F32)
            gsel = sb([NG], F32)
            top2 = sb([NT * NE], F32)
            den = sb([NT], F32)
            g3 = lambda a: a.rearrange("p (g e) -> p g e", e=4)
            t3 = lambda a: a.rearrange("p (t e) -> p t e", t=NT)
            act(s_t, logits, AF.Sigmoid, r=["logits"], w=["s_t"])
            tt("dve", t3(sbv), t3(s_t), biasB.unsqueeze(1).to_broadcast([128, NT, NE]), ALU.add, r=["s_t", "biasB"], w=["sbv"])
            P.add("dve", lambda: nc.vector.tensor_reduce(out=m1, in_=g3(sbv), axis=AX.X, op=ALU.max), r=["sbv"], w=["m1"])
            tt("dve", g3(is1), g3(sbv), m1.unsqueeze(2).to_broadcast([128, NG, 4]), ALU.is_ge, r=["sbv", "m1"], w=["is1"])
            stt(G2, is1, -1.0e9, sbv, ALU.mult, ALU.add, r=["is1", "sbv"], w=["G2"])
            P.add("dve", lambda: nc.vector.tensor_reduce(out=m2, in_=g3(G2), axis=AX.X, op=ALU.max), r=["G2"], w=["m2"])
            tt("dve", gs, m1, m2, ALU.add, r=["m1", "m2"], w=["gs"])
            P.add("dve", lambda: nc.vector.tensor_reduce(out=gmax, in_=gs.rearrange("p (t g) -> p t g", g=4), axis=AX.X, op=ALU.max),
                  r=["gs"], w=["gmax"])
            tt("dve", gsel.rearrange("p (t g) -> p t g", g=4), gs.rearrange("p (t g) -> p t g", g=4),
               gmax.unsqueeze(2).to_broadcast([128, NT, 4]), ALU.is_ge, r=["gs", "gmax"], w=["gsel"])
            tt("dve", g3(top2), g3(sbv), m2.unsqueeze(2).to_broadcast([128, NG, 4]), ALU.is_ge, r=["sbv", "m2"], w=["top2"])
            tt("dve", g3(top2), g3(top2), gsel.unsqueeze(2).to_broadcast([128, NG, 4]), ALU.mult, r=["top2", "gsel"], w=["top2"])
            tt("dve", top2, top2, s_t, ALU.mult, r=["top2", "s_t"], w=["top2"])
            P.add("dve", lambda: nc.vector.tensor_reduce(out=den, in_=t3(top2), axis=AX.X, op=ALU.add), r=["top2"], w=["den"])
            P.add("dve", lambda: nc.vector.reciprocal(out=den, in_=den), r=["den"], w=["den"])
            tt("dve", t3(comb), t3(top2), den.unsqueeze(2).to_broadcast([128, NT, NE]), ALU.mult, r=["top2", "den"], w=["comb"])
            selb = sb([NT * NE], BF16)
            rank = sb([NT * NE], F32)
            totA = sb([NT * NE], F32)
            totB = sb([NT * NE], F32)
            cnt = sb([NE], F32)
            nst = sb([NE], F32)
            cA = sb([NE], F32)
            cB = sb([NE], F32)
            sstart = sb([NE], F32)
            slot = sb([NT * NE], F32)
            mm_ = sb([NT * NE], F32)
            m2_ = sb([NT * NE], F32)
            slA = sb([NT], F32)
            slB = sb([NT], F32)
            eqm = sb([NT * NE], F32)
            iot = sb([NT], F32)
            ioti = sb([NT], mybir.dt.int32)
            pidx = sb([1], F32)
            pidxi = sb([1], mybir.dt.int32)
            cmp3 = sb([NT * NE], F32)
            eidf = sb([NT], F32)
            SUb = sb([128], BF16)
            onesb = sb([128], BF16)
            SUf = sb([128], F32)
            xg = [sb([D], BF16) for _ in range(2)]
            ts("dve", selb, comb, 0.0, None, ALU.is_gt, None, r=["comb"], w=["selb"])
            memset("pool", SUf, 1.0, w=["SUf"])
            P.add("pool", lambda: nc.gpsimd.affine_select(out=SUf, in_=SUf, pattern=[[1, 128]], compare_op=ALU.is_ge,
                                                          fill=0.0, base=-1, channel_multiplier=-1), r=["SUf"], w=["SUf"])
            cp("pool", SUb, SUf, r=["SUf"], w=["SUb"])
            memset("pool", onesb, 1.0, w=["onesb"])
            mm(bank(0), SUb, selb, True, True, r=["SUb", "selb"], w=[bk(0)])
            mm(bank(1), onesb, selb, True, True, r=["onesb", "selb"], w=[bk(1)])
            cp("dve", rank, bank(0), r=[bk(0)], w=["rank"])
            cp("act", totA, bank(1), r=[bk(1)], w=["totA"])
            src_, dst_ = totA, totB
            sk, dk_ = "totA", "totB"
            dd = 1
            while dd < NT:
                cp("pool", t3(dst_)[:, 0:dd, :], t3(src_)[:, 0:dd, :], r=[sk], w=[dk_])
                tt("dve", t3(dst_)[:, dd:NT, :], t3(src_)[:, dd:NT, :], t3(src_)[:, 0:NT - dd, :], ALU.add, r=[sk], w=[dk_])
                src_, dst_ = dst_, src_
                sk, dk_ = dk_, sk
                dd *= 2
            incl, inclk = src_, sk
            other, otherk = dst_, dk_
            cp("dve", cnt, t3(incl)[:, NT - 1, :], r=[inclk], w=["cnt"])
            tt("dve", rank, rank, incl, ALU.add, r=["rank", inclk], w=["rank"])
            cp("act", other, bank(1), r=[bk(1), otherk], w=[otherk])
            tt("dve", rank, rank, other, ALU.subtract, r=["rank", otherk], w=["rank"])
            memset("pool", nst, 0.0, w=["nst"])
            for jj in range(8):
                stt(nst, cnt, float(512 * jj), nst, ALU.is_gt, ALU.add, r=["cnt", "nst"], w=["nst"])
            src_, dst_, sk, dk_ = nst, cA, "nst", "cA"
            first = True
            dd = 1
            cp("dve", cB, nst, r=["nst"], w=["cB"])
            src_, sk = cB, "cB"
            dst_, dk_ = cA, "cA"
            while dd < NE:
                cp("pool", dst_[:, 0:dd], src_[:, 0:dd], r=[sk], w=[dk_])
                tt("dve", dst_[:, dd:NE], src_[:, dd:NE], src_[:, 0:NE - dd], ALU.add, r=[sk], w=[dk_])
                src_, dst_ = dst_, src_
                sk, dk_ = dk_, sk
                dd *= 2
            send, sendk = src_, sk
            tt("dve", sstart, send, nst, ALU.subtract, r=[sendk, "nst"], w=["sstart"])
            ts("dve", sstart, sstart, 512.0, 1.0, ALU.mult, ALU.add, r=["sstart"], w=["sstart"])
            tt("dve", t3(slot), t3(rank), sstart.unsqueeze(1).to_broadcast([128, NT, NE]), ALU.add, r=["rank", "sstart"], w=["slot"])
            tt("dve", mm_, slot, selb, ALU.mult, r=["slot", "selb"], w=["mm_"])
            P.add("dve", lambda: nc.vector.tensor_reduce(out=slB, in_=t3(mm_), axis=AX.X, op=ALU.max), r=["mm_"], w=["slB"])
            ts("dve", m2_, selb, -1.0e7, 1.0e7, ALU.mult, ALU.add, r=["selb"], w=["m2_"])
            tt("dve", m2_, m2_, mm_, ALU.add, r=["m2_", "mm_"], w=["m2_"])
            P.add("dve", lambda: nc.vector.tensor_reduce(out=slA, in_=t3(m2_), axis=AX.X, op=ALU.min), r=["m2_"], w=["slA"])
            tt("dve", t3(eqm), t3(mm_), slA.unsqueeze(2).to_broadcast([128, NT, NE]), ALU.is_equal, r=["mm_", "slA"], w=["eqm"])
            tt("dve", eqm, eqm, comb, ALU.mult, r=["eqm", "comb"], w=["eqm"])
            P.add("dve", lambda: nc.vector.tensor_reduce(out=cwA, in_=t3(eqm), axis=AX.X, op=ALU.add), r=["eqm"], w=["cwA"])
            tt("dve", t3(eqm), t3(mm_), slB.unsqueeze(2).to_broadcast([128, NT, NE]), ALU.is_equal, r=["mm_", "slB"], w=["eqm"])
            tt("dve", eqm, eqm, comb, ALU.mult, r=["eqm", "comb"], w=["eqm"])
            P.add("dve", lambda: nc.vector.tensor_reduce(out=cwB, in_=t3(eqm), axis=AX.X, op=ALU.add), r=["eqm"], w=["cwB"])
            ts("dve", slA, slA, -1.0, None, ALU.add, None, r=["slA"], w=["slA"])
            ts("dve", slB, slB, -1.0, None, ALU.add, None, r=["slB"], w=["slB"])
            cp("dve", slAi, slA, r=["slA"], w=["slAi"])
            cp("dve", slBi, slB, r=["slB"], w=["slBi"])
            P.add("pool", lambda: nc.gpsimd.iota(ioti, pattern=[[1, NT]], base=0, channel_multiplier=0), w=["ioti"], cost=0.3)
            cp("dve", iot, ioti, r=["ioti"], w=["iot"])
            P.add("pool", lambda: nc.gpsimd.iota(pidxi, pattern=[[1, 1]], base=0, channel_multiplier=1), w=["pidxi"], cost=0.3)
            cp("dve", pidx, pidxi, r=["pidxi"], w=["pidx"])
            tt("dve", t3(cmp3), send.unsqueeze(1).to_broadcast([128, NT, NE]), iot.unsqueeze(2).to_broadcast([128, NT, NE]), ALU.is_le,
               r=[sendk, "iot"], w=["cmp3"])
            P.add("dve", lambda: nc.vector.tensor_reduce(out=eidf, in_=t3(cmp3), axis=AX.X, op=ALU.add), r=["cmp3"], w=["eidf"])
            ts("dve", eidf, eidf, 15.0, 128.0, ALU.min, ALU.mult, r=["eidf"], w=["eidf"])
            ts("dve", eidf, eidf, pidx, float(l * NE * 128), ALU.add, ALU.add, r=["eidf", "pidx"], w=["eidf"])
            cp("dve", widx, eidf, r=["eidf"], w=["widx"])
            if dbg:
                dma("sp", combd, comb, r=["comb"], w=["combd"], sem="combd")
                dma("sp", slotd[:, 0:NT], slA, r=["slA"], w=["slotd0"], sem="slotd0")
                dma("sp", slotd[:, NT:2 * NT], slB, r=["slB"], w=["slotd1"], sem="slotd1")
                dma("sp", slotd[:, 2 * NT:3 * NT], cwA, r=["cwA"], w=["slotd2"], sem="slotd2")
                dma("sp", slotd[:, 3 * NT:4 * NT], eidf, r=["eidf"], w=["slotd3"], sem="slotd3")
                final_keys += ["combd", "slotd0", "slotd1", "slotd2", "slotd3"]
            for t in range(NT):
                s2 = t % 2
                dma("sp", xg[s2], xn2d[t * 128:(t + 1) * 128, :], r=[("xn2d", t)], w=[("xg", s2)], sem=("xg", s2))
                for which, idxt, ik in ((0, slAi, "slAi"), (1, slBi, "slBi")):
                    def sc(idxt=idxt, t=t, s2=s2):
                        return nc.gpsimd.indirect_dma_start(out=Ud[:, :], out_offset=bass.IndirectOffsetOnAxis(ap=idxt[:, t:t + 1], axis=0),
                                                            in_=xg[s2][:, :], in_offset=None)
                    P.dma("pool", sc, r=[("xg", s2), ik], w=[("Ud", t, which)], sem=("usc", s2, which), nbytes=262144)
            P.barrier()
            if stage == 3:
                break

            sb.reset(moe_mark)
            NSTEP = 32
            wgs = sb([8, 512], F32)
            wus = sb([8, 512], F32)
            wds = sb([4, D], F32)
            wg = [sb([8, 512], BF16) for _ in range(2)]
            wu = [sb([8, 512], BF16) for _ in range(2)]
            wd = [sb([4, D], BF16) for _ in range(2)]
            ugm = [sb([D], BF16) for _ in range(4)]
            ugT = [sb([8, 512], BF16) for _ in range(2)]
            sgt = [sb([512], F32) for _ in range(2)]
            hTm = [sb([4, 512], BF16) for _ in range(2)]
            ysb = [sb([D], F32) for _ in range(2)]
            wgl = w_gate.rearrange("l e (p j) f -> (l e p) (j f)", j=8)
            wul = w_up.rearrange("l e (p j) f -> (l e p) (j f)", j=8)
            wdl = w_down.rearrange("l e (p j) n -> (l e p) (j n)", j=4)
            ycnt = 0
            for i in range(NSTEP):
                ws = i % 2
                for (stg, srcw, key, nb_) in ((wgs, wgl, "wgs", 2097152), (wus, wul, "wus", 2097152), (wds, wdl, "wds", 2097152)):
                    def gw(stg=stg, srcw=srcw, i=i):
                        return nc.gpsimd.indirect_dma_start(out=stg.rearrange("p a b -> p (a b)"), out_offset=None, in_=srcw,
                                                            in_offset=bass.IndirectOffsetOnAxis(ap=widx[:, i:i + 1], axis=0))
                    P.dma("pool", gw, r=["widx"], w=[key], sem=key, nbytes=nb_)
                cp("dve", wg[ws][:, 0:4, :], wgs[:, 0:4, :], r=["wgs"], w=[("wg", ws)])
                cp("act", wg[ws][:, 4:8, :], wgs[:, 4:8, :], r=["wgs"], w=[("wg", ws)])
                cp("pool", wu[ws][:, 0:3, :], wus[:, 0:3, :], r=["wus"], w=[("wu", ws)])
                cp("act", wu[ws][:, 3:8, :], wus[:, 3:8, :], r=["wus"], w=[("wu", ws)])
                cp("dve", wd[ws][:, 0:2, :], wds[:, 0:2, :], r=["wds"], w=[("wd", ws)])
                cp("pool", wd[ws][:, 2:3, :], wds[:, 2:3, :], r=["wds"], w=[("wd", ws)])
                cp("act", wd[ws][:, 3:4, :], wds[:, 3:4, :], r=["wds"], w=[("wd", ws)])
                for j4 in range(4):
                    r0 = i * 512 + j4 * 128
                    dma("sp", ugm[j4], Ud[r0:r0 + 128, :], r=[], w=[("ugm", j4)], sem=("ugm", j4))
                for jp in range(4):
                    hb_ = jp % 2
                    for hh in range(2):
                        j = 2 * jp + hh
                        dstp = bankb(hb_)[:, hh * 512:(hh + 1) * 512]
                        for j4 in range(4):
                            tr(dstp[:, j4 * 128:(j4 + 1) * 128], ugm[j4][:, j:D:8], identB, r=[("ugm", j4), "identB"], w=[bk(hb_)])
                    for hh in range(2):
                        j = 2 * jp + hh
                        dstp = bankb(hb_)[:, hh * 512:(hh + 1) * 512]
                        act(ugT[ws][:, j, :], dstp, AF.Identity, r=[bk(hb_), "modP"], w=[("ugT", ws, j)],
                            bias=modP[:, 0, j:j + 1], scale=modP[:, 1, j:j + 1])
                ugk = [("ugT", ws, j) for j in range(8)]
                for fc in range(4):
                    bg, bu = 2 + fc % 2, 4 + fc % 2
                    for j in range(8):
                        mm(bank(bg), wg[ws][:, j, fc:512:4], ugT[ws][:, j, :], j == 0, j == 7, r=[("wg", ws), ("ugT", ws, j)], w=[bk(bg)])
                    for j in range(8):
                        mm(bank(bu), wu[ws][:, j, fc:512:4], ugT[ws][:, j, :], j == 0, j == 7, r=[("wu", ws), ("ugT", ws, j)], w=[bk(bu)])
                    act(sgt[fc % 2], bank(bg), AF.Silu, r=[bk(bg)], w=[("sgt", fc % 2)])
                    tt("dve", hTm[ws][:, fc, :], sgt[fc % 2], bank(bu), ALU.mult, r=[("sgt", fc % 2), bk(bu)], w=[("hTm", ws, fc)])
                for j4 in range(4):
                    y2 = ycnt % 2
                    ycnt += 1
                    for hf in range(2):
                        for fc in range(4):
                            mm(bank(6 + hf), hTm[ws][:, fc, j4 * 128:(j4 + 1) * 128], wd[ws][:, fc, hf * 512:(hf + 1) * 512], fc == 0, fc == 3,
                               r=[("hTm", ws, fc), ("wd", ws)], w=[bk(6 + hf)])
                    cp("act" if j4 % 2 == 0 else "dve", ysb[y2], ps[:, 6:8, :].rearrange("p a b -> p (a b)"), r=[bk(6), bk(7)], w=[("ysb", y2)])
                    r0 = i * 512 + j4 * 128
                    dma("sp", Yd[r0:r0 + 128, :], ysb[y2], r=[("ysb", y2)], w=[("Yd", i, j4)], sem=("yst", y2))
            P.barrier()

            sb.reset(moe_mark)
            ln2gB = sb([D], F32)
            ln2bB = sb([D], F32)
            ya = [sb([D], F32) for _ in range(2)]
            yb = [sb([D], F32) for _ in range(2)]
            xe = [sb([D], F32) for _ in range(2)]
            ze = [sb([D], F32) for _ in range(2)]
            xo = [sb([D], F32) for _ in range(2)]
            statse = [sb([2, 6], F32) for _ in range(2)]
            mve = [sb([2], F32) for _ in range(2)]
            rstde = [sb([1], F32) for _ in range(2)]
            nmre = [sb([1], F32) for _ in range(2)]
            dma("sp", ln2gB, ln2_g[l].partition_broadcast(128), r=[], w=["ln2gB"], sem="ln2gB")
            dma("sp", ln2bB, ln2_b[l].partition_broadcast(128), r=[], w=["ln2bB"], sem="ln2bB")
            for tg in range(NT):
                s2 = tg % 2
                for (dst, idxt, ik, key) in ((ya[s2], slAi, "slAi", ("ya", s2)), (yb[s2], slBi, "slBi", ("yb", s2))):
                    def gy(dst=dst, idxt=idxt, tg=tg):
                        return nc.gpsimd.indirect_dma_start(out=dst[:, :], out_offset=None, in_=Yd[:, :],
                                                            in_offset=bass.IndirectOffsetOnAxis(ap=idxt[:, tg:tg + 1], axis=0))
                    P.dma("pool", gy, r=[ik], w=[key], sem=key, nbytes=524288)
                dma("sp", xe[s2], x1d[tg * 128:(tg + 1) * 128, :], r=[("x1d", tg)], w=[("xe", s2)], sem=("xe", s2))
                act(ya[s2], ya[s2], AF.Identity, r=[("ya", s2), "cwA"], w=[("ya", s2)], scale=cwA[:, tg:tg + 1])
                stt(ze[s2], yb[s2], cwB[:, tg:tg + 1], ya[s2], ALU.mult, ALU.add, r=[("yb", s2), "cwB", ("ya", s2)], w=[("ze", s2)])
                tt("pool", ze[s2], ze[s2], g2B, ALU.mult, r=[("ze", s2), "g2B"], w=[("ze", s2)])
                stt(ze[s2], xe[s2], ALPHA, ze[s2], ALU.mult, ALU.add, r=[("xe", s2), ("ze", s2)], w=[("ze", s2)])
                ln_stats(ze[s2], statse[s2], mve[s2], rstde[s2], nmre[s2], ("pl2", s2), [("ze", s2)])
                act(xo[s2], ze[s2], AF.Identity, r=[("ze", s2), (("pl2", s2), "rstd"), (("pl2", s2), "nmr")], w=[("xo", s2)],
                    bias=nmre[s2], scale=rstde[s2])
                tt("dve", xo[s2], xo[s2], ln2gB, ALU.mult, r=[("xo", s2), "ln2gB"], w=[("xo", s2)])
                tt("pool", xo[s2], xo[s2], ln2bB, ALU.add, r=[("xo", s2), "ln2bB"], w=[("xo", s2)])
                dma("sp", xdst[tg * 128:(tg + 1) * 128, :], xo[s2], r=[("xo", s2)], w=[("xdst", l, tg)], sem=("xost", s2))
                if l == nlayers - 1 or stage == 4:
                    final_keys.append(("xdst", l, tg))
            P.barrier()
            if stage == 4:
                break
        P.emit(top, final_keys=final_keys)
        build.stats = dict(P.stats, sb_peak=sb.peak)
    return nc


_IN_NAMES = ["ada_w", "ada_b", "w_in", "b_in", "hgrn_lb", "hgrn_norm_w", "conv_w", "conv_b", "conv_ln_g", "conv_ln_b",
             "w_out", "b_out", "ln1_g", "ln1_b", "router_w", "router_bias", "w_gate", "w_up", "w_down", "ln2_g", "ln2_b"]


def make_in_maps(inputs, ncores=NCORES):
    shared = {k: np.ascontiguousarray(np.asarray(inputs[k], dtype=np.float32)) for k in _IN_NAMES}
    x = np.asarray(inputs["x"], dtype=np.float32)
    c = np.asarray(inputs["c"], dtype=np.float32)
    maps = []
    for i in range(ncores):
        m = dict(shared)
        m["x"] = np.ascontiguousarray(x[i])
        m["c"] = np.ascontiguousarray(c[i])
        maps.append(m)
    return maps


def kernel(**inputs):
    nc = build()
    in_maps = make_in_maps(inputs)
    res = run_bass_kernel_spmd(nc, in_maps, core_ids=list(range(NCORES)))
    return np.stack([np.asarray(r["out"], dtype=np.float32) for r in res.results], axis=0)
```

```python
import numpy as np
from contextlib import ExitStack
import concourse.bass as bass
import concourse.mybir as mybir
from concourse.bass_utils import run_bass_kernel_spmd

F32 = mybir.dt.float32
BF16 = mybir.dt.bfloat16
U8 = mybir.dt.uint8
AF = mybir.ActivationFunctionType
ALU = mybir.AluOpType
AX = mybir.AxisListType

T = 4096
D = 1024
NT = 32
NB = 8
NE = 16
ALPHA = 4.0 ** 0.25
EPS = 1e-5
NCORES = 8
SL = 512
NSTEP = NE + 2 * T // SL
SAME_ENG_SYNC = True


class Prog:
    DMA_BW = 300e3

    def __init__(self, nc):
        self.nc = nc
        self.ops = []
        self.eng = {"pe": nc.tensor, "act": nc.scalar, "dve": nc.vector,
                    "pool": nc.gpsimd, "sp": nc.sync}

    def add(self, eng, fn, r=(), w=(), cost=0.2):
        self.ops.append(dict(eng=eng, fn=fn, r=tuple(r), w=tuple(w), dma=None, bar=False, cost=cost, nbytes=0))

    def dma(self, q, fn, r=(), w=(), sem=None, nbytes=0):
        assert sem is not None
        self.ops.append(dict(eng=q, fn=fn, r=tuple(r), w=tuple(w), dma=sem, bar=False,
                             cost=(1.0 if q == "pool" else 0.1), nbytes=nbytes))

    def barrier(self):
        self.ops.append(dict(eng=None, fn=None, r=(), w=(), dma=None, bar=True, cost=0.0, nbytes=0))

    def _raw_deps(self):
        ops = self.ops
        n = len(ops)
        last_writer = {}
        readers = {}
        deps = [None] * n
        for i, op in enumerate(ops):
            if op["bar"]:
                deps[i] = set()
                continue
            d = set()
            rk = [k for k in op["r"] if not (isinstance(k, tuple) and k and k[0] == "ps")]
            wk = list(op["w"]) + [k for k in op["r"] if isinstance(k, tuple) and k and k[0] == "ps" and k not in op["w"]]
            for k in rk:
                if k in last_writer:
                    d.add(last_writer[k])
            for k in wk:
                if k in last_writer:
                    d.add(last_writer[k])
                d.update(readers.get(k, ()))
            d.discard(i)
            deps[i] = d
            for k in rk:
                readers.setdefault(k, []).append(i)
            for k in wk:
                last_writer[k] = i
                readers[k] = []
        return deps, last_writer

    def _schedule_segment(self, idxs, deps):
        ops = self.ops
        inseg = set(idxs)
        nrem = {}
        users = {}
        for i in idxs:
            dl = [j for j in deps[i] if j in inseg]
            nrem[i] = len(dl)
            for j in dl:
                users.setdefault(j, []).append(i)
        ready = {e: [] for e in self.eng}
        ready_time = {}
        finish = {}
        import heapq
        for i in idxs:
            if nrem[i] == 0:
                ready_time[i] = 0.0
                heapq.heappush(ready[ops[i]["eng"]], (0.0, i))
        etime = {e: 0.0 for e in self.eng}
        dma_free = 0.0
        order = []
        remaining = len(idxs)
        while remaining:
            best = None
            for e in self.eng:
                if not ready[e]:
                    continue
                rt, i = ready[e][0]
                st = max(etime[e], rt)
                cand = (st, i, e)
                cands = [(max(etime[e], r_), i_) for (r_, i_) in ready[e] if r_ <= st]
                if cands:
                    i2 = min(c[1] for c in cands)
                    cand = (st, i2, e)
                if best is None or cand < best:
                    best = cand
            st, i, e = best
            lst = ready[e]
            for k_, (r_, i_) in enumerate(lst):
                if i_ == i:
                    lst[k_] = lst[-1]
                    lst.pop()
                    heapq.heapify(lst)
                    break
            op = ops[i]
            if op["dma"] is not None:
                etime[e] = st + op["cost"]
                t0 = max(st + op["cost"], dma_free)
                dma_free = t0 + op["nbytes"] / self.DMA_BW
                fin = dma_free + 2.0
            else:
                fin = st + op["cost"]
                etime[e] = fin
            finish[i] = fin
            order.append(i)
            remaining -= 1
            for u in users.get(i, ()):
                nrem[u] -= 1
                rt_u = max(ready_time.get(u, 0.0), fin + 0.1)
                ready_time[u] = rt_u
                if nrem[u] == 0:
                    heapq.heappush(ready[ops[u]["eng"]], (rt_u, u))
        mk = max(finish.values()) if finish else 0.0
        busy = {e: 0.0 for e in self.eng}
        dmab = 0.0
        for i in idxs:
            busy[ops[i]["eng"]] += ops[i]["cost"]
            dmab += ops[i]["nbytes"] / self.DMA_BW
        self.seg_busy = getattr(self, "seg_busy", []) + [dict(mk=round(mk), dma=round(dmab), **{e: round(v) for e, v in busy.items()})]
        return order, mk

    def emit(self, stack, final_keys=(), schedule=True):
        nc = self.nc
        ops = self.ops
        n = len(ops)
        deps, last_writer = self._raw_deps()
        order = []
        seg = []
        seg_times = []
        for i, op in enumerate(ops):
            if op["bar"]:
                if seg:
                    if schedule:
                        o, mk = self._schedule_segment(seg, deps)
                    else:
                        o, mk = list(seg), 0.0
                    order += o
                    seg_times.append(mk)
                order.append(i)
                seg = []
            else:
                seg.append(i)
        if seg:
            if schedule:
                o, mk = self._schedule_segment(seg, deps)
            else:
                o, mk = list(seg), 0.0
            order += o
            seg_times.append(mk)
        pos = {i: p for p, i in enumerate(order)}
        last_on_eng = {}
        dmas_since = []
        pend = {e: set() for e in self.eng}
        fdeps = [None] * n
        for i in order:
            op = ops[i]
            if op["bar"]:
                d = set(last_on_eng.values()) | set(dmas_since)
                for e in self.eng:
                    pend[e] |= d
                dmas_since = []
                fdeps[i] = set()
                continue
            d = set(deps[i])
            e = op["eng"]
            if pend[e]:
                d |= pend[e]
                pend[e] = set()
            best = {}
            dd = set()
            for j in d:
                oj = ops[j]
                if oj["dma"] is not None:
                    dd.add(j)
                elif oj["eng"] not in best or pos[best[oj["eng"]]] < pos[j]:
                    best[oj["eng"]] = j
            dd.update(best.values())
            fdeps[i] = dd
            last_on_eng[e] = i
            if op["dma"] is not None:
                dmas_since.append(i)
        need_sig = [False] * n
        for i in order:
            op = ops[i]
            if op["bar"]:
                continue
            for j in fdeps[i]:
                oj = ops[j]
                if oj["dma"] is not None:
                    continue
                if oj["eng"] == op["eng"] and op["dma"] is None and (oj["eng"] == "pe" or not SAME_ENG_SYNC):
                    continue
                need_sig[j] = True
        final_deps = set(last_writer[k] for k in final_keys)
        esem = {e: stack.enter_context(nc.semaphore("s_" + e)) for e in self.eng}
        dsem = {}
        dcount = {}
        ecount = {e: 0 for e in self.eng}
        waited = {e: {} for e in self.eng}
        sig = [None] * n
        nwaits = 0
        phys = []
        pcount = []
        free_phys = []
        for i in order:
            op = ops[i]
            if op["bar"]:
                free_phys = list(range(len(phys)))
                dsem = {}
                continue
            e = op["eng"]
            eng = self.eng[e]
            for j in sorted(fdeps[i], key=lambda j: pos[j]):
                oj = ops[j]
                if oj["dma"] is None and oj["eng"] == e and op["dma"] is None and (e == "pe" or not SAME_ENG_SYNC):
                    continue
                assert pos[j] < pos[i], (i, j)
                sname, val = sig[j]
                if waited[e].get(sname, 0) >= val:
                    continue
                semh = esem[sname] if sname in esem else phys[sname]
                eng.wait_ge(semh, val)
                nwaits += 1
                waited[e][sname] = val
            inst = op["fn"]()
            if op["dma"] is not None:
                key = op["dma"]
                if key not in dsem:
                    if free_phys:
                        dsem[key] = free_phys.pop()
                    else:
                        phys.append(stack.enter_context(nc.semaphore("d_%d" % len(phys))))
                        pcount.append(0)
                        dsem[key] = len(phys) - 1
                pid = dsem[key]
                pcount[pid] += 16
                inst.then_inc(phys[pid], 16)
                sig[i] = (pid, pcount[pid])
            elif need_sig[i]:
                ecount[e] += 1
                inst.then_inc(esem[e], 1)
                sig[i] = (e, ecount[e])
        eng = self.eng["sp"]
        for j in sorted(final_deps):
            sname, val = sig[j]
            semh = esem[sname] if sname in esem else phys[sname]
            eng.wait_ge(semh, val)
        self.stats = dict(nops=n, nwaits=nwaits, ecount=ecount, ndsem=len(phys),
                          seg_us=[round(t) for t in seg_times])


class SBAlloc:
    def __init__(self, big, nbytes):
        self.big = big
        self.cap = nbytes
        self.off = 0
        self.peak = 0

    def mark(self):
        return self.off

    def reset(self, m):
        self.off = m

    def __call__(self, free_shape, dt, parts=128):
        esz = {F32: 4, BF16: 2, U8: 1, mybir.dt.int32: 4, mybir.dt.uint32: 4}[dt]
        nel = int(np.prod(free_shape))
        nbytes = (nel * esz + 63) // 64 * 64
        assert self.off + nbytes <= self.cap, ("SBUF overflow", self.off, nbytes, self.cap)
        ap = self.big[0:parts, self.off:self.off + nel * esz].bitcast(dt)
        self.off += nbytes
        self.peak = max(self.peak, self.off)
        if len(free_shape) == 2:
            ap = ap.rearrange("p (a b) -> p a b", a=free_shape[0], b=free_shape[1])
        elif len(free_shape) == 3:
            ap = ap.rearrange("p (a b c) -> p a b c", a=free_shape[0], b=free_shape[1], c=free_shape[2])
        return ap


def build(stage=99, dbg=False):
    nc = bass.Bass("TRN2", target_bir_lowering=False)
    dk = "ExternalOutput" if dbg else "Internal"

    def din(name, shape):
        return nc.dram_tensor(name, list(shape), F32, kind="ExternalInput").ap()

    x_d = din("x", [T, D])
    c_d = din("c", [D])
    ada_w = din("ada_w", [2, D, 6 * D])
    ada_b = din("ada_b", [2, 6 * D])
    w_in = din("w_in", [2, D, 3072])
    b_in = din("b_in", [2, 3072])
    hgrn_lb = din("hgrn_lb", [2, 512])
    hgrn_nw = din("hgrn_norm_w", [2, 128])
    conv_w = din("conv_w", [2, 31, 512])
    conv_b = din("conv_b", [2, 512])
    conv_g = din("conv_ln_g", [2, 512])
    conv_bb = din("conv_ln_b", [2, 512])
    w_out = din("w_out", [2, D, D])
    b_out = din("b_out", [2, D])
    ln1_g = din("ln1_g", [2, D])
    ln1_b = din("ln1_b", [2, D])
    router_w = din("router_w", [D, NE])
    router_bias = din("router_bias", [NE])
    w_gate = din("w_gate", [2, NE, D, 512])
    w_up = din("w_up", [2, NE, D, 512])
    w_down = din("w_down", [2, NE, 512, D])
    ln2_g = din("ln2_g", [2, D])
    ln2_b = din("ln2_b", [2, D])
    out_d = nc.dram_tensor("out", [T, D], F32, kind="ExternalOutput").ap()
    hTd = nc.dram_tensor("hTd", [NB, 128, 8, 512], BF16, kind=dk).ap()
    uTd = nc.dram_tensor("uTd", [NB, 128, 8, 512], BF16, kind="Internal").ap()
    winb_d = nc.dram_tensor("winb_d", [D, 3072], BF16, kind="Internal").ap()
    woutb_d = nc.dram_tensor("woutb_d", [D, D], BF16, kind="Internal").ap()
    boutb_d = nc.dram_tensor("boutb_d", [1, D], BF16, kind="Internal").ap()
    x1d = nc.dram_tensor("x1d", [T, D], F32, kind=dk).ap()
    xn2d = nc.dram_tensor("xn2d", [T, D], BF16, kind=dk).ap()
    NSLOT = NSTEP * SL
    Ud = nc.dram_tensor("Ud", [NSLOT, D], BF16, kind="Internal").ap()
    Yd = nc.dram_tensor("Yd", [NSLOT, D], F32, kind="Internal").ap()
    modrow_d = nc.dram_tensor("modrow_d", [2, D], F32, kind="Internal").ap()
    slotd = nc.dram_tensor("slotd", [128, 4 * NT], F32, kind=dk).ap()
    xmid = nc.dram_tensor("xmid", [T, D], F32, kind=dk).ap()
    logd = nc.dram_tensor("logd", [128, NT * NE], F32, kind=dk).ap()
    combd = nc.dram_tensor("combd", [128, NT * NE], F32, kind=dk).ap()
    modTd = nc.dram_tensor("modTd", [128, 32], F32, kind=dk).ap()

    SB_BYTES = 207 * 1024
    with ExitStack() as top:
        big = top.enter_context(nc.sbuf_tensor("big", [128, SB_BYTES], U8))
        ps = top.enter_context(nc.psum_tensor("ps", [128, 8, 512], F32))
        sb = SBAlloc(big, SB_BYTES)
        P = Prog(nc)
        bcreg = nc.gpsimd.alloc_register("bc")
        nc.gpsimd.reg_mov(bcreg, 2 * NE * 128 - 1)

        def bank(b):
            return ps[:, b, :]

        def bankb(b):
            return ps[:, b, :].bitcast(BF16)

        def bk(b):
            return ("ps", b)

        def fsz(ap):
            return int(np.prod(ap.shape[1:]))

        def mm(out, lhsT, rhs, start, stop, r, w):
            c = max(fsz(out), 64) * (4 if rhs.dtype == F32 else 1) / 2000.0 + 0.02
            P.add("pe", lambda: nc.tensor.matmul(out, lhsT=lhsT, rhs=rhs, start=start, stop=stop), r=r, w=w, cost=c)

        def tr(out, in_, ident, r, w):
            P.add("pe", lambda: nc.tensor.transpose(out=out, in_=in_, identity=ident), r=r, w=w, cost=0.1)

        def act(out, in_, func, r, w, bias=0.0, scale=1.0, accum=None):
            c = 0.2 + fsz(out) * 0.00085
            if accum is None:
                P.add("act", lambda: nc.scalar.activation(out=out, in_=in_, func=func, bias=bias, scale=scale), r=r, w=w, cost=c)
            else:
                P.add("act", lambda: nc.scalar.activation(out=out, in_=in_, func=func, bias=bias, scale=scale, accum_out=accum), r=r, w=w, cost=c)

        def tt(eng, out, in0, in1, op, r, w):
            e = nc.vector if eng == "dve" else nc.gpsimd
            c = (0.1 + fsz(out) * 0.00105) if eng == "dve" else (0.2 + fsz(out) * 0.0021)
            P.add(eng, lambda: e.tensor_tensor(out=out, in0=in0, in1=in1, op=op), r=r, w=w, cost=c)

        def ts(eng, out, in0, s1, s2, op0, op1, r, w):
            e = nc.vector if eng == "dve" else nc.gpsimd
            c = (0.1 + fsz(out) * 0.00105) if eng == "dve" else (0.2 + fsz(out) * 0.0021)
            if op1 is None:
                P.add(eng, lambda: e.tensor_scalar(out=out, in0=in0, scalar1=s1, scalar2=None, op0=op0), r=r, w=w, cost=c)
            else:
                P.add(eng, lambda: e.tensor_scalar(out=out, in0=in0, scalar1=s1, scalar2=s2, op0=op0, op1=op1), r=r, w=w, cost=c)

        def stt(out, in0, scalar, in1, op0, op1, r, w):
            P.add("dve", lambda: nc.vector.scalar_tensor_tensor(out=out, in0=in0, scalar=scalar, in1=in1, op0=op0, op1=op1), r=r, w=w,
                  cost=0.1 + fsz(out) * 0.00105)

        def cp(eng, out, in_, r, w):
            if eng == "act":
                P.add("act", lambda: nc.scalar.copy(out=out, in_=in_), r=r, w=w, cost=0.2 + fsz(out) * 0.00085)
            else:
                e = nc.vector if eng == "dve" else nc.gpsimd
                c = (0.1 + fsz(out) * 0.00105) if eng == "dve" else (0.2 + fsz(out) * 0.0021)
                P.add(eng, lambda: e.tensor_copy(out=out, in_=in_), r=r, w=w, cost=c)

        def memset(eng, ap, val, w):
            e = nc.vector if eng == "dve" else nc.gpsimd
            P.add(eng, lambda: e.memset(ap, val), w=w, cost=0.1 + fsz(ap) * 0.001)

        def dma(q, out, in_, r, w, sem, slow=False):
            e = {"sp": nc.sync, "pool": nc.gpsimd, "act": nc.scalar}[q]
            nb = int(np.prod(out.shape)) * (2 if out.dtype == BF16 else 4)
            if in_.dtype == F32 and out.dtype == BF16:
                nb *= 2
            if slow:
                def f():
                    with nc.allow_non_contiguous_dma(reason="small strided load"):
                        return e.dma_start(out=out, in_=in_)
                P.dma(q, f, r=r, w=w, sem=sem, nbytes=nb)
            else:
                P.dma(q, lambda: e.dma_start(out=out, in_=in_), r=r, w=w, sem=sem, nbytes=nb)

        def ln_stats(src, stats, mv, rstd, nmr, tag, rkeys):
            for h in range(2):
                P.add("dve", (lambda h=h: nc.vector.bn_stats(out=stats[:, h, :], in_=src[:, h * 512:(h + 1) * 512])),
                      r=rkeys, w=[(tag, "st", h)], cost=0.65)
            P.add("dve", lambda: nc.vector.bn_aggr(out=mv, in_=stats.rearrange("p a b -> p (a b)")),
                  r=[(tag, "st", 0), (tag, "st", 1)], w=[(tag, "mv")])
            act(rstd, mv[:, 1:2], AF.Ln, r=[(tag, "mv")], w=[(tag, "rstd")], bias=eps_t)
            act(rstd, rstd, AF.Exp, r=[(tag, "rstd")], w=[(tag, "rstd")], scale=-0.5)
            if nmr is not None:
                ts("dve", nmr, mv[:, 0:1], rstd, -1.0, ALU.mult, ALU.mult, r=[(tag, "mv"), (tag, "rstd")], w=[(tag, "nmr")])

        identF = sb([128], F32)
        identB = sb([128], BF16)
        Lincl = sb([128], F32)
        M1 = sb([128], F32)
        maskB = sb([128], BF16)
        onesM = sb([128], F32)
        ones_row = sb([128], BF16, parts=1)
        condT = sb([8], F32)
        cond_bc = sb([8, 128], F32)
        biasB = sb([NE], F32)
        eps_t = sb([1], F32)
        mhalf = sb([512], F32)
        one_t = sb([1], F32)

        memset("pool", identF, 1.0, w=["identF"])
        P.add("pool", lambda: nc.gpsimd.affine_select(out=identF, in_=identF, pattern=[[-1, 128]], compare_op=ALU.is_equal,
                                                      fill=0.0, base=0, channel_multiplier=1), r=["identF"], w=["identF"])
        cp("pool", identB, identF, r=["identF"], w=["identB"])
        memset("pool", Lincl, 1.0, w=["Lincl"])
        P.add("pool", lambda: nc.gpsimd.affine_select(out=Lincl, in_=Lincl, pattern=[[1, 128]], compare_op=ALU.is_ge,
                                                      fill=0.0, base=0, channel_multiplier=-1), r=["Lincl"], w=["Lincl"])
        memset("pool", Lincl[0:64, 64:128], 0.0, w=["Lincl"])
        cp("pool", maskB, Lincl, r=["Lincl"], w=["maskB"])
        memset("pool", M1, 1.0, w=["M1"])
        P.add("pool", lambda: nc.gpsimd.affine_select(out=M1, in_=M1, pattern=[[-1, 128]], compare_op=ALU.is_ge,
                                                      fill=0.0, base=-1, channel_multiplier=1), r=["M1"], w=["M1"])
        memset("pool", M1[64:128, 0:64], 0.0, w=["M1"])
        memset("pool", onesM, 1.0 / 512.0, w=["onesM"])
        memset("pool", ones_row, 1.0, w=["ones_row"])
        memset("pool", mhalf, -0.5, w=["mhalf"])
        memset("pool", eps_t, EPS, w=["eps_t"])
        memset("pool", one_t, 1.0, w=["one_t"])
        dma("sp", condT, c_d.rearrange("(k p) -> p k", p=128), r=[], w=["condT"], sem="condT", slow=True)
        act(condT, condT, AF.Silu, r=["condT"], w=["condT"])
        for k in range(8):
            ts("dve", cond_bc[:, k, :], onesM, condT[:, k:k + 1], 512.0, ALU.mult, ALU.mult, r=["onesM", "condT"], w=["cond_bc"])
        dma("sp", biasB, router_bias.partition_broadcast(128), r=[], w=["biasB"], sem="biasB")
        perm_mark = sb.mark()

        final_keys = []
        nlayers = 2
        for l in range(nlayers):
            sb.reset(perm_mark)
            xsrc = x_d if l == 0 else xmid
            xdst = xmid if l == 0 else out_d
            modT = sb([4, 8], F32)
            g1B = sb([D], F32)
            g2B = sb([D], F32)
            logits = sb([NT * NE], F32)
            comb = sb([NT * NE], F32)
            modP = sb([2, 8], F32)
            layer_mark = sb.mark()

            def phase0(l):
                adaw = [sb([8, 512], F32) for _ in range(2)]
                adab = [sb([512], F32) for _ in range(2)]
                piece = [sb([512], F32) for _ in range(2)]
                kind_of = {0: 0, 1: 0, 2: 1, 3: 1, 6: 2, 7: 2, 8: 3, 9: 3}
                for j in range(12):
                    s2 = j % 2
                    dma("sp", adaw[s2], ada_w[l, :, j * 512:(j + 1) * 512].rearrange("(k p) n -> p k n", p=128),
                        r=[], w=[("adaw", s2)], sem=("adaw", s2))
                    dma("sp", adab[s2], ada_b[l, j * 512:(j + 1) * 512].partition_broadcast(128), r=[], w=[("adab", s2)], sem=("adab", s2))
                    for kc in range(8):
                        mm(bank(s2), cond_bc[:, kc, :], adaw[s2][:, kc, :], kc == 0, kc == 7,
                           r=["cond_bc", ("adaw", s2)], w=[bk(s2)])
                    if j in (4, 5):
                        dst = g1B[:, (j - 4) * 512:(j - 3) * 512]
                        tt("dve", dst, bank(s2), adab[s2], ALU.add, r=[bk(s2), ("adab", s2)], w=["g1B"])
                    elif j in (10, 11):
                        dst = g2B[:, (j - 10) * 512:(j - 9) * 512]
                        tt("dve", dst, bank(s2), adab[s2], ALU.add, r=[bk(s2), ("adab", s2)], w=["g2B"])
                    else:
                        tt("dve", piece[s2], bank(s2), adab[s2], ALU.add, r=[bk(s2), ("adab", s2)], w=[("piece", s2)])
                        if j in (6, 7, 8, 9):
                            rr = 0 if j in (6, 7) else 1
                            dma("sp", modrow_d[rr:rr + 1, (j % 2) * 512:(j % 2 + 1) * 512], piece[s2][0:1, :], r=[("piece", s2)],
                                w=[("modrow", rr, j % 2)], sem=("modrow", s2))
                        for b in range(4):
                            tr(bank(2 + s2)[:, b * 128:(b + 1) * 128], piece[s2][:, b * 128:(b + 1) * 128], identF,
                               r=[("piece", s2), "identF"], w=[bk(2 + s2)])
                        kd = kind_of[j]
                        half = j % 2
                        dstm = modT[:, kd, half * 4:half * 4 + 4]
                        src = bank(2 + s2).rearrange("p (b c) -> p b c", c=128)[:, :, 0]
                        if kd in (1, 3):
                            ts("dve", dstm, src, 1.0, None, ALU.add, None, r=[bk(2 + s2)], w=["modT"])
                        else:
                            cp("dve", dstm, src, r=[bk(2 + s2)], w=["modT"])
                for rr in range(2):
                    dma("sp", modP[:, rr, :], modrow_d[rr].rearrange("(p j) -> p j", j=8), r=[("modrow", rr, 0), ("modrow", rr, 1)],
                        w=["modP"], sem=("modP", rr))
                ts("dve", modP[:, 1, :], modP[:, 1, :], 1.0, None, ALU.add, None, r=["modP"], w=["modP"])
                wst = [sb([1536], F32) for _ in range(2)]
                wbf = [sb([1536], BF16) for _ in range(2)]
                pc = 0
                for kc in range(8):
                    for (c0, c1) in ((0, 1536), (1536, 3072)):
                        q2 = pc % 2
                        pc += 1
                        dma("sp", wst[q2], w_in[l, kc * 128:(kc + 1) * 128, c0:c1], r=[], w=[("wst", q2)], sem=("wst", q2))
                        cp("dve" if q2 == 0 else "act", wbf[q2], wst[q2], r=[("wst", q2)], w=[("wbf", q2)])
                        dma("sp", winb_d[kc * 128:(kc + 1) * 128, c0:c1], wbf[q2], r=[("wbf", q2)], w=[("winb_d", kc)], sem=("wbfst", q2))
                for kc in range(8):
                    q2 = pc % 2
                    pc += 1
                    dma("sp", wst[q2][:, 0:1024], w_out[l, kc * 128:(kc + 1) * 128, :], r=[], w=[("wst", q2)], sem=("wst", q2))
                    tt("dve" if q2 == 0 else "pool", wbf[q2][:, 0:1024], wst[q2][:, 0:1024], g1B, ALU.mult, r=[("wst", q2), "g1B"], w=[("wbf", q2)])
                    dma("sp", woutb_d[kc * 128:(kc + 1) * 128, :], wbf[q2][:, 0:1024], r=[("wbf", q2)], w=[("woutb_d", kc)], sem=("wbfst", q2))
                q2 = pc % 2
                dma("sp", wst[q2][0:1, 0:1024], b_out[l:l + 1, :], r=[], w=[("wst", q2)], sem=("wst", q2))
                tt("dve", wbf[q2][0:1, 0:1024], wst[q2][0:1, 0:1024], g1B[0:1, :], ALU.mult, r=[("wst", q2), "g1B"], w=[("wbf", q2)])
                dma("sp", boutb_d, wbf[q2][0:1, 0:1024], r=[("wbf", q2)], w=["boutb_d"], sem=("wbfst", q2))
                if dbg and l == 0:
                    dma("sp", modTd, modT.rearrange("p a b -> p (a b)"), r=["modT"], w=["modTd"], sem="modTd")
                    final_keys.append("modTd")
            if l == 0:
                phase0(0)
                P.barrier()
            if stage == 0:
                break

            sb.reset(layer_mark)
            winc = sb([8, 1024], BF16)
            dg = sb([124, 128], BF16)
            b_inT = sb([12], F32)
            b_inH = sb([12], F32)
            cwT = sb([4, 31], F32)
            cbT = sb([4], F32)
            cgT = sb([4], F32)
            cbbT = sb([4], F32)
            abuf = sb([4, 30 + T], BF16)
            xt = [sb([D], F32) for _ in range(2)]
            xn = [sb([D], F32) for _ in range(2)]
            uT = [sb([8, 512], BF16) for _ in range(2)]
            sg = [sb([512], F32) for _ in range(2)]
            ac = [sb([4, 512], F32) for _ in range(2)]
            sq = [sb([512], F32) for _ in range(2)]
            t1 = [sb([512], F32) for _ in range(2)]
            meanS = [sb([512], F32) for _ in range(2)]
            varS = [sb([512], F32) for _ in range(2)]
            rstdB = [sb([512], F32) for _ in range(2)]
            hTc = [sb([4, 512], BF16) for _ in range(2)]
            stats = [sb([2, 6], F32) for _ in range(2)]
            mv = [sb([2], F32) for _ in range(2)]
            rstd = [sb([1], F32) for _ in range(2)]
            cw_tm = sg[0][0:31, :]
            yv = [sb([512], F32) for _ in range(2)]
            ncgT = sb([4], F32)
            ncbbT = sb([4], F32)
            print("A1 sbuf", sb.off)

            for kc in range(8):
                dma("sp", winc[:, kc, :], winb_d[kc * 128:(kc + 1) * 128, 2048:3072], r=[("winb_d", kc)], w=[("winc", kc)], sem=("winc", kc))
            dma("sp", b_inT[:, 0:4], b_in[l, 0:512].rearrange("(c p) -> p c", p=128), r=[], w=["b_inT"], sem="b_inT0", slow=True)
            dma("sp", b_inT[:, 4:12], b_in[l, 2048:3072].rearrange("(c p) -> p c", p=128), r=[], w=["b_inT"], sem="b_inT1", slow=True)
            ts("dve", b_inH, b_inT, -1.0, None, ALU.mult, None, r=["b_inT"], w=["b_inH"])
            dma("sp", cw_tm, conv_w[l], r=[], w=[("sg", 0)], sem="cw_tm")
            for ch in range(4):
                tr(bank(0)[:, ch * 32:ch * 32 + 31], cw_tm[0:31, ch * 128:(ch + 1) * 128], identF[0:31, 0:31],
                   r=[("sg", 0), "identF"], w=[bk(0)])
                cp("dve", cwT[:, ch, :], bank(0)[:, ch * 32:ch * 32 + 31], r=[bk(0)], w=["cwT"])
            for ch in range(4):
                for j in range(31):
                    ts("dve", dg[:, ch * 31 + j, :], identF, cwT[:, ch, j:j + 1], None, ALU.mult, None,
                       r=["identF", "cwT"], w=[("dg", ch)])
            dma("sp", cbT, conv_b[l].rearrange("(c p) -> p c", p=128), r=[], w=["cbT"], sem="cbT", slow=True)
            dma("sp", cgT, conv_g[l].rearrange("(c p) -> p c", p=128), r=[], w=["cgT"], sem="cgT", slow=True)
            dma("sp", cbbT, conv_bb[l].rearrange("(c p) -> p c", p=128), r=[], w=["cbbT"], sem="cbbT", slow=True)
            ts("dve", ncgT, cgT, -1.0, None, ALU.mult, None, r=["cgT"], w=["ncgT"])
            ts("dve", ncbbT, cbbT, -1.0, None, ALU.mult, None, r=["cbbT"], w=["ncbbT"])
            memset("pool", abuf[:, :, 0:30], 0.0, w=[("abuf", ch, -1) for ch in range(4)])

            for blk in range(NB):
                pb = blk % 2
                for t4 in range(4):
                    t = blk * 4 + t4
                    s2 = t % 2
                    dma("sp", xt[s2], xsrc[t * 128:(t + 1) * 128, :], r=[], w=[("xt", s2)], sem=("xt", s2))
                    ln_stats(xt[s2], stats[s2], mv[s2], rstd[s2], None, ("ln1", s2), [("xt", s2)])
                    ts("dve", xn[s2], xt[s2], mv[s2][:, 0:1], rstd[s2], ALU.subtract, ALU.mult,
                       r=[("xt", s2), (("ln1", s2), "mv"), (("ln1", s2), "rstd")], w=[("xn", s2)])
                    for rnd in range(2):
                        for kc in range(rnd * 4, rnd * 4 + 4):
                            tr(bank(0)[:, (kc % 4) * 128:(kc % 4 + 1) * 128], xn[s2][:, kc * 128:(kc + 1) * 128], identF,
                               r=[("xn", s2), "identF"], w=[bk(0)])
                        for kc in range(rnd * 4, rnd * 4 + 4):
                            act(uT[pb][:, kc, t4 * 128:(t4 + 1) * 128], bank(0)[:, (kc % 4) * 128:(kc % 4 + 1) * 128], AF.Identity,
                                r=[bk(0), "modT"], w=[("uT", pb, t4)], bias=modT[:, 0, kc:kc + 1], scale=modT[:, 1, kc:kc + 1])
                uTk = [("uT", pb, i) for i in range(4)]
                dma("sp", uTd[blk], uT[pb], r=uTk, w=[("uTd", blk)], sem=("uTst", pb))
                for ch in range(4):
                    c2 = ch % 2
                    bca, bcg = 1 + 2 * c2, 2 + 2 * c2
                    for kc in range(8):
                        mm(bank(bca), winc[:, kc, ch * 128:(ch + 1) * 128], uT[pb][:, kc, :], kc == 0, kc == 7,
                           r=uTk + [("winc", kc)], w=[bk(bca)])
                    for kc in range(8):
                        mm(bank(bcg), winc[:, kc, 512 + ch * 128:512 + (ch + 1) * 128], uT[pb][:, kc, :], kc == 0, kc == 7,
                           r=uTk + [("winc", kc)], w=[bk(bcg)])
                    act(sg[c2], bank(bcg), AF.Exp, r=[bk(bcg), "b_inH"], w=[("sg", c2)], bias=b_inH[:, 8 + ch:9 + ch], scale=-1.0)
                    act(sg[c2], sg[c2], AF.Ln, r=[("sg", c2)], w=[("sg", c2)], bias=one_t)
                    act(sg[c2], sg[c2], AF.Exp, r=[("sg", c2)], w=[("sg", c2)], scale=-1.0)
                    stt(abuf[:, ch, 30 + blk * 512:30 + (blk + 1) * 512], bank(bca), b_inT[:, 4 + ch:5 + ch], sg[c2], ALU.add, ALU.mult,
                        r=[bk(bca), ("sg", c2), "b_inT"], w=[("abuf", ch, blk)])
                    for j in range(31):
                        mm(bank(5), dg[:, ch * 31 + j, :], abuf[:, ch, blk * 512 + j:blk * 512 + j + 512], j == 0, j == 30,
                           r=[("dg", ch), ("abuf", ch, blk), ("abuf", ch, blk - 1)], w=[bk(5)])
                    act(ac[pb][:, ch, :], bank(5), AF.Identity, r=[bk(5), "cbT"], w=[("ac", pb, ch)], bias=cbT[:, ch:ch + 1])
                    act(sq[c2], bank(5), AF.Square, r=[bk(5), "cbT"], w=[("sq", c2)], bias=cbT[:, ch:ch + 1])
                    mm(bank(6), onesM, ac[pb][:, ch, :], ch == 0, ch == 3, r=["onesM", ("ac", pb, ch)], w=[bk(6)])
                    mm(bank(7), onesM, sq[c2], ch == 0, ch == 3, r=["onesM", ("sq", c2)], w=[bk(7)])
                cp("act", meanS[pb], bank(6), r=[bk(6)], w=[("meanS", pb)])
                tt("dve", varS[pb], meanS[pb], meanS[pb], ALU.mult, r=[("meanS", pb)], w=[("varS", pb)])
                tt("dve", varS[pb], bank(7), varS[pb], ALU.subtract, r=[bk(7), ("varS", pb)], w=[("varS", pb)])
                act(rstdB[pb], varS[pb], AF.Ln, r=[("varS", pb)], w=[("rstdB", pb)], bias=eps_t)
                act(rstdB[pb], rstdB[pb], AF.Exp, r=[("rstdB", pb)], w=[("rstdB", pb)], scale=-0.5)
                for ch in range(4):
                    c2 = ch % 2
                    tt("dve", t1[c2], ac[pb][:, ch, :], meanS[pb], ALU.subtract, r=[("ac", pb, ch), ("meanS", pb)], w=[("t1", c2)])
                    tt("pool", t1[c2], t1[c2], rstdB[pb], ALU.mult, r=[("t1", c2), ("rstdB", pb)], w=[("t1", c2)])
                    act(yv[c2], t1[c2], AF.Identity, r=[("t1", c2), "cgT", "cbbT"], w=[("yv", c2)],
                        bias=cbbT[:, ch:ch + 1], scale=cgT[:, ch:ch + 1])
                    act(t1[c2], t1[c2], AF.Exp, r=[("t1", c2), "ncgT", "ncbbT"], w=[("t1", c2)],
                        bias=ncbbT[:, ch:ch + 1], scale=ncgT[:, ch:ch + 1])
                    act(t1[c2], t1[c2], AF.Ln, r=[("t1", c2)], w=[("t1", c2)], bias=one_t)
                    act(t1[c2], t1[c2], AF.Exp, r=[("t1", c2)], w=[("t1", c2)], scale=-1.0)
                    tt("dve", hTc[pb][:, ch, :], yv[c2], t1[c2], ALU.mult, r=[("yv", c2), ("t1", c2)], w=[("hTc", pb, ch)])
                dma("sp", hTd[blk, :, 4:8, :], hTc[pb], r=[("hTc", pb, ch) for ch in range(4)], w=[("hTd", blk, 1)], sem=("hTcst", pb))
            P.barrier()

            sb.reset(layer_mark)
            winh = sb([8, 2048], BF16)
            lbB = sb([512], F32)
            omlbB = sb([512], F32)
            nwB = sb([512], F32)
            b_inT = sb([4], F32)
            b_inH = sb([4], F32)
            omlbH = sb([512], F32)
            nwBH = sb([512], F32)
            brow = sb([1536], BF16, parts=1)
            uT = [sb([8, 512], BF16) for _ in range(2)]
            qT = [sb([4, 512], F32) for _ in range(2)]
            qb = [sb([512], F32) for _ in range(2)]
            zs = [sb([512], F32) for _ in range(2)]
            tmp = [sb([512], F32) for _ in range(2)]
            gt4 = [sb([4, 512], F32) for _ in range(2)]
            kk4 = [sb([4, 512], F32) for _ in range(2)]
            ec = [sb([512], F32) for _ in range(2)]
            sog = [sb([512], F32) for _ in range(2)]
            gate = [sb([512], F32) for _ in range(2)]
            sqo = [sb([512], F32) for _ in range(2)]
            khat = [sb([512], BF16) for _ in range(2)]
            vt = [sb([512], BF16) for _ in range(2)]
            eb = [sb([4, 128], F32) for _ in range(2)]
            enc = [sb([4, 128], F32) for _ in range(2)]
            qtA = [sb([4, 128], BF16) for _ in range(2)]
            qtB = [sb([4, 128], BF16) for _ in range(2)]
            qp = [sb([4, 128], BF16) for _ in range(2)]
            khT = [sb([4, 128], BF16) for _ in range(2)]
            AT = [sb([4, 128], BF16) for _ in range(2)]
            S = sb([4, 128], F32)
            Sb0 = [sb([4, 128], BF16) for _ in range(2)]
            Sb1 = [sb([4, 128], BF16) for _ in range(2)]
            ha = [sb([512], BF16) for _ in range(2)]
            hTa = [sb([4, 512], BF16) for _ in range(2)]
            ss = [sb([4], F32) for _ in range(2)]
            rs = [sb([4], F32) for _ in range(2)]
            lb2 = sb([2, 512], F32)
            print("A2 sbuf", sb.off)

            for kc in range(8):
                dma("sp", winh[:, kc, :], winb_d[kc * 128:(kc + 1) * 128, 0:2048], r=[("winb_d", kc)], w=[("winh", kc)], sem=("winh", kc))
            dma("sp", b_inT, b_in[l, 0:512].rearrange("(c p) -> p c", p=128), r=[], w=["b_inT"], sem="b_inTq", slow=True)
            ts("dve", b_inH, b_inT, -1.0, None, ALU.mult, None, r=["b_inT"], w=["b_inH"])
            dma("pool", brow, b_in[l:l + 1, 512:2048], r=[], w=["brow"], sem="brow")
            if l == 0:
                memset("pool", lbB, 0.0, w=["lbB"])
                memset("pool", omlbB, 1.0, w=["omlbB"])
            else:
                dma("sp", lb2.rearrange("p a b -> p (a b)"), hgrn_lb.rearrange("a b -> (a b)").partition_broadcast(128),
                    r=[], w=["lb2"], sem="lb2")
                act(lb2, lb2, AF.Exp, r=["lb2"], w=["lb2"])
                tt("dve", tmp[0], lb2[:, 0, :], lb2[:, 1, :], ALU.add, r=["lb2"], w=[("tmp", 0)])
                P.add("dve", lambda: nc.vector.reciprocal(out=tmp[0], in_=tmp[0]), r=[("tmp", 0)], w=[("tmp", 0)], cost=4.2)
                tt("dve", lbB, lb2[:, 1, :], tmp[0], ALU.mult, r=["lb2", ("tmp", 0)], w=["lbB"])
                tt("dve", omlbB, lb2[:, 0, :], tmp[0], ALU.mult, r=["lb2", ("tmp", 0)], w=["omlbB"])
            for h in range(4):
                dma("sp", nwB[:, h * 128:(h + 1) * 128], hgrn_nw[l].partition_broadcast(128), r=[], w=["nwB"], sem=("nwB", h))
            ts("dve", omlbH, omlbB, 0.5, None, ALU.mult, None, r=["omlbB"], w=["omlbH"])
            ts("dve", nwBH, nwB, 0.5, None, ALU.mult, None, r=["nwB"], w=["nwBH"])
            memset("pool", S, 0.0, w=["S"])
            memset("pool", Sb0[0], 0.0, w=[("Sb0", 0)])
            for p in range(2):
                memset("pool", qtA[p], 0.0, w=[("qtA", p)])
                memset("pool", qtB[p], 0.0, w=[("qtB", p)])

            for blk in range(NB):
                pb = blk % 2
                dma("sp", uT[pb], uTd[blk], r=[("uTd", blk)], w=[("uT", pb)], sem=("uTld", pb))
                for h in range(4):
                    b_ = 0 + 4 * (h % 2)
                    for kc in range(8):
                        mm(bank(b_), winh[:, kc, h * 128:(h + 1) * 128], uT[pb][:, kc, :], kc == 0, kc == 7,
                           r=[("uT", pb), ("winh", kc)], w=[bk(b_)])
                    h2 = h % 2
                    act(zs[h2], bank(b_), AF.Exp, r=[bk(b_), "b_inH"], w=[("zs", h2)], bias=b_inH[:, h:h + 1], scale=-1.0)
                    act(qb[h2], bank(b_), AF.Identity, r=[bk(b_), "b_inT"], w=[("qb", h2)], bias=b_inT[:, h:h + 1])
                    act(zs[h2], zs[h2], AF.Ln, r=[("zs", h2)], w=[("zs", h2)], bias=one_t)
                    act(zs[h2], zs[h2], AF.Exp, r=[("zs", h2)], w=[("zs", h2)], scale=-1.0)
                    tt("dve", qT[pb][:, h, :], zs[h2], qb[h2], ALU.mult, r=[("zs", h2), ("qb", h2)], w=[("qT", pb, h)])
                qk = [("qT", pb, h) for h in range(4)]
                for t4 in range(4):
                    p = (blk * 4 + t4) % 2
                    fb = 4 * p + 2
                    tsl = slice(t4 * 128, (t4 + 1) * 128)
                    for kc in range(8):
                        mm(bank(fb), uT[pb][:, kc, tsl], winh[:, kc, 512:1024], kc == 0, False,
                           r=[("uT", pb), ("winh", kc)], w=[bk(fb)])
                    mm(bank(fb), ones_row, brow[:, 0:512], False, True, r=["ones_row", "brow"], w=[bk(fb)])
                    act(zs[p], bank(fb), AF.Exp, r=[bk(fb)], w=[("zs", p)], scale=-1.0)
                    act(zs[p], zs[p], AF.Ln, r=[("zs", p)], w=[("zs", p)], bias=one_t)
                    act(zs[p], zs[p], AF.Exp, r=[("zs", p)], w=[("zs", p)], scale=-1.0)
                    tt("dve", tmp[p], zs[p], omlbB, ALU.mult, r=[("zs", p), "omlbB"], w=[("tmp", p)])
                    tt("pool", kk4[pb][:, t4, :], omlbB, tmp[p], ALU.subtract, r=["omlbB", ("tmp", p)], w=[("kk4", pb, t4)])
                    tt("dve", gt4[pb][:, t4, :], tmp[p], lbB, ALU.add, r=[("tmp", p), "lbB"], w=[("gt4", pb, t4)])
                g4k = [("gt4", pb, i) for i in range(4)]
                act(gt4[pb], gt4[pb], AF.Ln, r=g4k, w=g4k)
                for t4 in range(4):
                    t = blk * 4 + t4
                    p = t % 2
                    pn = 1 - p
                    B0 = 4 * p
                    tsl = slice(t4 * 128, (t4 + 1) * 128)
                    gt = [gt4[pb][:, t4, :]] * 2
                    kk = [kk4[pb][:, t4, :]] * 2
                    for pi, (pbk, col0) in ((1, (B0 + 1, 1024)), (2, (B0 + 2, 1536))):
                        for kc in range(8):
                            mm(bank(pbk), uT[pb][:, kc, tsl], winh[:, kc, col0:col0 + 512], kc == 0, False,
                               r=[("uT", pb), ("winh", kc)], w=[bk(pbk)])
                        mm(bank(pbk), ones_row, brow[:, pi * 512:(pi + 1) * 512], False, True, r=["ones_row", "brow"], w=[bk(pbk)])
                    cp("dve", vt[p], bank(B0 + 1), r=[bk(B0 + 1)], w=[("vt", p)])
                    act(sog[p], bank(B0 + 2), AF.Exp, r=[bk(B0 + 2)], w=[("sog", p)], scale=-1.0)
                    act(sog[p], sog[p], AF.Ln, r=[("sog", p)], w=[("sog", p)], bias=one_t)
                    act(sog[p], sog[p], AF.Exp, r=[("sog", p)], w=[("sog", p)], scale=-1.0)
                    tt("dve", sog[p], sog[p], bank(B0 + 2), ALU.mult, r=[("sog", p), bk(B0 + 2)], w=[("sog", p)])
                    tt("pool", gate[p], sog[p], nwB, ALU.mult, r=[("sog", p), "nwB"], w=[("gate", p)])
                    mm(bank(B0 + 3), M1, gt[p], True, True, r=["M1", ("gt4", pb, t4)], w=[bk(B0 + 3)])
                    for h in range(4):
                        mm(bank(B0 + 0)[:, h * 128:(h + 1) * 128], gt[p][:, h * 128:(h + 1) * 128], Lincl, True, True,
                           r=[("gt4", pb, t4), "Lincl"], w=[bk(B0 + 0)])
                    for h in range(4):
                        mm(bank(B0 + 1)[:, h * 128:(h + 1) * 128], gt[p][:, h * 128:(h + 1) * 128], M1, True, True,
                           r=[("gt4", pb, t4), "M1"], w=[bk(B0 + 1)])
                    act(ec[p], bank(B0 + 3), AF.Exp, r=[bk(B0 + 3)], w=[("ec", p)])
                    tt("dve", khat[p], kk[p], ec[p], ALU.mult, r=[("kk4", pb, t4), ("ec", p)], w=[("khat", p)])
                    act(eb[p].rearrange("p a b -> p (a b)"), bank(B0 + 0), AF.Exp, r=[bk(B0 + 0)], w=[("eb", p)])
                    ts("dve", enc[p].rearrange("p a b -> p (a b)"), bank(B0 + 1), -1.0, 75.0, ALU.mult, ALU.min, r=[bk(B0 + 1)], w=[("enc", p)])
                    act(enc[p], enc[p], AF.Exp, r=[("enc", p)], w=[("enc", p)])
                    tt("dve", qp[p], qT[pb][:, :, tsl], enc[p], ALU.mult, r=qk + [("enc", p)], w=[("qp", p)])
                    tt("pool", qtA[p][:, :, 0:64], qT[pb][:, :, t4 * 128:t4 * 128 + 64], eb[p][:, :, 0:64], ALU.mult,
                       r=qk + [("eb", p)], w=[("qtA", p)])
                    tt("pool", qtB[p][:, :, 64:128], qT[pb][:, :, t4 * 128 + 64:t4 * 128 + 128], eb[p][:, :, 64:128], ALU.mult,
                       r=qk + [("eb", p)], w=[("qtB", p)])
                    for h in range(4):
                        tr(bankb(B0 + 2)[:, h * 128:(h + 1) * 128], khat[p][:, h * 128:(h + 1) * 128], identB,
                           r=[("khat", p), "identB"], w=[bk(B0 + 2)])
                    cp("act", khT[p].rearrange("p a b -> p (a b)"), bankb(B0 + 2)[:, 0:512], r=[bk(B0 + 2)], w=[("khT", p)])
                    for h in range(4):
                        mm(bank(B0 + 3)[:, h * 128:(h + 1) * 128], khT[p][:, h, :], qp[p][:, h, :], True, True,
                           r=[("khT", p), ("qp", p)], w=[bk(B0 + 3)])
                    tt("dve", AT[p], bank(B0 + 3).rearrange("p (a b) -> p a b", a=4), maskB.unsqueeze(1).to_broadcast([128, 4, 128]), ALU.mult,
                       r=[bk(B0 + 3), "maskB"], w=[("AT", p)])
                    for h in range(4):
                        mm(bank(B0 + 0)[:, h * 128:(h + 1) * 128], khat[p][0:64, h * 128:(h + 1) * 128], vt[p][0:64, h * 128:(h + 1) * 128],
                           True, True, r=[("khat", p), ("vt", p)], w=[bk(B0 + 0)])
                    for h in range(4):
                        stt(S[:, h, :], S[:, h, :], eb[p][:, h, 63:64], bank(B0 + 0)[:, h * 128:(h + 1) * 128], ALU.mult, ALU.add,
                            r=["S", ("eb", p), bk(B0 + 0)], w=["S"])
                    cp("pool", Sb1[p], S, r=["S"], w=[("Sb1", p)])
                    for h in range(4):
                        hs = slice(h * 128, (h + 1) * 128)
                        mm(bank(B0 + 1)[:, hs], AT[p][:, h, :], vt[p][:, hs], True, False, r=[("AT", p), ("vt", p)], w=[bk(B0 + 1)])
                        mm(bank(B0 + 1)[:, hs], qtA[p][:, h, :], Sb0[p][:, h, :], False, False, r=[("qtA", p), ("Sb0", p)], w=[bk(B0 + 1)])
                        mm(bank(B0 + 1)[:, hs], qtB[p][:, h, :], Sb1[p][:, h, :], False, True, r=[("qtB", p), ("Sb1", p)], w=[bk(B0 + 1)])
                    for h in range(4):
                        mm(bank(B0 + 2)[:, h * 128:(h + 1) * 128], khat[p][64:128, h * 128:(h + 1) * 128], vt[p][64:128, h * 128:(h + 1) * 128],
                           True, True, r=[("khat", p), ("vt", p)], w=[bk(B0 + 2)])
                    for h in range(4):
                        stt(S[:, h, :], S[:, h, :], eb[p][:, h, 127:128], bank(B0 + 2)[:, h * 128:(h + 1) * 128], ALU.mult, ALU.add,
                            r=["S", ("eb", p), bk(B0 + 2)], w=["S"])
                    cp("pool", Sb0[pn], S, r=["S"], w=[("Sb0", pn)])
                    act(sqo[p], bank(B0 + 1), AF.Square, r=[bk(B0 + 1)], w=[("sqo", p)])
                    P.add("dve", (lambda p=p: nc.vector.tensor_reduce(out=ss[p], in_=sqo[p].rearrange("p (a b) -> p a b", a=4), axis=AX.X, op=ALU.add)),
                          r=[("sqo", p)], w=[("ss", p)], cost=0.65)
                    act(rs[p], ss[p], AF.Ln, r=[("ss", p)], w=[("rs", p)], bias=eps_t, scale=1.0 / 128.0)
                    act(rs[p], rs[p], AF.Exp, r=[("rs", p)], w=[("rs", p)], scale=-0.5)
                    for h in range(4):
                        hs = slice(h * 128, (h + 1) * 128)
                        stt(ha[p][:, hs], bank(B0 + 1)[:, hs], rs[p][:, h:h + 1], gate[p][:, hs], ALU.mult, ALU.mult,
                            r=[bk(B0 + 1), ("rs", p), ("gate", p)], w=[("ha", p)])
                    for h in range(4):
                        tr(bankb(B0 + 3)[:, h * 128:(h + 1) * 128], ha[p][:, h * 128:(h + 1) * 128], identB, r=[("ha", p), "identB"], w=[bk(B0 + 3)])
                    for h in range(4):
                        cp("act", hTa[pb][:, h, tsl], bankb(B0 + 3)[:, h * 128:(h + 1) * 128], r=[bk(B0 + 3)], w=[("hTa", pb, t4)])
                dma("sp", hTd[blk, :, 0:4, :], hTa[pb], r=[("hTa", pb, i) for i in range(4)], w=[("hTd", blk, 0)], sem=("hTast", pb))
            P.barrier()
            if stage == 1:
                final_keys.append(("hTd", NB - 1, 0))
                break

            sb.reset(layer_mark)
            wout = sb([8, D], BF16)
            brow_o = sb([D], BF16, parts=1)
            ln1gB = sb([D], F32)
            ln1bB = sb([D], F32)
            rw = sb([8, NE], F32)
            hTb = [sb([8, 512], BF16) for _ in range(2)]
            xtb = [sb([D], F32) for _ in range(2)]
            zt = [sb([D], F32) for _ in range(2)]
            x1 = [sb([D], F32) for _ in range(2)]
            xn2 = [sb([D], F32) for _ in range(2)]
            u2T = [sb([8, 128], F32) for _ in range(2)]
            xn2b = [sb([D], BF16) for _ in range(2)]
            statsb = [sb([2, 6], F32) for _ in range(2)]
            mvb = [sb([2], F32) for _ in range(2)]
            rstdb = [sb([1], F32) for _ in range(2)]
            nmrb = [sb([1], F32) for _ in range(2)]
            stats2 = [sb([2, 6], F32) for _ in range(2)]
            mv2 = [sb([2], F32) for _ in range(2)]
            rstd2 = [sb([1], F32) for _ in range(2)]
            nmr2 = [sb([1], F32) for _ in range(2)]
            for kc in range(8):
                dma("sp", wout[:, kc, :], woutb_d[kc * 128:(kc + 1) * 128, :], r=[("woutb_d", kc)], w=["wout"], sem=("wout", kc))
            dma("sp", brow_o, boutb_d, r=["boutb_d"], w=["brow_o"], sem="brow_o")
            dma("sp", ln1gB, ln1_g[l].partition_broadcast(128), r=[], w=["ln1gB"], sem="ln1gB")
            dma("sp", ln1bB, ln1_b[l].partition_broadcast(128), r=[], w=["ln1bB"], sem="ln1bB")
            dma("sp", rw, router_w.rearrange("(k p) e -> p k e", p=128), r=[], w=["rw"], sem="rw", slow=True)
            for blk in range(NB):
                hb = blk % 2
                dma("sp", hTb[hb], hTd[blk], r=[("hTd", blk, 0), ("hTd", blk, 1)], w=[("hTb", hb)], sem=("hTb", hb))
                for t4 in range(4):
                    t = blk * 4 + t4
                    s2 = t % 2
                    Y0 = 2 * s2
                    X0 = 4 + 2 * s2
                    tsl = slice(t4 * 128, (t4 + 1) * 128)
                    dma("sp", xtb[s2], xsrc[t * 128:(t + 1) * 128, :], r=[], w=[("xtb", s2)], sem=("xtb", s2))
                    for hf in range(2):
                        for cc in range(8):
                            mm(bank(Y0 + hf), hTb[hb][:, cc, tsl], wout[:, cc, hf * 512:(hf + 1) * 512], cc == 0, False,
                               r=[("hTb", hb), "wout"], w=[bk(Y0 + hf)])
                        mm(bank(Y0 + hf), ones_row, brow_o[:, hf * 512:(hf + 1) * 512], False, True, r=["ones_row", "brow_o"], w=[bk(Y0 + hf)])
                    stt(zt[s2], xtb[s2], ALPHA, ps[:, Y0:Y0 + 2, :].rearrange("p a b -> p (a b)"), ALU.mult, ALU.add,
                        r=[("xtb", s2), bk(Y0), bk(Y0 + 1)], w=[("zt", s2)])
                    ln_stats(zt[s2], statsb[s2], mvb[s2], rstdb[s2], nmrb[s2], ("pl1", s2), [("zt", s2)])
                    act(x1[s2], zt[s2], AF.Identity, r=[("zt", s2), (("pl1", s2), "rstd"), (("pl1", s2), "nmr")], w=[("x1", s2)],
                        bias=nmrb[s2], scale=rstdb[s2])
                    tt("dve", x1[s2], x1[s2], ln1gB, ALU.mult, r=[("x1", s2), "ln1gB"], w=[("x1", s2)])
                    tt("pool", x1[s2], x1[s2], ln1bB, ALU.add, r=[("x1", s2), "ln1bB"], w=[("x1", s2)])
                    dma("sp", x1d[t * 128:(t + 1) * 128, :], x1[s2], r=[("x1", s2)], w=[("x1d", t)], sem=("x1st", s2))
                    ln_stats(x1[s2], stats2[s2], mv2[s2], rstd2[s2], nmr2[s2], ("ln2", s2), [("x1", s2)])
                    act(xn2[s2], x1[s2], AF.Identity, r=[("x1", s2), (("ln2", s2), "rstd"), (("ln2", s2), "nmr")], w=[("xn2", s2)],
                        bias=nmr2[s2], scale=rstd2[s2])
                    for kc in range(8):
                        tr(bank(X0 + kc // 4)[:, (kc % 4) * 128:(kc % 4 + 1) * 128], xn2[s2][:, kc * 128:(kc + 1) * 128], identF,
                           r=[("xn2", s2), "identF"], w=[bk(X0 + kc // 4)])
                    for kc in range(8):
                        act(u2T[s2][:, kc, :], bank(X0 + kc // 4)[:, (kc % 4) * 128:(kc % 4 + 1) * 128], AF.Identity,
                            r=[bk(X0 + kc // 4), "modT"], w=[("u2T", s2)], bias=modT[:, 2, kc:kc + 1], scale=modT[:, 3, kc:kc + 1])
                    for kc in range(8):
                        mm(bank(X0)[:, 0:NE], u2T[s2][:, kc, :], rw[:, kc, :], kc == 0, kc == 7, r=[("u2T", s2), "rw"], w=[bk(X0)])
                    cp("dve", logits[:, t * NE:(t + 1) * NE], bank(X0)[:, 0:NE], r=[bk(X0)], w=["logits"])
                    cp("pool", xn2b[s2], xn2[s2], r=[("xn2", s2)], w=[("xn2b", s2)])
                    dma("sp", xn2d[t * 128:(t + 1) * 128, :], xn2b[s2], r=[("xn2b", s2)], w=[("xn2d", t)], sem=("xn2st", s2))
            if dbg:
                dma("sp", logd, logits, r=["logits"], w=["logd"], sem="logd")
                final_keys.append("logd")
            P.barrier()
            if stage == 2:
                final_keys += [("x1d", NT - 1), ("xn2d", NT - 1)]
                break


            sb.reset(layer_mark)
            slAi = sb([NT], mybir.dt.int32)
            slBi = sb([NT], mybir.dt.int32)
            cwA = sb([NT], F32)
            cwB = sb([NT], F32)
            widx = sb([NSTEP], mybir.dt.int32)
            moe_mark = sb.mark()
            NG = NT * 4
            s_t = sb([NT * NE], F32)
            sbv = sb([NT * NE], F32)
            m1 = sb([NG], F32)
            is1 = sb([NT * NE], F32)
            G2 = sb([NT * NE], F32)
            m2 = sb([NG], F32)
            gs = sb([NG], F32)
            gmax = sb([NT], F32)
            gsel = sb([NG], F32)
            top2 = sb([NT * NE], F32)
            den = sb([NT], F32)
            g3 = lambda a: a.rearrange("p (g e) -> p g e", e=4)
            t3 = lambda a: a.rearrange("p (t e) -> p t e", t=NT)
            act(s_t, logits, AF.Sigmoid, r=["logits"], w=["s_t"])
            tt("dve", t3(sbv), t3(s_t), biasB.unsqueeze(1).to_broadcast([128, NT, NE]), ALU.add, r=["s_t", "biasB"], w=["sbv"])
            P.add("dve", lambda: nc.vector.tensor_reduce(out=m1, in_=g3(sbv), axis=AX.X, op=ALU.max), r=["sbv"], w=["m1"])
            tt("dve", g3(is1), g3(sbv), m1.unsqueeze(2).to_broadcast([128, NG, 4]), ALU.is_ge, r=["sbv", "m1"], w=["is1"])
            stt(G2, is1, -1.0e9, sbv, ALU.mult, ALU.add, r=["is1", "sbv"], w=["G2"])
            P.add("dve", lambda: nc.vector.tensor_reduce(out=m2, in_=g3(G2), axis=AX.X, op=ALU.max), r=["G2"], w=["m2"])
            tt("dve", gs, m1, m2, ALU.add, r=["m1", "m2"], w=["gs"])
            P.add("dve", lambda: nc.vector.tensor_reduce(out=gmax, in_=gs.rearrange("p (t g) -> p t g", g=4), axis=AX.X, op=ALU.max),
                  r=["gs"], w=["gmax"])
            tt("dve", gsel.rearrange("p (t g) -> p t g", g=4), gs.rearrange("p (t g) -> p t g", g=4),
               gmax.unsqueeze(2).to_broadcast([128, NT, 4]), ALU.is_ge, r=["gs", "gmax"], w=["gsel"])
            tt("dve", g3(top2), g3(sbv), m2.unsqueeze(2).to_broadcast([128, NG, 4]), ALU.is_ge, r=["sbv", "m2"], w=["top2"])
            tt("dve", g3(top2), g3(top2), gsel.unsqueeze(2).to_broadcast([128, NG, 4]), ALU.mult, r=["top2", "gsel"], w=["top2"])
            tt("dve", top2, top2, s_t, ALU.mult, r=["top2", "s_t"], w=["top2"])
            P.add("dve", lambda: nc.vector.tensor_reduce(out=den, in_=t3(top2), axis=AX.X, op=ALU.add), r=["top2"], w=["den"])
            P.add("dve", lambda: nc.vector.reciprocal(out=den, in_=den), r=["den"], w=["den"])
            tt("dve", t3(comb), t3(top2), den.unsqueeze(2).to_broadcast([128, NT, NE]), ALU.mult, r=["top2", "den"], w=["comb"])
            selb = sb([NT * NE], BF16)
            rank = sb([NT * NE], F32)
            totA = sb([NT * NE], F32)
            totB = sb([NT * NE], F32)
            cnt = sb([NE], F32)
            nst = sb([NE], F32)
            cA = sb([NE], F32)
            cB = sb([NE], F32)
            sstart = sb([NE], F32)
            slot = sb([NT * NE], F32)
            mm_ = sb([NT * NE], F32)
            m2_ = sb([NT * NE], F32)
            slA = sb([NT], F32)
            slB = sb([NT], F32)
            eqm = sb([NT * NE], F32)
            iot = sb([NSTEP], F32)
            ioti = sb([NSTEP], mybir.dt.int32)
            pidx = sb([1], F32)
            pidxi = sb([1], mybir.dt.int32)
            cmp3 = sb([NSTEP * NE], F32)
            eidf = sb([NSTEP], F32)
            s3 = lambda a: a.rearrange("p (t e) -> p t e", t=NSTEP)
            SUb = sb([128], BF16)
            onesb = sb([128], BF16)
            SUf = sb([128], F32)
            ts("dve", selb, comb, 0.0, None, ALU.is_gt, None, r=["comb"], w=["selb"])
            memset("pool", SUf, 1.0, w=["SUf"])
            P.add("pool", lambda: nc.gpsimd.affine_select(out=SUf, in_=SUf, pattern=[[1, 128]], compare_op=ALU.is_ge,
                                                          fill=0.0, base=-1, channel_multiplier=-1), r=["SUf"], w=["SUf"])
            cp("pool", SUb, SUf, r=["SUf"], w=["SUb"])
            memset("pool", onesb, 1.0, w=["onesb"])
            mm(bank(0), SUb, selb, True, True, r=["SUb", "selb"], w=[bk(0)])
            mm(bank(1), onesb, selb, True, True, r=["onesb", "selb"], w=[bk(1)])
            cp("dve", rank, bank(0), r=[bk(0)], w=["rank"])
            cp("act", totA, bank(1), r=[bk(1)], w=["totA"])
            src_, dst_ = totA, totB
            sk, dk_ = "totA", "totB"
            dd = 1
            while dd < NT:
                cp("pool", t3(dst_)[:, 0:dd, :], t3(src_)[:, 0:dd, :], r=[sk], w=[dk_])
                tt("dve", t3(dst_)[:, dd:NT, :], t3(src_)[:, dd:NT, :], t3(src_)[:, 0:NT - dd, :], ALU.add, r=[sk], w=[dk_])
                src_, dst_ = dst_, src_
                sk, dk_ = dk_, sk
                dd *= 2
            incl, inclk = src_, sk
            other, otherk = dst_, dk_
            cp("dve", cnt, t3(incl)[:, NT - 1, :], r=[inclk], w=["cnt"])
            tt("dve", rank, rank, incl, ALU.add, r=["rank", inclk], w=["rank"])
            cp("act", other, bank(1), r=[bk(1), otherk], w=[otherk])
            tt("dve", rank, rank, other, ALU.subtract, r=["rank", otherk], w=["rank"])
            memset("pool", nst, 0.0, w=["nst"])
            for jj in range(T // SL):
                stt(nst, cnt, float(SL * jj), nst, ALU.is_gt, ALU.add, r=["cnt", "nst"], w=["nst"])
            src_, dst_, sk, dk_ = nst, cA, "nst", "cA"
            first = True
            dd = 1
            cp("dve", cB, nst, r=["nst"], w=["cB"])
            src_, sk = cB, "cB"
            dst_, dk_ = cA, "cA"
            while dd < NE:
                cp("pool", dst_[:, 0:dd], src_[:, 0:dd], r=[sk], w=[dk_])
                tt("dve", dst_[:, dd:NE], src_[:, dd:NE], src_[:, 0:NE - dd], ALU.add, r=[sk], w=[dk_])
                src_, dst_ = dst_, src_
                sk, dk_ = dk_, sk
                dd *= 2
            send, sendk = src_, sk
            tt("dve", sstart, send, nst, ALU.subtract, r=[sendk, "nst"], w=["sstart"])
            ts("dve", sstart, sstart, float(SL), 1.0, ALU.mult, ALU.add, r=["sstart"], w=["sstart"])
            tt("dve", t3(slot), t3(rank), sstart.unsqueeze(1).to_broadcast([128, NT, NE]), ALU.add, r=["rank", "sstart"], w=["slot"])
            tt("dve", mm_, slot, selb, ALU.mult, r=["slot", "selb"], w=["mm_"])
            P.add("dve", lambda: nc.vector.tensor_reduce(out=slB, in_=t3(mm_), axis=AX.X, op=ALU.max), r=["mm_"], w=["slB"])
            ts("dve", m2_, selb, -1.0e7, 1.0e7, ALU.mult, ALU.add, r=["selb"], w=["m2_"])
            tt("dve", m2_, m2_, mm_, ALU.add, r=["m2_", "mm_"], w=["m2_"])
            P.add("dve", lambda: nc.vector.tensor_reduce(out=slA, in_=t3(m2_), axis=AX.X, op=ALU.min), r=["m2_"], w=["slA"])
            tt("dve", t3(eqm), t3(mm_), slA.unsqueeze(2).to_broadcast([128, NT, NE]), ALU.is_equal, r=["mm_", "slA"], w=["eqm"])
            tt("dve", eqm, eqm, comb, ALU.mult, r=["eqm", "comb"], w=["eqm"])
            P.add("dve", lambda: nc.vector.tensor_reduce(out=cwA, in_=t3(eqm), axis=AX.X, op=ALU.add), r=["eqm"], w=["cwA"])
            tt("dve", t3(eqm), t3(mm_), slB.unsqueeze(2).to_broadcast([128, NT, NE]), ALU.is_equal, r=["mm_", "slB"], w=["eqm"])
            tt("dve", eqm, eqm, comb, ALU.mult, r=["eqm", "comb"], w=["eqm"])
            P.add("dve", lambda: nc.vector.tensor_reduce(out=cwB, in_=t3(eqm), axis=AX.X, op=ALU.add), r=["eqm"], w=["cwB"])
            ts("dve", slA, slA, -1.0, None, ALU.add, None, r=["slA"], w=["slA"])
            ts("dve", slB, slB, -1.0, None, ALU.add, None, r=["slB"], w=["slB"])
            cp("dve", slAi, slA, r=["slA"], w=["slAi"])
            cp("dve", slBi, slB, r=["slB"], w=["slBi"])
            P.add("pool", lambda: nc.gpsimd.iota(ioti, pattern=[[1, NSTEP]], base=0, channel_multiplier=0), w=["ioti"], cost=0.3)
            cp("dve", iot, ioti, r=["ioti"], w=["iot"])
            P.add("pool", lambda: nc.gpsimd.iota(pidxi, pattern=[[1, 1]], base=0, channel_multiplier=1), w=["pidxi"], cost=0.3)
            cp("dve", pidx, pidxi, r=["pidxi"], w=["pidx"])
            tt("dve", s3(cmp3), send.unsqueeze(1).to_broadcast([128, NSTEP, NE]), iot.unsqueeze(2).to_broadcast([128, NSTEP, NE]), ALU.is_le,
               r=[sendk, "iot"], w=["cmp3"])
            P.add("dve", lambda: nc.vector.tensor_reduce(out=eidf, in_=s3(cmp3), axis=AX.X, op=ALU.add), r=["cmp3"], w=["eidf"])
            ts("dve", eidf, eidf, 15.0, 128.0, ALU.min, ALU.mult, r=["eidf"], w=["eidf"])
            memset("pool", iot, 0.0, w=["iot2"])
            tt("dve", iot[:, 1:NSTEP], eidf[:, 1:NSTEP], eidf[:, 0:NSTEP - 1], ALU.is_equal, r=["eidf", "iot2", "cmp3"], w=["iot2"])
            ts("dve", eidf, eidf, pidx, float(l * NE * 128), ALU.add, ALU.add, r=["eidf", "pidx", "iot2"], w=["eidf"])
            stt(eidf, iot, 1.0e6, eidf, ALU.mult, ALU.add, r=["iot2", "eidf"], w=["eidf"])
            cp("dve", widx, eidf, r=["eidf"], w=["widx"])
            if dbg:
                dma("sp", combd, comb, r=["comb"], w=["combd"], sem="combd")
                dma("sp", slotd[:, 0:NT], slA, r=["slA"], w=["slotd0"], sem="slotd0")
                dma("sp", slotd[:, NT:2 * NT], slB, r=["slB"], w=["slotd1"], sem="slotd1")
                dma("sp", slotd[:, 2 * NT:3 * NT], cwA, r=["cwA"], w=["slotd2"], sem="slotd2")
                dma("sp", slotd[:, 3 * NT:4 * NT], eidf[:, 0:NT], r=["eidf"], w=["slotd3"], sem="slotd3")
                final_keys += ["combd", "slotd0", "slotd1", "slotd2", "slotd3"]
            P.barrier()
            if stage == 3:
                break

            sb.reset(moe_mark)
            NSUB = SL // 128
            wgs = sb([8, 512], F32)
            wus = sb([8, 512], F32)
            wds = sb([4, D], F32)
            wg = [sb([8, 512], BF16) for _ in range(2)]
            wu = [sb([8, 512], BF16) for _ in range(2)]
            wd = [sb([4, D], BF16) for _ in range(2)]
            ugm = [sb([D], BF16) for _ in range(2 * NSUB)]
            ugT = [sb([8, SL], BF16) for _ in range(2)]
            sgt = [sb([SL], F32) for _ in range(2)]
            hTm = [sb([4, SL], BF16) for _ in range(2)]
            ysb = [sb([D], F32) for _ in range(2)]
            xg = [sb([D], BF16) for _ in range(2)]
            for t in range(NT):
                s2 = t % 2
                dma("sp", xg[s2], xn2d[t * 128:(t + 1) * 128, :], r=[("xn2d", t)], w=[("xg", s2)], sem=("xg", s2))
                for which, idxt, ik in ((0, slAi, "slAi"), (1, slBi, "slBi")):
                    def sc(idxt=idxt, t=t, s2=s2):
                        return nc.gpsimd.indirect_dma_start(out=Ud[:, :], out_offset=bass.IndirectOffsetOnAxis(ap=idxt[:, t:t + 1], axis=0),
                                                            in_=xg[s2][:, :], in_offset=None)
                    P.dma("pool", sc, r=[("xg", s2), ik], w=[("Ud", t, which)], sem=("usc", s2, which), nbytes=262144)
            udk = [("Ud", t, which) for t in range(NT) for which in range(2)]
            wgl = w_gate.rearrange("l e (p j) f -> (l e p) (j f)", j=8)
            wul = w_up.rearrange("l e (p j) f -> (l e p) (j f)", j=8)
            wdl = w_down.rearrange("l e (p j) n -> (l e p) (j n)", j=4)
            ycnt = 0
            for i in range(NSTEP):
                ws = i % 2
                for (stg, srcw, key, nb_) in ((wgs, wgl, "wgs", 800000), (wus, wul, "wus", 800000), (wds, wdl, "wds", 800000)):
                    def gw(stg=stg, srcw=srcw, i=i):
                        return nc.gpsimd.indirect_dma_start(out=stg.rearrange("p a b -> p (a b)"), out_offset=None, in_=srcw,
                                                            in_offset=bass.IndirectOffsetOnAxis(ap=widx[:, i:i + 1], axis=0),
                                                            bounds_check=bcreg, oob_is_err=False)
                    P.dma("pool", gw, r=["widx"], w=[key], sem=key, nbytes=nb_)
                cp("dve", wg[ws][:, 0:4, :], wgs[:, 0:4, :], r=["wgs"], w=[("wg", ws)])
                cp("act", wg[ws][:, 4:8, :], wgs[:, 4:8, :], r=["wgs"], w=[("wg", ws)])
                cp("pool", wu[ws][:, 0:2, :], wus[:, 0:2, :], r=["wus"], w=[("wu", ws)])
                cp("act", wu[ws][:, 2:8, :], wus[:, 2:8, :], r=["wus"], w=[("wu", ws)])
                tt("dve", wd[ws][:, 0:3, :], wds[:, 0:3, :], g2B.unsqueeze(1).to_broadcast([128, 3, D]), ALU.mult, r=["wds", "g2B"], w=[("wd", ws)])
                tt("pool", wd[ws][:, 3:4, :], wds[:, 3:4, :], g2B.unsqueeze(1).to_broadcast([128, 1, D]), ALU.mult, r=["wds", "g2B"], w=[("wd", ws)])
                uo = (i % 2) * NSUB
                for j4 in range(NSUB):
                    r0 = i * SL + j4 * 128
                    dma("sp", ugm[uo + j4], Ud[r0:r0 + 128, :], r=udk, w=[("ugm", uo + j4)], sem=("ugm", uo + j4))
                JPB = 1024 // SL
                for jg in range(8 // JPB):
                    hb_ = jg % 2
                    for hh in range(JPB):
                        j = jg * JPB + hh
                        dstp = bankb(hb_)[:, hh * SL:(hh + 1) * SL]
                        for j4 in range(NSUB):
                            tr(dstp[:, j4 * 128:(j4 + 1) * 128], ugm[uo + j4][:, j:D:8], identB, r=[("ugm", uo + j4), "identB"], w=[bk(hb_)])
                    for hh in range(JPB):
                        j = jg * JPB + hh
                        dstp = bankb(hb_)[:, hh * SL:(hh + 1) * SL]
                        act(ugT[ws][:, j, :], dstp, AF.Identity, r=[bk(hb_), "modP"], w=[("ugT", ws, j)],
                            bias=modP[:, 0, j:j + 1], scale=modP[:, 1, j:j + 1])
                for fc in range(4):
                    bg, bu = 2 + fc % 2, 4 + fc % 2
                    for j in range(8):
                        mm(bank(bg)[:, 0:SL], wg[ws][:, j, fc:512:4], ugT[ws][:, j, :], j == 0, j == 7, r=[("wg", ws), ("ugT", ws, j)], w=[bk(bg)])
                    for j in range(8):
                        mm(bank(bu)[:, 0:SL], wu[ws][:, j, fc:512:4], ugT[ws][:, j, :], j == 0, j == 7, r=[("wu", ws), ("ugT", ws, j)], w=[bk(bu)])
                    act(sgt[fc % 2], bank(bg)[:, 0:SL], AF.Silu, r=[bk(bg)], w=[("sgt", fc % 2)])
                    tt("dve", hTm[ws][:, fc, :], sgt[fc % 2], bank(bu)[:, 0:SL], ALU.mult, r=[("sgt", fc % 2), bk(bu)], w=[("hTm", ws, fc)])
                for j4 in range(NSUB):
                    y2 = ycnt % 2
                    ycnt += 1
                    for hf in range(2):
                        for fc in range(4):
                            mm(bank(6 + hf), hTm[ws][:, fc, j4 * 128:(j4 + 1) * 128], wd[ws][:, fc, hf * 512:(hf + 1) * 512], fc == 0, fc == 3,
                               r=[("hTm", ws, fc), ("wd", ws)], w=[bk(6 + hf)])
                    cp("act", ysb[y2][:, 0:512], bank(6), r=[bk(6)], w=[("ysb", y2, 0)])
                    cp("dve", ysb[y2][:, 512:1024], bank(7), r=[bk(7)], w=[("ysb", y2, 1)])
                    r0 = i * SL + j4 * 128
                    dma("sp", Yd[r0:r0 + 128, :], ysb[y2], r=[("ysb", y2, 0), ("ysb", y2, 1)], w=[("Yd", i, j4)], sem=("yst", y2))
            P.barrier()

            sb.reset(moe_mark)
            ln2gB = sb([D], F32)
            ln2bB = sb([D], F32)
            ya = [sb([D], F32) for _ in range(2)]
            yb = [sb([D], F32) for _ in range(2)]
            xe = [sb([D], F32) for _ in range(2)]
            ze = [sb([D], F32) for _ in range(2)]
            xo = [sb([D], F32) for _ in range(2)]
            statse = [sb([2, 6], F32) for _ in range(2)]
            mve = [sb([2], F32) for _ in range(2)]
            rstde = [sb([1], F32) for _ in range(2)]
            nmre = [sb([1], F32) for _ in range(2)]
            dma("sp", ln2gB, ln2_g[l].partition_broadcast(128), r=[], w=["ln2gB"], sem="ln2gB")
            dma("sp", ln2bB, ln2_b[l].partition_broadcast(128), r=[], w=["ln2bB"], sem="ln2bB")
            for tg in range(NT):
                s2 = tg % 2
                for (dst, idxt, ik, key) in ((ya[s2], slAi, "slAi", ("ya", s2)), (yb[s2], slBi, "slBi", ("yb", s2))):
                    def gy(dst=dst, idxt=idxt, tg=tg):
                        return nc.gpsimd.indirect_dma_start(out=dst[:, :], out_offset=None, in_=Yd[:, :],
                                                            in_offset=bass.IndirectOffsetOnAxis(ap=idxt[:, tg:tg + 1], axis=0))
                    P.dma("pool", gy, r=[ik], w=[key], sem=key, nbytes=524288)
                dma("sp", xe[s2], x1d[tg * 128:(tg + 1) * 128, :], r=[("x1d", tg)], w=[("xe", s2)], sem=("xe", s2))
                act(ya[s2], ya[s2], AF.Identity, r=[("ya", s2), "cwA"], w=[("ya", s2)], scale=cwA[:, tg:tg + 1])
                stt(ze[s2], yb[s2], cwB[:, tg:tg + 1], ya[s2], ALU.mult, ALU.add, r=[("yb", s2), "cwB", ("ya", s2)], w=[("ze", s2)])
                stt(ze[s2], xe[s2], ALPHA, ze[s2], ALU.mult, ALU.add, r=[("xe", s2), ("ze", s2)], w=[("ze", s2)])
                ln_stats(ze[s2], statse[s2], mve[s2], rstde[s2], nmre[s2], ("pl2", s2), [("ze", s2)])
                act(xo[s2], ze[s2], AF.Identity, r=[("ze", s2), (("pl2", s2), "rstd"), (("pl2", s2), "nmr")], w=[("xo", s2)],
                    bias=nmre[s2], scale=rstde[s2])
                tt("dve", xo[s2], xo[s2], ln2gB, ALU.mult, r=[("xo", s2), "ln2gB"], w=[("xo", s2)])
                tt("pool", xo[s2], xo[s2], ln2bB, ALU.add, r=[("xo", s2), "ln2bB"], w=[("xo", s2)])
                dma("sp", xdst[tg * 128:(tg + 1) * 128, :], xo[s2], r=[("xo", s2)], w=[("xdst", l, tg)], sem=("xost", s2))
                if l == nlayers - 1 or stage == 4:
                    final_keys.append(("xdst", l, tg))
            if l + 1 < nlayers and stage > 4:
                phase0(l + 1)
            P.barrier()
            if stage == 4:
                break
        P.emit(top, final_keys=final_keys)
        build.stats = dict(P.stats, sb_peak=sb.peak)
        build.seg_busy = getattr(P, "seg_busy", [])
    return nc


_IN_NAMES = ["ada_w", "ada_b", "w_in", "b_in", "hgrn_lb", "hgrn_norm_w", "conv_w", "conv_b", "conv_ln_g", "conv_ln_b",
             "w_out", "b_out", "ln1_g", "ln1_b", "router_w", "router_bias", "w_gate", "w_up", "w_down", "ln2_g", "ln2_b"]


def make_in_maps(inputs, ncores=NCORES):
    shared = {k: np.ascontiguousarray(np.asarray(inputs[k], dtype=np.float32)) for k in _IN_NAMES}
    x = np.asarray(inputs["x"], dtype=np.float32)
    c = np.asarray(inputs["c"], dtype=np.float32)
    maps = []
    for i in range(ncores):
        m = dict(shared)
        m["x"] = np.ascontiguousarray(x[i])
        m["c"] = np.ascontiguousarray(c[i])
        maps.append(m)
    return maps


def kernel(**inputs):
    nc = build()
    in_maps = make_in_maps(inputs)
    res = run_bass_kernel_spmd(nc, in_maps, core_ids=list(range(NCORES)))
    return np.stack([np.asarray(r["out"], dtype=np.float32) for r in res.results], axis=0)
```
